# Optimizing a Trainium2 kernel written in Bass

```python
import math
import jax, jax.numpy as jnp
from jax import lax
import numpy as np

D_MODEL = 2048
BATCH = 1
SEQ = 16384
DEPTH = 2

SB_HEADS = 8
SB_HEAD_DIM = 128
SB_WIDTH = SB_HEADS * SB_HEAD_DIM
Q_BLOCK = 128
S5_WIDTH = 1024
S5_GROUP = 16
S5_GROUPS = S5_WIDTH // S5_GROUP
S5_STATE = 64
ML_HEADS = 4
ML_HEAD_DIM = 256
ML_WIDTH = ML_HEADS * ML_HEAD_DIM
ML_CHUNK = 64
ML_CONV = 4
N_GROUPS = 4
EXPERTS_PER_GROUP = 8
N_EXPERTS = N_GROUPS * EXPERTS_PER_GROUP
TOP_K = 2
EXPERT_FF = 256
EPS = 1e-6

IN_SIZES = (SB_WIDTH, SB_WIDTH, SB_WIDTH,
            S5_WIDTH,
            ML_WIDTH, ML_WIDTH, ML_WIDTH, ML_WIDTH,
            ML_HEADS, ML_HEADS,
            D_MODEL, D_MODEL, D_MODEL)
IN_COLS = sum(IN_SIZES)

kernel_name = "hybrid_sb_s5_mlstm_hmoe_adaln"


def rms_norm(x, g):
    xf = x.astype(jnp.float32)
    y = xf * lax.rsqrt(jnp.mean(xf * xf, axis=-1, keepdims=True) + EPS)
    return (y * g.astype(jnp.float32)).astype(x.dtype)


def modulate(h, shift, scale):
    return h * (1.0 + scale[:, None, :]) + shift[:, None, :]


def stick_breaking_attention(q, k, v):
    bsz, seq, nh, dh = q.shape
    nb = seq // Q_BLOCK
    qb = q.reshape(bsz, nb, Q_BLOCK, nh, dh).transpose(1, 0, 3, 2, 4)
    kpos = jnp.arange(seq)
    scale = dh ** -0.5

    def block(args):
        qi, bi = args
        qpos = bi * Q_BLOCK + jnp.arange(Q_BLOCK)
        z = jnp.einsum('bhqd,bshd->bhqs', qi, k).astype(jnp.float32) * scale
        causal = kpos[None, :] < qpos[:, None]
        log_beta = jax.nn.log_sigmoid(z)
        log_keep = jnp.where(causal, jax.nn.log_sigmoid(-z), 0.0)
        rest = lax.cumsum(log_keep, axis=3, reverse=True) - log_keep
        w = jnp.where(causal, jnp.exp(log_beta + rest), 0.0)
        return jnp.einsum('bhqs,bshd->bhqd', w.astype(v.dtype), v)

    out = lax.map(block, (qb, jnp.arange(nb)))
    return out.transpose(1, 0, 3, 2, 4).reshape(bsz, seq, nh * dh)


def _linear_recurrence_combine(left, right):
    a_l, b_l = left
    a_r, b_r = right
    return a_r * a_l, a_r * b_l + b_r


def s5_layer(u, lam_re, lam_im, log_dt, b_re, b_im, c_re, c_im, d_skip, w_glu):
    bsz, seq, _ = u.shape
    f32 = jnp.float32
    uf = u.astype(f32).reshape(bsz, seq, S5_GROUPS, S5_GROUP)
    lam = lax.complex(lam_re.astype(f32), lam_im.astype(f32))
    dt = jnp.exp(log_dt.astype(f32))[:, None]
    lam_bar = jnp.exp(lam * dt)
    b_bar = ((lam_bar - 1.0) / lam)[:, :, None] * lax.complex(b_re.astype(f32), b_im.astype(f32))
    bu = jnp.einsum('bsgh,gph->bsgp', uf.astype(jnp.complex64), b_bar)
    a = jnp.broadcast_to(lam_bar, bu.shape)
    _, states = lax.associative_scan(_linear_recurrence_combine, (a, bu), axis=1)
    c_mat = lax.complex(c_re.astype(f32), c_im.astype(f32))
    y = jnp.einsum('bsgp,ghp->bsgh', states, c_mat).real \
        + d_skip.astype(f32).reshape(S5_GROUPS, S5_GROUP) * uf
    y = jax.nn.gelu(y.reshape(bsz, seq, S5_WIDTH))
    y = y * jax.nn.sigmoid(y @ w_glu.astype(f32))
    return y.astype(u.dtype)


def causal_depthwise_conv(x, w):
    width, ch = w.shape
    return lax.conv_general_dilated(x, w[:, None, :].astype(x.dtype), window_strides=(1,),
                                    padding=[(width - 1, 0)],
                                    dimension_numbers=('NWC', 'WIO', 'NWC'),
                                    feature_group_count=ch)


def mlstm_chunkwise(q, k, v, i_pre, f_pre):
    bsz, seq, nh, dh = q.shape
    f32 = jnp.float32
    L = ML_CHUNK
    nc = seq // L
    q = q.astype(f32).reshape(bsz, nc, L, nh, dh)
    k = k.astype(f32).reshape(bsz, nc, L, nh, dh) * (dh ** -0.5)
    v = v.astype(f32).reshape(bsz, nc, L, nh, dh)
    log_i = i_pre.astype(f32).reshape(bsz, nc, L, nh)
    log_f = jax.nn.log_sigmoid(f_pre.astype(f32)).reshape(bsz, nc, L, nh)
    b = jnp.cumsum(log_f, axis=2)
    b_tot = b[:, :, -1]
    a = b_tot[:, :, None] - b + log_i
    m_loc = jnp.max(a, axis=2)
    wa = jnp.exp(a - m_loc[:, :, None])
    d_c = jnp.einsum('bclh,bclhv,bclhk->bchvk', wa, v, k)
    d_n = jnp.einsum('bclh,bclhk->bchk', wa, k)

    def step(carry, inp):
        c_st, n_st, m_st = carry
        dc, dn, ml, bt = inp
        m_new = jnp.maximum(bt + m_st, ml)
        s_old = jnp.exp(bt + m_st - m_new)
        s_new = jnp.exp(ml - m_new)
        c_new = s_old[..., None, None] * c_st + s_new[..., None, None] * dc
        n_new = s_old[..., None] * n_st + s_new[..., None] * dn
        return (c_new, n_new, m_new), (c_st, n_st, m_st)

    init = (jnp.zeros((bsz, nh, dh, dh), f32), jnp.zeros((bsz, nh, dh), f32), jnp.zeros((bsz, nh), f32))
    _, (c0, n0, m0) = lax.scan(step, init, (jnp.moveaxis(d_c, 1, 0), jnp.moveaxis(d_n, 1, 0),
                                             jnp.moveaxis(m_loc, 1, 0), jnp.moveaxis(b_tot, 1, 0)))
    c0 = jnp.moveaxis(c0, 0, 1)
    n0 = jnp.moveaxis(n0, 0, 1)
    m0 = jnp.moveaxis(m0, 0, 1)
    tril = jnp.tril(jnp.ones((L, L), dtype=bool))
    log_d = b[:, :, :, None, :] - b[:, :, None, :, :] + log_i[:, :, None, :, :]
    log_d = jnp.where(tril[None, None, :, :, None], log_d, -jnp.inf)
    inter_log = b + m0[:, :, None, :]
    m_t = jnp.maximum(inter_log, jnp.max(log_d, axis=3))
    d_w = jnp.exp(log_d - m_t[:, :, :, None, :])
    w_inter = jnp.exp(inter_log - m_t)
    s_mat = d_w * jnp.einsum('bclhd,bcshd->bclsh', q, k)
    num = jnp.einsum('bclsh,bcshv->bclhv', s_mat, v) \
        + w_inter[..., None] * jnp.einsum('bchvk,bclhk->bclhv', c0, q)
    den = jnp.sum(s_mat, axis=3) + w_inter * jnp.einsum('bchk,bclhk->bclh', n0, q)
    h = num / jnp.maximum(jnp.abs(den), jnp.exp(-m_t))[..., None]
    return h.reshape(bsz, seq, nh * dh)


def hierarchical_moe(h, w_group, b_group, w_router, b_router, w1, w3, w2):
    bsz, seq, dm = h.shape
    t = h.reshape(-1, dm)
    g_logits = (t @ w_group).astype(jnp.float32) + b_group.astype(jnp.float32)
    g_prob = jax.nn.softmax(g_logits, axis=-1)
    g_idx = jnp.argmax(g_logits, axis=-1)
    g_w = jnp.take_along_axis(g_prob, g_idx[:, None], axis=-1)
    e_logits = ((t @ w_router).astype(jnp.float32) + b_router.astype(jnp.float32)).reshape(-1, N_GROUPS, EXPERTS_PER_GROUP)
    e_in_group = jnp.take_along_axis(e_logits, g_idx[:, None, None], axis=1)[:, 0]
    top_v, top_i = lax.top_k(e_in_group, TOP_K)
    top_w = jax.nn.softmax(top_v, axis=-1) * g_w
    within = jnp.sum(jax.nn.one_hot(top_i, EXPERTS_PER_GROUP, dtype=jnp.float32) * top_w[..., None], axis=1)
    gates = (jax.nn.one_hot(g_idx, N_GROUPS, dtype=jnp.float32)[:, :, None] * within[:, None, :]).reshape(-1, N_EXPERTS)
    hid = jax.nn.silu(jnp.einsum('td,edf->tef', t, w1)) * jnp.einsum('td,edf->tef', t, w3)
    y = jnp.einsum('tef,efd->td', hid * gates.astype(hid.dtype)[:, :, None], w2)
    return y.reshape(bsz, seq, dm)


def setup_inputs(seed: int = 0) -> dict:
    key = jax.random.key(seed)
    ks = jax.random.split(key, 40)
    f32 = jnp.float32
    L = DEPTH
    D = D_MODEL

    def nrm(k, shape, scale):
        return jax.random.normal(k, shape, f32) * scale

    n_idx = jnp.arange(S5_STATE, dtype=f32)
    return {
        'x': nrm(ks[0], (BATCH, SEQ, D), 1.0),
        'c': nrm(ks[1], (BATCH, D), 1.0),
        'norm_mix_g': 1.0 + nrm(ks[2], (L, D), 0.02),
        'norm_moe_g': 1.0 + nrm(ks[3], (L, D), 0.02),
        'final_g': 1.0 + nrm(ks[4], (D,), 0.02),
        'w_ada': nrm(ks[5], (L, D, 6 * D), 0.5 * D ** -0.5),
        'b_ada': nrm(ks[6], (L, 6 * D), 0.02),
        'w_in': nrm(ks[7], (L, D, IN_COLS), D ** -0.5),
        'ml_conv': nrm(ks[8], (L, ML_CONV, 2 * ML_WIDTH), 0.5),
        'ml_i_bias': nrm(ks[9], (L, ML_HEADS), 0.1),
        'ml_f_bias': jnp.linspace(3.0, 6.0, ML_HEADS, dtype=f32)[None, :] + nrm(ks[10], (L, ML_HEADS), 0.1),
        's5_lam_re': -0.5 + nrm(ks[11], (L, S5_GROUPS, S5_STATE), 0.01),
        's5_lam_im': math.pi * n_idx[None, None, :] + nrm(ks[12], (L, S5_GROUPS, S5_STATE), 0.01),
        's5_log_dt': jax.random.uniform(ks[13], (L, S5_GROUPS), f32, math.log(1e-3), math.log(1e-1)),
        's5_b_re': nrm(ks[14], (L, S5_GROUPS, S5_STATE, S5_GROUP), (2 * S5_GROUP) ** -0.5),
        's5_b_im': nrm(ks[15], (L, S5_GROUPS, S5_STATE, S5_GROUP), (2 * S5_GROUP) ** -0.5),
        's5_c_re': nrm(ks[16], (L, S5_GROUPS, S5_GROUP, S5_STATE), S5_STATE ** -0.5),
        's5_c_im': nrm(ks[17], (L, S5_GROUPS, S5_GROUP, S5_STATE), S5_STATE ** -0.5),
        's5_d': nrm(ks[18], (L, S5_WIDTH), 0.5),
        's5_w_glu': nrm(ks[19], (L, S5_WIDTH, S5_WIDTH), S5_WIDTH ** -0.5),
        'p_sb': nrm(ks[20], (L, SB_WIDTH, D), SB_WIDTH ** -0.5),
        'p_s5': nrm(ks[21], (L, S5_WIDTH, D), S5_WIDTH ** -0.5),
        'p_ml': nrm(ks[22], (L, ML_WIDTH, D), ML_WIDTH ** -0.5),
        'w_out': nrm(ks[23], (L, D, D), D ** -0.5),
        'moe_w_group': nrm(ks[24], (L, D, N_GROUPS), D ** -0.5),
        'moe_b_group': nrm(ks[25], (L, N_GROUPS), 0.01),
        'moe_w_router': nrm(ks[26], (L, D, N_EXPERTS), D ** -0.5),
        'moe_b_router': nrm(ks[27], (L, N_EXPERTS), 0.01),
        'moe_w1': nrm(ks[28], (L, N_EXPERTS, D, EXPERT_FF), D ** -0.5),
        'moe_w3': nrm(ks[29], (L, N_EXPERTS, D, EXPERT_FF), D ** -0.5),
        'moe_w2': nrm(ks[30], (L, N_EXPERTS, EXPERT_FF, D), EXPERT_FF ** -0.5),
    }


def reference(x, c, norm_mix_g, norm_moe_g, final_g, w_ada, b_ada, w_in, ml_conv, ml_i_bias, ml_f_bias,
              s5_lam_re, s5_lam_im, s5_log_dt, s5_b_re, s5_b_im, s5_c_re, s5_c_im, s5_d, s5_w_glu,
              p_sb, p_s5, p_ml, w_out, moe_w_group, moe_b_group, moe_w_router, moe_b_router,
              moe_w1, moe_w3, moe_w2):
    bsz, seq, _ = x.shape
    split_points = [int(p) for p in np.cumsum(IN_SIZES)[:-1]]
    c_act = jax.nn.silu(c)
    for l in range(DEPTH):
        mod = c_act @ w_ada[l] + b_ada[l]
        shift1, scale1, gate1, shift2, scale2, gate2 = jnp.split(mod, 6, axis=-1)

        h = modulate(rms_norm(x, norm_mix_g[l]), shift1, scale1)
        proj = h @ w_in[l]
        (sb_q, sb_k, sb_v, s5_u, ml_q, ml_k, ml_v, ml_o, ml_i, ml_f,
         g_sb, g_s5, g_ml) = jnp.split(proj, split_points, axis=-1)

        sb_shape = (bsz, seq, SB_HEADS, SB_HEAD_DIM)
        y_sb = stick_breaking_attention(sb_q.reshape(sb_shape), sb_k.reshape(sb_shape), sb_v.reshape(sb_shape))

        y_s5 = s5_layer(s5_u, s5_lam_re[l], s5_lam_im[l], s5_log_dt[l], s5_b_re[l], s5_b_im[l],
                        s5_c_re[l], s5_c_im[l], s5_d[l], s5_w_glu[l])

        qk = jax.nn.silu(causal_depthwise_conv(jnp.concatenate([ml_q, ml_k], axis=-1), ml_conv[l]))
        ml_q_c, ml_k_c = jnp.split(qk, 2, axis=-1)
        ml_shape = (bsz, seq, ML_HEADS, ML_HEAD_DIM)
        y_ml = mlstm_chunkwise(ml_q_c.reshape(ml_shape), ml_k_c.reshape(ml_shape), ml_v.reshape(ml_shape),
                               ml_i + ml_i_bias[l], ml_f + ml_f_bias[l])
        y_ml = (y_ml * jax.nn.sigmoid(ml_o.astype(jnp.float32))).astype(x.dtype)

        merged = jax.nn.sigmoid(g_sb) * (y_sb @ p_sb[l]) \
            + jax.nn.sigmoid(g_s5) * (y_s5 @ p_s5[l]) \
            + jax.nn.sigmoid(g_ml) * (y_ml @ p_ml[l])
        x = x + gate1[:, None, :] * (merged @ w_out[l])

        h = modulate(rms_norm(x, norm_moe_g[l]), shift2, scale2)
        x = x + gate2[:, None, :] * hierarchical_moe(h, moe_w_group[l], moe_b_group[l], moe_w_router[l],
                                                     moe_b_router[l], moe_w1[l], moe_w3[l], moe_w2[l])
    return rms_norm(x, final_g)
```

```python
import contextlib
import numpy as np
import ml_dtypes
import concourse.bass as bass
import concourse.mybir as mybir
from concourse.bass_utils import run_bass_kernel_spmd

F32 = mybir.dt.float32
BF16 = mybir.dt.bfloat16
AF = mybir.ActivationFunctionType
ALU = mybir.AluOpType
AX = mybir.AxisListType

NCORES = 8
D = 2048
T = 16384
DEPTH = 2
IN_COLS = 14344
EPS = 1e-6


class Buf:
    __slots__ = ("name", "w", "r")

    def __init__(self, name):
        self.name = name
        self.w = None
        self.r = {}


class K:
    NDMA = 12

    def __init__(self, nc, stack):
        self.nc = nc
        self.stack = stack
        self.eng = {"pe": nc.tensor, "act": nc.scalar, "dve": nc.vector, "pool": nc.gpsimd, "sp": nc.sync}
        self.sem = {}
        self.cnt = {}
        for e in self.eng:
            self.sem[e] = stack.enter_context(nc.semaphore("s_" + e))
            self.cnt[e] = 0
        self.dsem = [stack.enter_context(nc.semaphore("s_dma%d" % i)) for i in range(self.NDMA)]
        self.dcnt = [0] * self.NDMA
        self.dnext = 0
        self.waited = {e: {} for e in self.eng}
        self.nbuf = 0

    def sb(self, name, shape, dt):
        return self.stack.enter_context(self.nc.sbuf_tensor(name, list(shape), dt))

    def ps(self, name, shape, dt=F32):
        return self.stack.enter_context(self.nc.psum_tensor(name, list(shape), dt))

    def buf(self, name=None):
        self.nbuf += 1
        return Buf(name or "b%d" % self.nbuf)

    def bufs(self, n, name="b"):
        return [self.buf("%s%d" % (name, i)) for i in range(n)]

    def _semof(self, key):
        if isinstance(key, int):
            return self.dsem[key]
        return self.sem[key]

    def _collect(self, e, ins, outs):
        need = {}

        def add(tok):
            if tok is None:
                return
            k, v = tok
            if need.get(k, 0) < v:
                need[k] = v

        for b in ins:
            add(b.w)
        for b in outs:
            add(b.w)
            for k, v in b.r.items():
                if k == e:
                    continue
                add((k, v))
        return need

    def _emit_waits(self, e, need):
        eng = self.eng[e]
        wd = self.waited[e]
        for k, v in need.items():
            if e == "pe" and k == "pe":
                continue
            if wd.get(k, 0) >= v:
                continue
            eng.wait_ge(self._semof(k), v)
            wd[k] = v

    def _finish(self, tok, ins, outs):
        k, v = tok
        for b in ins:
            if b.r.get(k, 0) < v:
                b.r[k] = v
        for b in outs:
            b.w = tok
            b.r = {}

    def op(self, e, fn, ins=(), outs=()):
        need = self._collect(e, ins, outs)
        self._emit_waits(e, need)
        inst = fn(self.eng[e])
        self.cnt[e] += 1
        inst.then_inc(self.sem[e], 1)
        tok = (e, self.cnt[e])
        self._finish(tok, ins, outs)
        return tok

    def dma(self, out_ap, in_ap, ins=(), outs=(), q="sp", **kw):
        i = self.dnext
        self.dnext = (self.dnext + 1) % self.NDMA
        need = self._collect(q, ins, outs)
        if self.dcnt[i] > 0:
            need[i] = max(need.get(i, 0), self.dcnt[i])
        self._emit_waits(q, need)
        inst = self.eng[q].dma_start(out=out_ap, in_=in_ap, **kw)
        self.dcnt[i] += 16
        inst.then_inc(self.dsem[i], 16)
        tok = (i, self.dcnt[i])
        self._finish(tok, ins, outs)
        return tok

    def cc(self, kind, out_ap, in_ap, ins=(), outs=(), groups=None):
        q = "pool"
        i = self.dnext
        self.dnext = (self.dnext + 1) % self.NDMA
        need = self._collect(q, ins, outs)
        if self.dcnt[i] > 0:
            need[i] = max(need.get(i, 0), self.dcnt[i])
        self._emit_waits(q, need)
        inst = self.nc.gpsimd.collective_compute(kind, ALU.bypass, replica_groups=groups or [list(range(NCORES))],
                                                 ins=[in_ap], outs=[out_ap])
        self.dcnt[i] += 16
        inst.then_inc(self.dsem[i], 16)
        tok = (i, self.dcnt[i])
        self._finish(tok, ins, outs)
        return tok

    def barrier(self):
        need = {}
        for e in ("pe", "act", "dve", "pool", "sp"):
            if self.cnt[e]:
                need[e] = self.cnt[e]
        for i in range(self.NDMA):
            if self.dcnt[i]:
                need[i] = self.dcnt[i]
        need.pop("sp", None)
        for e in ("pe", "act", "dve", "pool", "sp"):
            self._emit_waits(e, dict(need))

    def finish(self, out_bufs):
        need = {}
        for b in out_bufs:
            if b.w is not None:
                k, v = b.w
                need[k] = max(need.get(k, 0), v)
        for e in ("pe", "act", "dve", "pool"):
            if self.cnt[e]:
                need[e] = self.cnt[e]
        for i in range(self.NDMA):
            if self.dcnt[i]:
                need[i] = self.dcnt[i]
        self._emit_waits("sp", need)


def new_nc():
    return bass.Bass("TRN2", target_bir_lowering=False)


def _evac(k, idx, out_ap, in_ap, ins, outs):
    if idx % 2 == 0:
        return k.op("act", lambda g: g.activation(out=out_ap, in_=in_ap, func=AF.Identity), ins=ins, outs=outs)
    return k.op("dve", lambda g: g.tensor_copy(out=out_ap, in_=in_ap), ins=ins, outs=outs)


def emit_modnorm(k, xt, bx, hT_ap_fn, bh, vec, bvec, ones_bf, bones, sq, bsq, psb, bpsb, rstd, brstd, tmp, btmp, NT=512):
    k.op("act", lambda g: g.activation(out=sq[:], in_=xt[:], func=AF.Square), ins=[bx], outs=[bsq])
    for kc in range(16):
        k.op("pe", lambda g, kc=kc: g.matmul(psb[:, 0:NT], lhsT=ones_bf[:], rhs=sq[:, kc, :], start=(kc == 0), stop=(kc == 15)),
             ins=[bones, bsq], outs=[bpsb])
    k.op("act", lambda g: g.activation(out=rstd[:], in_=psb[:, 0:NT], func=AF.Sqrt, scale=1.0 / D, bias=EPS), ins=[bpsb], outs=[brstd])
    k.op("dve", lambda g: g.reciprocal(out=rstd[:], in_=rstd[:]), ins=[brstd], outs=[brstd])
    for kc in range(16):
        k.op("dve", lambda g, kc=kc: g.tensor_tensor(out=tmp[:, kc % 2, :], in0=xt[:, kc, :], in1=rstd[:], op=ALU.mult),
             ins=[bx, brstd], outs=[btmp[kc % 2]])
        k.op("act", lambda g, kc=kc: g.activation(out=hT_ap_fn(kc), in_=tmp[:, kc % 2, :], func=AF.Identity,
                                                  scale=vec[:, 0, kc:kc + 1], bias=vec[:, 1, kc:kc + 1]),
             ins=[btmp[kc % 2], bvec], outs=[bh])


def build_A():
    NTOK = 2048
    nc = new_nc()
    xT = nc.dram_tensor("xT", [D, NTOK], F32, kind="ExternalInput").ap()
    vecs = nc.dram_tensor("vecs", [128, 3, 16], F32, kind="ExternalInput").ap()
    w = nc.dram_tensor("w", [D, IN_COLS], F32, kind="ExternalInput").ap()
    pa = nc.dram_tensor("pa", [8192, NTOK], BF16, kind="ExternalOutput").ap()
    pif = nc.dram_tensor("pif", [8, NTOK], F32, kind="ExternalOutput").ap()
    pg = nc.dram_tensor("pg", [6144, NTOK], F32, kind="ExternalOutput").ap()
    xT_v = xT.rearrange("(kc p) t -> p kc t", p=128)
    w_v = w.rearrange("(kc p) n -> p kc n", p=128)
    with contextlib.ExitStack() as st:
        k = K(nc, st)
        xt = k.sb("xt", [128, 16, 512], F32); bx = k.buf()
        sq = k.sb("sq", [128, 16, 512], BF16); bsq = k.buf()
        hT = k.sb("hT", [128, 16, NTOK], BF16); bh = k.bufs(4, "bh")
        vin = k.sb("vin", [128, 3, 16], F32); bvin = k.buf()
        vec = k.sb("vec", [128, 2, 16], F32); bvec = k.buf()
        ones_bf = k.sb("ones_bf", [128, 128], BF16); bones = k.buf()
        rstd = k.sb("rstd", [128, 512], F32); brstd = k.buf()
        tmp = k.sb("tmp", [128, 2, 512], F32); btmp = k.bufs(2, "btmp")
        wst = [k.sb("wst%d" % i, [128, 16, 128], F32) for i in range(2)]; bwst = k.bufs(2, "bwst")
        wbf = [k.sb("wbf%d" % i, [128, 16, 128], BF16) for i in range(2)]; bwbf = k.bufs(2, "bwbf")
        ost = [k.sb("ost%d" % i, [128, NTOK], F32) for i in range(2)]; bost = k.bufs(2, "bost")
        ostb = [k.sb("ostb%d" % i, [128, NTOK], BF16) for i in range(2)]; bostb = k.bufs(2, "bostb")
        ps = [k.ps("ps%d" % i, [128, 512]) for i in range(8)]; bps = k.bufs(8, "bps")
        bout = k.buf("out")

        k.op("pool", lambda g: g.memset(ones_bf[:], 1.0), outs=[bones])
        k.dma(vin[:], vecs, outs=[bvin])
        k.op("dve", lambda g: g.scalar_tensor_tensor(out=vec[:, 0, :], in0=vin[:, 1, :], scalar=1.0, in1=vin[:, 0, :],
                                                      op0=ALU.add, op1=ALU.mult), ins=[bvin], outs=[bvec])
        k.op("dve", lambda g: g.tensor_copy(out=vec[:, 1, :], in_=vin[:, 2, :]), ins=[bvin, bvec], outs=[bvec])
        for tt in range(4):
            k.dma(xt[:], xT_v[:, :, tt * 512:(tt + 1) * 512], outs=[bx])
            emit_modnorm(k, xt, bx, lambda kc, tt=tt: hT[:, kc, tt * 512:(tt + 1) * 512], bh[tt], vec, bvec, ones_bf, bones,
                         sq, bsq, ps[0], bps[0], rstd, brstd, tmp, btmp)
        tiles = [(n0, 128) for n0 in range(0, 8192, 128)] + [(8192, 8)] + [(n0, 128) for n0 in range(8200, IN_COLS, 128)]
        for ti, (n0, ncol) in enumerate(tiles):
            s = ti % 2
            k.dma(wst[s][:, :, 0:ncol], w_v[:, :, n0:n0 + ncol], outs=[bwst[s]])
            eng = ("dve", "act", "dve", "pool")[ti % 4]
            if eng == "act":
                k.op("act", lambda g, s=s, ncol=ncol: g.activation(out=wbf[s][:, :, 0:ncol], in_=wst[s][:, :, 0:ncol], func=AF.Identity),
                     ins=[bwst[s]], outs=[bwbf[s]])
            else:
                k.op(eng, lambda g, s=s, ncol=ncol: g.tensor_copy(out=wbf[s][:, :, 0:ncol], in_=wst[s][:, :, 0:ncol]),
                     ins=[bwst[s]], outs=[bwbf[s]])
            is_bf = n0 < 8192
            o_t, o_b = (ostb[s], bostb[s]) if is_bf else (ost[s], bost[s])
            for tt in range(4):
                pi = (ti % 2) * 4 + tt
                for kc in range(16):
                    k.op("pe", lambda g, pi=pi, s=s, kc=kc, tt=tt, ncol=ncol: g.matmul(
                        ps[pi][0:ncol, :], lhsT=wbf[s][:, kc, 0:ncol], rhs=hT[:, kc, tt * 512:(tt + 1) * 512],
                        start=(kc == 0), stop=(kc == 15)), ins=[bwbf[s], bh[tt]], outs=[bps[pi]])
                _evac(k, tt, o_t[0:ncol, tt * 512:(tt + 1) * 512], ps[pi][0:ncol, :], [bps[pi]], [o_b])
            if is_bf:
                dst = pa[n0:n0 + ncol, :]
            elif ncol == 8:
                dst = pif[:, :]
            else:
                dst = pg[n0 - 8200:n0 - 8200 + ncol, :]
            k.dma(dst, o_t[0:ncol, :], ins=[o_b], outs=[bout])
        k.finish([bout])
    return nc


def emit_sb(k, qT, bq, kT, bk, vS, bv, y_dram, by, tag="sb"):
    NQ = T // 512
    SC = 128.0 ** -0.5
    tri = k.sb(tag + "tri", [128, 128], BF16); omt = k.sb(tag + "omt", [128, 128], BF16); bconst = k.buf()
    onesf = k.sb(tag + "onesf", [128, 512], F32)
    masks = k.sb(tag + "masks", [128, 4, 512], F32)
    k.op("pool", lambda g: g.memset(onesf[:], 1.0), outs=[bconst])
    k.op("pool", lambda g: g.affine_select(out=tri[:], in_=onesf[:, 0:128], pattern=[[-1, 128]], compare_op=ALU.is_ge,
                                           fill=0.0, base=0, channel_multiplier=1), ins=[bconst], outs=[bconst])
    k.op("pool", lambda g: g.affine_select(out=omt[:], in_=onesf[:, 0:128], pattern=[[1, 128]], compare_op=ALU.is_gt,
                                           fill=0.0, base=0, channel_multiplier=-1), ins=[bconst], outs=[bconst])
    for j in range(4):
        k.op("pool", lambda g, j=j: g.affine_select(out=masks[:, j, :], in_=onesf[:], pattern=[[1, 512]], compare_op=ALU.is_gt,
                                                    fill=0.0, base=-128 * j, channel_multiplier=-1), ins=[bconst], outs=[bconst])

    class Stream:
        pass

    streams = []
    for si in range(2):
        s = Stream()
        s.z = k.ps("%sz%d" % (tag, si), [128, 512]); s.bz = k.buf()
        s.p = k.ps("%sp%d" % (tag, si), [128, 512]); s.bp = k.buf()
        s.y = [k.ps("%sy%d_%d" % (tag, si, i), [128, 512]) for i in range(2)]; s.by = k.bufs(2)
        s.u = [k.sb("%su%d_%d" % (tag, si, i), [128, 512], F32) for i in range(2)]; s.bu = k.bufs(2)
        s.sp = [k.sb("%ssp%d_%d" % (tag, si, i), [128, 512], BF16) for i in range(2)]; s.bsp = k.bufs(2)
        s.ec = [k.sb("%sec%d_%d" % (tag, si, i), [128, 512], BF16) for i in range(2)]; s.bec = k.bufs(2)
        s.w = [k.sb("%sw%d_%d" % (tag, si, i), [128, 512], BF16) for i in range(2)]; s.bw = k.bufs(2)
        s.yo = [k.sb("%syo%d_%d" % (tag, si, i), [128, 512], BF16) for i in range(2)]; s.byo = k.bufs(2)
        s.steps = []
        for qt in range(si, NQ, 2):
            nb = 4 * qt + 4
            for i, b in enumerate(range(nb - 1, -1, -1)):
                s.steps.append((qt, b, i == 0, i == nb - 1))
        streams.append(s)

    def st_z(s, n):
        qt, b, first, last = s.steps[n]
        k.op("pe", lambda g: g.matmul(s.z[:], lhsT=kT[:, b * 128:(b + 1) * 128], rhs=qT[:, qt * 512:(qt + 1) * 512],
                                      start=True, stop=True), ins=[bk, bq], outs=[s.bz])

    def st_u(s, n):
        qt, b, first, last = s.steps[n]
        i = n % 2
        k.op("act", lambda g: g.activation(out=s.u[i][:], in_=s.z[:], func=AF.Exp, scale=SC), ins=[s.bz], outs=[s.bu[i]])
        j = b - 4 * qt
        if j >= 0:
            k.op("dve", lambda g: g.tensor_tensor(out=s.u[i][:], in0=s.u[i][:], in1=masks[:, j, :], op=ALU.mult),
                 ins=[s.bu[i], bconst], outs=[s.bu[i]])

    def st_sp(s, n):
        i = n % 2
        k.op("act", lambda g: g.activation(out=s.sp[i][:], in_=s.u[i][:], func=AF.Ln, bias=1.0), ins=[s.bu[i]], outs=[s.bsp[i]])

    def st_tri(s, n):
        qt, b, first, last = s.steps[n]
        i = n % 2
        k.op("pe", lambda g: g.matmul(s.p[:], lhsT=tri[:], rhs=s.sp[i][:], start=first, stop=True, skip_group_check=True),
             ins=[bconst, s.bsp[i]], outs=[s.bp])

    def st_ec(s, n):
        i = n % 2
        k.op("act", lambda g: g.activation(out=s.ec[i][:], in_=s.p[:], func=AF.Exp, scale=-1.0), ins=[s.bp], outs=[s.bec[i]])

    def st_omt(s, n):
        qt, b, first, last = s.steps[n]
        i = n % 2
        if not last:
            k.op("pe", lambda g: g.matmul(s.p[:], lhsT=omt[:], rhs=s.sp[i][:], start=False, stop=True, skip_group_check=True),
                 ins=[bconst, s.bsp[i]], outs=[s.bp])

    def st_w(s, n):
        i = n % 2
        k.op("dve", lambda g: g.tensor_tensor(out=s.w[i][:], in0=s.u[i][:], in1=s.ec[i][:], op=ALU.mult),
             ins=[s.bu[i], s.bec[i]], outs=[s.bw[i]])

    def st_wv(s, n):
        qt, b, first, last = s.steps[n]
        i = n % 2
        yi = (qt // 2) % 2
        k.op("pe", lambda g: g.matmul(s.y[yi][:], lhsT=vS[:, b, :], rhs=s.w[i][:], start=first, stop=last),
             ins=[bv, s.bw[i]], outs=[s.by[yi]])
        if last:
            k.op("act", lambda g: g.activation(out=s.yo[yi][:], in_=s.y[yi][:], func=AF.Identity), ins=[s.by[yi]], outs=[s.byo[yi]])
            k.dma(y_dram[:, qt * 512:(qt + 1) * 512], s.yo[yi][:], ins=[s.byo[yi]], outs=[by])

    nmax = max(len(s.steps) for s in streams)
    for s in streams:
        st_z(s, 0)
    for n in range(nmax):
        act = [s for s in streams if n < len(s.steps)]
        for s in act:
            st_u(s, n)
        for s in act:
            st_sp(s, n)
        for s in act:
            st_tri(s, n)
        for s in act:
            if n + 1 < len(s.steps):
                st_z(s, n + 1)
        for s in act:
            st_ec(s, n)
        for s in act:
            st_omt(s, n)
        for s in act:
            st_w(s, n)
        for s in act:
            st_wv(s, n)


def build_SB():
    nc = new_nc()
    qTd = nc.dram_tensor("qT", [128, T], BF16, kind="ExternalInput").ap()
    kTd = nc.dram_tensor("kT", [128, T], BF16, kind="ExternalInput").ap()
    vd = nc.dram_tensor("v", [T, 128], BF16, kind="ExternalInput").ap()
    yd = nc.dram_tensor("ysb", [128, T], BF16, kind="ExternalOutput").ap()
    with contextlib.ExitStack() as st:
        k = K(nc, st)
        qT = k.sb("qTs", [128, T], BF16); bq = k.buf()
        kT = k.sb("kTs", [128, T], BF16); bk = k.buf()
        vS = k.sb("vS", [128, T // 128, 128], BF16); bv = k.buf()
        by = k.buf()
        for i in range(4):
            sl = slice(i * 4096, (i + 1) * 4096)
            k.dma(qT[:, sl], qTd[:, sl], outs=[bq]); k.dma(kT[:, sl], kTd[:, sl], outs=[bk])
            k.dma(vS[:, i * 32:(i + 1) * 32, :], vd.rearrange("(b p) d -> p b d", p=128)[:, i * 32:(i + 1) * 32, :], outs=[bv])
        emit_sb(k, qT, bq, kT, bk, vS, bv, yd, by)
        k.finish([by])
    return nc


def emit_ml(k, mqk_d, mv_d, mo_d, gif_d, cw_d, gb_d, y_dram, by, tag="ml"):
    TB = 2048
    NBLK = T // TB
    CPB = TB // 128
    LN16 = float(np.log(1.0 / 16.0))
    identf = k.sb(tag + "identf", [128, 128], F32); onesf = k.sb(tag + "onesf", [128, 128], F32)
    trile = k.sb(tag + "trile", [128, 128], F32); negmask = k.sb(tag + "negmask", [128, 128], F32)
    onesbf = k.sb(tag + "onesbf", [128, 128], BF16); identbf = k.sb(tag + "identbf", [128, 128], BF16)
    zerosf = k.sb(tag + "zerosf", [128, 128], F32)
    bc = k.buf()
    k.op("pool", lambda g: g.memset(onesf[:], 1.0), outs=[bc])
    k.op("pool", lambda g: g.memset(zerosf[:], 0.0), ins=[bc], outs=[bc])
    k.op("pool", lambda g: g.memset(onesbf[:], 1.0), ins=[bc], outs=[bc])
    k.op("pool", lambda g: g.affine_select(out=identf[:], in_=onesf[:], pattern=[[-1, 128]], compare_op=ALU.is_equal,
                                           fill=0.0, base=0, channel_multiplier=1), ins=[bc], outs=[bc])
    k.op("pool", lambda g: g.tensor_copy(out=identbf[:], in_=identf[:]), ins=[bc], outs=[bc])
    k.op("pool", lambda g: g.affine_select(out=trile[:], in_=onesf[:], pattern=[[1, 128]], compare_op=ALU.is_ge,
                                           fill=0.0, base=0, channel_multiplier=-1), ins=[bc], outs=[bc])
    k.op("pool", lambda g: g.affine_select(out=negmask[:], in_=zerosf[:], pattern=[[1, 128]], compare_op=ALU.is_ge,
                                           fill=-30000.0, base=0, channel_multiplier=-1), ins=[bc], outs=[bc])
    gif = k.sb(tag + "gif", [128, 2, 128], F32); bgif = k.buf()
    cw = k.sb(tag + "cw", [128, 4, 4], F32); bcw = k.buf()
    gb = k.sb(tag + "gb", [128, 2], F32); bgb = k.buf()
    k.dma(gif[:], gif_d, outs=[bgif]); k.dma(cw[:], cw_d, outs=[bcw]); k.dma(gb[:], gb_d, outs=[bgb])
    psA = k.ps(tag + "psA", [128, 512]); bpsA = k.buf()
    psB = k.ps(tag + "psB", [128, 512]); bpsB = k.buf()
    Eps = k.ps(tag + "Eps", [128, 512]); bEps = k.buf()
    Sps = k.ps(tag + "Sps", [128, 512]); bSps = k.buf()
    NDps = k.ps(tag + "NDps", [128, 512]); bND = k.buf()
    ktps = k.ps(tag + "ktps", [128, 1024], BF16); bktps = k.buf()
    dCps = k.ps(tag + "dCps", [128, 2, 256]); bdC = k.buf()
    sc = k.sb(tag + "sc", [128, 4], F32); bsc = k.buf()
    k.op("dve", lambda g: g.tensor_scalar(out=sc[:, 0:1], in0=gb[:, 1:2], scalar1=-1.0, scalar2=None, op0=ALU.mult), ins=[bgb], outs=[bsc])
    k.op("dve", lambda g: g.tensor_scalar(out=sc[:, 1:2], in0=gb[:, 0:1], scalar1=LN16, scalar2=None, op0=ALU.add), ins=[bgb, bsc], outs=[bsc])
    lfneg = k.sb(tag + "lfneg", [128, 128], F32); blf = k.buf()
    k.op("act", lambda g: g.activation(out=lfneg[:], in_=gif[:, 1, :], func=AF.Exp, scale=-1.0, bias=sc[:, 0:1]), ins=[bgif, bsc], outs=[blf])
    k.op("act", lambda g: g.activation(out=lfneg[:], in_=lfneg[:], func=AF.Ln, bias=1.0), ins=[blf], outs=[blf])
    k.op("pe", lambda g: g.matmul(psA[:, 0:128], lhsT=trile[:], rhs=lfneg[:], start=True, stop=True), ins=[bc, blf], outs=[bpsA])
    k.op("pe", lambda g: g.matmul(psB[:, 0:128], lhsT=onesf[:], rhs=lfneg[:], start=True, stop=True), ins=[bc, blf], outs=[bpsB])
    imb = k.sb(tag + "imb", [128, 128], F32); negb = k.sb(tag + "negb", [128, 128], F32)
    wa = k.sb(tag + "wa", [128, 128], F32); ebtot = k.sb(tag + "ebtot", [128, 128], F32); bg = k.buf()
    k.op("dve", lambda g: g.scalar_tensor_tensor(out=imb[:], in0=gif[:, 0, :], scalar=sc[:, 1:2], in1=psA[:, 0:128], op0=ALU.add, op1=ALU.add),
         ins=[bgif, bsc, bpsA], outs=[bg])
    k.op("dve", lambda g: g.tensor_scalar(out=negb[:], in0=psA[:, 0:128], scalar1=-1.0, scalar2=None, op0=ALU.mult), ins=[bpsA, bg], outs=[bg])
    k.op("dve", lambda g: g.tensor_tensor(out=wa[:], in0=imb[:], in1=psB[:, 0:128], op=ALU.subtract), ins=[bg, bpsB], outs=[bg])
    k.op("act", lambda g: g.activation(out=wa[:], in_=wa[:], func=AF.Exp), ins=[bg], outs=[bg])
    k.op("act", lambda g: g.activation(out=ebtot[:], in_=psB[:, 0:128], func=AF.Exp, scale=-1.0), ins=[bpsB, bg], outs=[bg])
    Cst = k.sb(tag + "Cst", [128, 2, 129], F32); bCst = k.buf()
    C0bf = k.sb(tag + "C0bf", [128, 2, 128], BF16); n0bc = k.sb(tag + "n0bc", [128, 2, 128], BF16); bC0 = k.buf()
    k.op("pool", lambda g: g.memset(Cst[:], 0.0), outs=[bCst])
    k.op("pool", lambda g: g.memset(C0bf[:], 0.0), outs=[bC0])
    k.op("pool", lambda g: g.memset(n0bc[:], 0.0), ins=[bC0], outs=[bC0])
    xin = [k.sb("%sxin%d" % (tag, i), [128, 4, TB + 4], BF16) for i in range(2)]; bxin = k.bufs(2)
    acc = k.sb(tag + "acc", [128, TB], F32); bacc = k.buf()
    qk = [k.sb("%sqk%d" % (tag, i), [128, 4, TB], BF16) for i in range(2)]; bqk = k.bufs(2)
    vaug = [k.sb("%svaug%d" % (tag, i), [128, CPB, 129], BF16) for i in range(2)]; bva = k.bufs(2)
    mo = [k.sb("%smo%d" % (tag, i), [128, TB], BF16) for i in range(2)]; bmo = k.bufs(2)
    sigo = [k.sb("%ssigo%d" % (tag, i), [128, TB], F32) for i in range(2)]; bsigo = k.bufs(2)
    yb = [k.sb("%syb%d" % (tag, i), [128, TB], BF16) for i in range(2)]; byb = k.bufs(2)
    diagb = [k.sb("%sdiagb%d" % (tag, i), [128, 128], F32) for i in range(2)]; bdiag = k.bufs(2)
    Dt = k.sb(tag + "Dt", [128, 128], F32); bDt = k.buf()
    ebb = k.sb(tag + "ebb", [128, 128], F32); bebb = k.buf()
    Pt = k.sb(tag + "Pt", [128, 128], BF16); bPt = k.buf()
    qtl = k.sb(tag + "qtl", [128, 2, 128], BF16); bqtl = k.buf()
    kt = k.sb(tag + "kt", [128, 256], BF16); bkt = k.buf()
    dn = k.sb(tag + "dn", [128, 128], F32); bdn = k.buf()
    hh = k.sb(tag + "hh", [128, 128], F32); bhh = k.buf()
    for i in range(2):
        k.op("pool", lambda g, i=i: g.memset(vaug[i][:], 1.0), outs=[bva[i]])
    mqk_v = mqk_d.rearrange("(x p) t -> p x t", p=128)
    mv_v = mv_d.rearrange("(c l) d -> l c d", l=128)
    for blk in range(NBLK):
        s = blk % 2
        t0 = blk * TB
        if blk == 0:
            k.op("pool", lambda g: g.memset(xin[s][:, :, 0:4], 0.0), outs=[bxin[s]])
            k.dma(xin[s][:, :, 4:4 + TB], mqk_v[:, :, 0:TB], outs=[bxin[s]])
        else:
            k.dma(xin[s][:, :, 0:4 + TB], mqk_v[:, :, t0 - 4:t0 + TB], outs=[bxin[s]])
        k.dma(vaug[s][:, :, 0:128], mv_v[:, blk * CPB:(blk + 1) * CPB, :], outs=[bva[s]])
        k.dma(mo[s][:], mo_d[:, t0:t0 + TB], outs=[bmo[s]])
        k.op("act", lambda g: g.activation(out=sigo[s][:], in_=mo[s][:], func=AF.Sigmoid), ins=[bmo[s]], outs=[bsigo[s]])
        for X in range(4):
            k.op("dve", lambda g: g.tensor_scalar(out=acc[:], in0=xin[s][:, X, 4:4 + TB], scalar1=cw[:, X, 3:4], scalar2=None, op0=ALU.mult),
                 ins=[bxin[s], bcw], outs=[bacc])
            for j in (2, 1, 0):
                k.op("dve", lambda g, j=j: g.scalar_tensor_tensor(out=acc[:], in0=xin[s][:, X, 1 + j:1 + j + TB], scalar=cw[:, X, j:j + 1],
                                                                  in1=acc[:], op0=ALU.mult, op1=ALU.add), ins=[bxin[s], bcw, bacc], outs=[bacc])
            k.op("act", lambda g: g.activation(out=qk[s][:, X, :], in_=acc[:], func=AF.Silu), ins=[bacc], outs=[bqk[s]])
        for ci in range(CPB):
            cg = blk * CPB + ci
            csl = slice(ci * 128, (ci + 1) * 128)
            d = cg % 2
            k.op("dve", lambda g: g.tensor_scalar(out=diagb[d][:], in0=identf[:], scalar1=negb[:, cg:cg + 1], scalar2=None, op0=ALU.mult),
                 ins=[bc, bg], outs=[bdiag[d]])
            k.op("pe", lambda g: g.matmul(Eps[:, 0:128], lhsT=onesf[:], rhs=diagb[d][:], start=True, stop=False), ins=[bc, bdiag[d]], outs=[bEps])
            k.op("pe", lambda g: g.matmul(Eps[:, 0:128], lhsT=identf[:], rhs=negmask[:], start=False, stop=True), ins=[bc], outs=[bEps])
            k.op("pe", lambda g: g.matmul(Eps[:, 128:256], lhsT=onesf[:], rhs=diagb[d][:], start=True, stop=True), ins=[bc, bdiag[d]], outs=[bEps])
            k.op("act", lambda g: g.activation(out=Dt[:], in_=Eps[:, 0:128], func=AF.Exp, bias=imb[:, cg:cg + 1]), ins=[bEps, bg], outs=[bDt])
            k.op("act", lambda g: g.activation(out=ebb[:], in_=Eps[:, 128:256], func=AF.Exp), ins=[bEps], outs=[bebb])
            for dk in range(2):
                k.op("pe", lambda g, dk=dk: g.matmul(Sps[:, 0:128], lhsT=qk[s][:, 2 + dk, csl], rhs=qk[s][:, dk, csl], start=(dk == 0), stop=(dk == 1)),
                     ins=[bqk[s]], outs=[bSps])
            k.op("dve", lambda g: g.tensor_tensor(out=Pt[:], in0=Sps[:, 0:128], in1=Dt[:], op=ALU.mult), ins=[bSps, bDt], outs=[bPt])
            for dk in range(2):
                k.op("dve", lambda g, dk=dk: g.tensor_tensor(out=qtl[:, dk, :], in0=qk[s][:, dk, csl], in1=ebb[:], op=ALU.mult),
                     ins=[bqk[s], bebb], outs=[bqtl])
            k.op("pe", lambda g: g.matmul(NDps[:, 0:128], lhsT=vaug[s][:, ci, 0:128], rhs=Pt[:], start=True, stop=False), ins=[bva[s], bPt], outs=[bND])
            for dk in range(2):
                k.op("pe", lambda g, dk=dk: g.matmul(NDps[:, 0:128], lhsT=C0bf[:, dk, :], rhs=qtl[:, dk, :], start=False, stop=(dk == 1)),
                     ins=[bC0, bqtl], outs=[bND])
            k.op("pe", lambda g: g.matmul(NDps[:, 128:256], lhsT=onesbf[:], rhs=Pt[:], start=True, stop=False), ins=[bc, bPt], outs=[bND])
            for dk in range(2):
                k.op("pe", lambda g, dk=dk: g.matmul(NDps[:, 128:256], lhsT=n0bc[:, dk, :], rhs=qtl[:, dk, :], start=False, stop=(dk == 1)),
                     ins=[bC0, bqtl], outs=[bND])
            k.op("act", lambda g: g.activation(out=dn[:], in_=NDps[:, 128:256], func=AF.Abs), ins=[bND], outs=[bdn])
            k.op("dve", lambda g: g.tensor_scalar(out=dn[:], in0=dn[:], scalar1=1.0, scalar2=None, op0=ALU.max), ins=[bdn], outs=[bdn])
            k.op("dve", lambda g: g.reciprocal(out=dn[:], in_=dn[:]), ins=[bdn], outs=[bdn])
            k.op("dve", lambda g: g.tensor_tensor(out=hh[:], in0=NDps[:, 0:128], in1=dn[:], op=ALU.mult), ins=[bND, bdn], outs=[bhh])
            k.op("dve", lambda g: g.tensor_tensor(out=yb[s][:, csl], in0=hh[:], in1=sigo[s][:, csl], op=ALU.mult), ins=[bhh, bsigo[s]], outs=[byb[s]])
            for dk in range(2):
                k.op("pe", lambda g, dk=dk: g.transpose(ktps[:, dk * 128:(dk + 1) * 128], qk[s][:, 2 + dk, csl], identbf[:]),
                     ins=[bqk[s], bc], outs=[bktps])
            k.op("dve", lambda g: g.tensor_scalar(out=kt[:], in0=ktps[:, 0:256], scalar1=wa[:, cg:cg + 1], scalar2=None, op0=ALU.mult),
                 ins=[bktps, bg], outs=[bkt])
            for dk in range(2):
                k.op("pe", lambda g, dk=dk: g.matmul(dCps[:, dk, 0:129], lhsT=kt[:, dk * 128:(dk + 1) * 128], rhs=vaug[s][:, ci, :], start=True, stop=True),
                     ins=[bkt, bva[s]], outs=[bdC])
            for dk in range(2):
                k.op("dve", lambda g, dk=dk: g.scalar_tensor_tensor(out=Cst[:, dk, :], in0=Cst[:, dk, :], scalar=ebtot[:, cg:cg + 1], in1=dCps[:, dk, 0:129],
                                                                    op0=ALU.mult, op1=ALU.add), ins=[bCst, bg, bdC], outs=[bCst])
            k.op("pool", lambda g: g.tensor_copy(out=C0bf[:], in_=Cst[:, :, 0:128]), ins=[bCst], outs=[bC0])
            for dk in range(2):
                k.op("dve", lambda g, dk=dk: g.tensor_scalar(out=n0bc[:, dk, :], in0=onesbf[:], scalar1=Cst[:, dk, 128:129], scalar2=None, op0=ALU.mult),
                     ins=[bc, bCst, bC0], outs=[bC0])
        k.dma(y_dram[:, t0:t0 + TB], yb[s][:], ins=[byb[s]], outs=[by])


def build_ML():
    nc = new_nc()
    mqk_d = nc.dram_tensor("mqk", [512, T], BF16, kind="ExternalInput").ap()
    mv_d = nc.dram_tensor("mv", [T, 128], BF16, kind="ExternalInput").ap()
    mo_d = nc.dram_tensor("mo", [128, T], BF16, kind="ExternalInput").ap()
    gif_d = nc.dram_tensor("gif", [128, 2, 128], F32, kind="ExternalInput").ap()
    cw_d = nc.dram_tensor("cw", [128, 4, 4], F32, kind="ExternalInput").ap()
    gb_d = nc.dram_tensor("gb", [128, 2], F32, kind="ExternalInput").ap()
    yd = nc.dram_tensor("yml", [128, T], BF16, kind="ExternalOutput").ap()
    with contextlib.ExitStack() as st:
        k = K(nc, st)
        by = k.buf()
        emit_ml(k, mqk_d, mv_d, mo_d, gif_d, cw_d, gb_d, yd, by)
        k.finish([by])
    return nc


def ml_host_inputs(c, l, mq, mk, mv, mo, mi, mf, ml_conv, ml_i_bias, ml_f_bias):
    bf = ml_dtypes.bfloat16
    hd, vh = c // 2, c % 2
    q = mq[:, hd * 256:(hd + 1) * 256]; kk = mk[:, hd * 256:(hd + 1) * 256]
    mqk = np.concatenate([q.T, kk.T], axis=0).astype(bf)
    v = mv[:, hd * 256 + vh * 128: hd * 256 + (vh + 1) * 128].astype(bf)
    o = mo[:, hd * 256 + vh * 128: hd * 256 + (vh + 1) * 128].T.astype(bf)
    gi = mi[:, hd].reshape(128, 128).T; gf = mf[:, hd].reshape(128, 128).T
    gif = np.stack([gi, gf], axis=1).astype(np.float32)
    cwq = ml_conv[:, hd * 256:(hd + 1) * 256]; cwk = ml_conv[:, 1024 + hd * 256:1024 + (hd + 1) * 256]
    cw = np.stack([cwq[:, 0:128].T, cwq[:, 128:256].T, cwk[:, 0:128].T, cwk[:, 128:256].T], axis=1).astype(np.float32)
    gb = np.tile(np.array([[ml_i_bias[hd], ml_f_bias[hd]]], np.float32), (128, 1))
    return {"mqk": np.ascontiguousarray(mqk), "mv": np.ascontiguousarray(v), "mo": np.ascontiguousarray(o),
            "gif": np.ascontiguousarray(gif), "cw": np.ascontiguousarray(cw), "gb": gb}


def emit_s5(k, u16_d, lamst_d, bst_d, cst_d, dsk_d, y_dram, by, tag="s5"):
    L = 16
    NBLK = 1024
    NB = T // NBLK
    CPB = NBLK // L
    NC = T // L
    NLEV = 10
    PI = float(np.pi)
    identf = k.sb(tag + "identf", [128, 128], F32); onesf = k.sb(tag + "onesf", [128, 128], F32)
    swap = k.sb(tag + "swap", [128, 128], F32); sw2 = k.sb(tag + "sw2", [128, 128], F32)
    sgn = k.sb(tag + "sgn", [128, 1], F32)
    bc = k.buf()
    k.op("pool", lambda g: g.memset(onesf[:], 1.0), outs=[bc])
    k.op("pool", lambda g: g.affine_select(out=identf[:], in_=onesf[:], pattern=[[-1, 128]], compare_op=ALU.is_equal,
                                           fill=0.0, base=0, channel_multiplier=1), ins=[bc], outs=[bc])
    k.op("pool", lambda g: g.affine_select(out=swap[:], in_=onesf[:], pattern=[[-1, 128]], compare_op=ALU.is_equal,
                                           fill=0.0, base=64, channel_multiplier=1), ins=[bc], outs=[bc])
    k.op("pool", lambda g: g.affine_select(out=sw2[:], in_=onesf[:], pattern=[[-1, 128]], compare_op=ALU.is_equal,
                                           fill=0.0, base=-64, channel_multiplier=1), ins=[bc], outs=[bc])
    k.op("pool", lambda g: g.tensor_tensor(out=swap[:], in0=swap[:], in1=sw2[:], op=ALU.add), ins=[bc], outs=[bc])
    k.op("pool", lambda g: g.memset(sgn[0:64, :], 1.0), ins=[bc], outs=[bc])
    k.op("pool", lambda g: g.memset(sgn[64:128, :], -1.0), ins=[bc], outs=[bc])
    pat01 = k.sb(tag + "pat01", [128, NBLK // L, L], F32)
    k.op("pool", lambda g: g.memset(pat01[:], 1.0), ins=[bc], outs=[bc])
    k.op("pool", lambda g: g.memset(pat01[:, :, 0:1], 0.0), ins=[bc], outs=[bc])
    lamst = k.sb(tag + "lamst", [128, 3, 8], F32); bst = k.sb(tag + "bst", [128, 8, 16], F32)
    cst = k.sb(tag + "cst", [128, 8, 16], F32); dsk = k.sb(tag + "dsk", [16, 8], F32)
    bpar = k.buf()
    k.dma(lamst[:], lamst_d, outs=[bpar]); k.dma(bst[:], bst_d, outs=[bpar]); k.dma(cst[:], cst_d, outs=[bpar]); k.dma(dsk[:], dsk_d, outs=[bpar])
    k.op("dve", lambda g: g.tensor_scalar(out=cst[64:128], in0=cst[64:128], scalar1=-1.0, scalar2=None, op0=ALU.mult), ins=[bpar], outs=[bpar])
    NTAB = 24
    tab = k.sb(tag + "tab", [128, NTAB, 8], F32); bt = k.buf()
    tmp = k.sb(tag + "tmpt", [128, 8, 8], F32)
    (I_DT, I_ER, I_ANG, I_SIN, I_COS, I_LR, I_LI, I_KR, I_KI, I_T0, I_T1, I_T2, I_T3) = range(13)
    tv = lambda i: tab[:, i, :]
    dv = lambda fn, **kw: k.op("dve", fn, ins=[bt, bpar, bc], outs=[bt])
    av = lambda fn: k.op("act", fn, ins=[bt, bpar], outs=[bt])
    TT = lambda o, a, b, op: dv(lambda g: g.tensor_tensor(out=o, in0=a, in1=b, op=op))
    TS = lambda o, a, s1, op0, s2=None, op1=None: dv(lambda g: g.tensor_scalar(out=o, in0=a, scalar1=s1, scalar2=s2, op0=op0, **({"op1": op1} if op1 is not None else {})))

    def cmul(o_r, o_i, ar, ai, br, bi, t1, t2):
        TT(t1, ar, br, ALU.mult); TT(t2, ai, bi, ALU.mult); TT(o_r, t1, t2, ALU.subtract)
        TT(t1, ar, bi, ALU.mult); TT(t2, ai, br, ALU.mult); TT(o_i, t1, t2, ALU.add)

    av(lambda g: g.activation(out=tv(I_DT), in_=lamst[:, 2, :], func=AF.Exp))
    TT(tv(I_T0), lamst[:, 0, :], tv(I_DT), ALU.mult)
    av(lambda g: g.activation(out=tv(I_ER), in_=tv(I_T0), func=AF.Exp))
    TT(tv(I_ANG), lamst[:, 1, :], tv(I_DT), ALU.mult)

    def rr(dst, src, shift):
        TS(dst, src, shift, ALU.add)
        for _ in range(8):
            TS(tv(I_T1), dst, PI, ALU.is_gt)
            dv(lambda g: g.scalar_tensor_tensor(out=dst, in0=tv(I_T1), scalar=-2.0 * PI, in1=dst, op0=ALU.mult, op1=ALU.add))
        TS(dst, dst, -PI, ALU.max)
        TS(dst, dst, PI, ALU.min)

    rr(tv(I_T2), tv(I_ANG), 0.0)
    av(lambda g: g.activation(out=tv(I_SIN), in_=tv(I_T2), func=AF.Sin))
    rr(tv(I_T2), tv(I_ANG), PI / 2)
    av(lambda g: g.activation(out=tv(I_COS), in_=tv(I_T2), func=AF.Sin))
    TT(tv(I_LR), tv(I_ER), tv(I_COS), ALU.mult)
    TT(tv(I_LI), tv(I_ER), tv(I_SIN), ALU.mult)
    TS(tv(I_T0), tv(I_LR), -1.0, ALU.add)
    TT(tv(I_T1), lamst[:, 0, :], lamst[:, 0, :], ALU.mult)
    TT(tv(I_T2), lamst[:, 1, :], lamst[:, 1, :], ALU.mult)
    TT(tv(I_T1), tv(I_T1), tv(I_T2), ALU.add)
    dv(lambda g: g.reciprocal(out=tv(I_T1), in_=tv(I_T1)))
    TT(tv(I_T2), tv(I_T0), lamst[:, 0, :], ALU.mult)
    TT(tv(I_T3), tv(I_LI), lamst[:, 1, :], ALU.mult)
    TT(tv(I_T2), tv(I_T2), tv(I_T3), ALU.add)
    TT(tv(I_KR), tv(I_T2), tv(I_T1), ALU.mult)
    TT(tv(I_T2), tv(I_LI), lamst[:, 0, :], ALU.mult)
    TT(tv(I_T3), tv(I_T0), lamst[:, 1, :], ALU.mult)
    TT(tv(I_T2), tv(I_T2), tv(I_T3), ALU.subtract)
    TT(tv(I_KI), tv(I_T2), tv(I_T1), ALU.mult)
    ct = k.sb(tag + "ct", [128, L, 8], F32); stt = k.sb(tag + "stt", [128, L, 8], F32)
    pwr = k.sb(tag + "pwr", [128, L, 8], F32); pwi = k.sb(tag + "pwi", [128, L, 8], F32)
    rhr = k.sb(tag + "rhr", [128, L, 8], F32); rhi = k.sb(tag + "rhi", [128, L, 8], F32)
    mur = k.sb(tag + "mur", [128, NLEV, 8], F32); mui = k.sb(tag + "mui", [128, NLEV, 8], F32)
    nst = k.sb(tag + "nst", [128, L, 8], F32)
    dv(lambda g: g.memset(ct[:, 0, :], 1.0)); dv(lambda g: g.memset(stt[:, 0, :], 0.0))
    dv(lambda g: g.tensor_copy(out=pwr[:, 0, :], in_=tv(I_LR))); dv(lambda g: g.tensor_copy(out=pwi[:, 0, :], in_=tv(I_LI)))
    for t in range(1, L):
        cmul(ct[:, t, :], stt[:, t, :], ct[:, t - 1, :], stt[:, t - 1, :], tv(I_COS), tv(I_SIN), tmp[:, 0, :], tmp[:, 1, :])
        cmul(pwr[:, t, :], pwi[:, t, :], pwr[:, t - 1, :], pwi[:, t - 1, :], tv(I_LR), tv(I_LI), tmp[:, 0, :], tmp[:, 1, :])
    dv(lambda g: g.tensor_scalar(out=nst[:], in0=stt[:], scalar1=-1.0, scalar2=None, op0=ALU.mult))
    for t in range(L):
        cmul(rhr[:, t, :], rhi[:, t, :], ct[:, t, :], nst[:, t, :], tv(I_KR), tv(I_KI), tmp[:, 0, :], tmp[:, 1, :])
    dv(lambda g: g.tensor_copy(out=mur[:, 0, :], in_=pwr[:, L - 1, :])); dv(lambda g: g.tensor_copy(out=mui[:, 0, :], in_=pwi[:, L - 1, :]))
    for lv in range(1, NLEV):
        cmul(mur[:, lv, :], mui[:, lv, :], mur[:, lv - 1, :], mui[:, lv - 1, :], mur[:, lv - 1, :], mui[:, lv - 1, :], tmp[:, 0, :], tmp[:, 1, :])
    rhis = k.sb(tag + "rhis", [128, L, 8], F32); nsts = k.sb(tag + "nsts", [128, L, 8], F32)
    npwis = k.sb(tag + "npwis", [128, L, 8], F32); muis = k.sb(tag + "muis", [128, NLEV, 8], F32)
    sts = k.sb(tag + "sts", [128, 8], F32)
    TS(rhis[:], rhi[:], sgn[:, 0:1], ALU.mult)
    TS(nsts[:], nst[:], sgn[:, 0:1], ALU.mult)
    TS(npwis[:], pwi[:], sgn[:, 0:1], ALU.mult, -1.0, ALU.mult)
    TS(muis[:], mui[:], sgn[:, 0:1], ALU.mult)
    TS(sts[:], stt[:, L - 1, :], sgn[:, 0:1], ALU.mult)
    Bin = k.sb(tag + "Bin", [16, 8, L, 128], BF16); Cloc = k.sb(tag + "Cloc", [128, 8, L, 16], BF16)
    Ccor = k.sb(tag + "Ccor", [128, 8, L, 16], BF16)
    ErT = k.sb(tag + "ErT", [128, 8, 128], F32); MkT = k.sb(tag + "MkT", [128, 2, NLEV, 128], F32)
    bw = k.buf()
    bm = [k.sb("%sbm%d" % (tag, i), [128, 128], F32) for i in range(4)]; bbm = k.bufs(4)
    pps = [k.ps("%spps%d" % (tag, i), [128, 512]) for i in range(2)]; bpps = k.bufs(2)
    cnt = [0]

    def blockmat(a_ap, bs_ap):
        i = cnt[0] % 4
        cnt[0] += 1
        k.op("dve", lambda g: g.tensor_scalar(out=bm[i][:], in0=identf[:], scalar1=a_ap, scalar2=None, op0=ALU.mult), ins=[bc, bt], outs=[bbm[i]])
        k.op("dve", lambda g: g.scalar_tensor_tensor(out=bm[i][:], in0=swap[:], scalar=bs_ap, in1=bm[i][:], op0=ALU.mult, op1=ALU.add),
             ins=[bc, bt, bbm[i]], outs=[bbm[i]])
        return bm[i], bbm[i]

    ev = [0]

    def evac(out_ap, in_ap, bin_, bout):
        ev[0] += 1
        _evac(k, ev[0], out_ap, in_ap, [bin_], [bout])

    for g_ in range(8):
        gs = slice(g_, g_ + 1)
        for t in range(L):
            m, bm_ = blockmat(rhr[:, t, gs], rhis[:, t, gs])
            p = cnt[0] % 2
            k.op("pe", lambda g, m=m, p=p: g.matmul(pps[p][0:16, 0:128], lhsT=bst[:, g_, :], rhs=m[:], start=True, stop=True), ins=[bpar, bm_], outs=[bpps[p]])
            evac(Bin[:, g_, t, :], pps[p][0:16, 0:128], bpps[p], bw)
            m, bm_ = blockmat(ct[:, t, gs], nsts[:, t, gs])
            p = cnt[0] % 2
            k.op("pe", lambda g, m=m, p=p: g.matmul(pps[p][:, 0:16], lhsT=m[:], rhs=cst[:, g_, :], start=True, stop=True), ins=[bpar, bm_], outs=[bpps[p]])
            evac(Cloc[:, g_, t, :], pps[p][:, 0:16], bpps[p], bw)
            m, bm_ = blockmat(pwr[:, t, gs], npwis[:, t, gs])
            p = cnt[0] % 2
            k.op("pe", lambda g, m=m, p=p: g.matmul(pps[p][:, 0:16], lhsT=m[:], rhs=cst[:, g_, :], start=True, stop=True), ins=[bpar, bm_], outs=[bpps[p]])
            evac(Ccor[:, g_, t, :], pps[p][:, 0:16], bpps[p], bw)
    def blockmat_into(out_ap, a_ap, bs_ap):
        k.op("dve", lambda g: g.tensor_scalar(out=out_ap, in0=identf[:], scalar1=a_ap, scalar2=None, op0=ALU.mult), ins=[bc, bt, bw], outs=[bw])
        k.op("dve", lambda g: g.scalar_tensor_tensor(out=out_ap, in0=swap[:], scalar=bs_ap, in1=out_ap, op0=ALU.mult, op1=ALU.add),
             ins=[bc, bt, bw], outs=[bw])
    for g_ in range(8):
        gs = slice(g_, g_ + 1)
        blockmat_into(ErT[:, g_, :], ct[:, L - 1, gs], sts[:, gs])
    ug = [k.sb("%sug%d" % (tag, i), [16, T], BF16) for i in range(1)] * 2; bug = [k.buf()] * 2
    zbf = k.sb(tag + "zbf", [128, T], BF16); bzbf = k.buf()
    Ag = k.sb(tag + "Ag", [128, NBLK], F32); bAg = k.buf()
    zin = [k.sb("%szin%d" % (tag, i), [128, NBLK], F32) for i in range(2)]; bzin = k.bufs(2)
    zf = [k.sb("%szf%d" % (tag, i), [128, NBLK], F32) for i in range(2)]; bzf = k.bufs(2)
    zps = [k.ps("%szps%d" % (tag, i), [128, L, CPB]) for i in range(2)]; bzps = k.bufs(2)
    yps = k.ps(tag + "yps", [128, L, CPB]); byps = k.buf()
    Ssc = [k.sb("%sSsc%d" % (tag, i), [128, NC], F32) for i in range(2)]; bS = k.bufs(2)
    Sbf = k.sb(tag + "Sbf", [128, NC + 1], BF16); bSbf = k.buf()
    y1 = [k.sb("%sy1_%d" % (tag, i), [16, NBLK], F32) for i in range(2)]; by1 = k.bufs(2)
    y2 = [k.sb("%sy2_%d" % (tag, i), [16, NBLK], F32) for i in range(2)]; by2 = k.bufs(2)
    yo = [k.sb("%syo_%d" % (tag, i), [16, NBLK], BF16) for i in range(2)]; byo = k.bufs(2)
    k.op("pool", lambda g: g.memset(Sbf[:, 0:1], 0.0), outs=[bSbf])
    for g_ in range(8):
        gs = slice(g_, g_ + 1)
        u_ = ug[g_ % 2]; bu_ = bug[g_ % 2]
        for i in range(4):
            k.dma(u_[:, i * 4096:(i + 1) * 4096], u16_d[:, g_, i * 4096:(i + 1) * 4096], outs=[bu_])
        k.op("dve", lambda g: g.tensor_scalar(out=Ag[:], in0=pat01[:].rearrange("p c l -> p (c l)"), scalar1=tab[:, I_ER, gs], scalar2=None, op0=ALU.mult),
             ins=[bc, bt], outs=[bAg])
        for lv in range(NLEV):
            blockmat_into(MkT[:, g_ % 2, lv, :], mur[:, lv, gs], muis[:, lv, gs])
        uv = u_[:].rearrange("p (c l) -> p c l", l=L)
        zbv = zbf[:].rearrange("p (c l) -> p c l", l=L)
        for blk in range(NB):
            s = blk % 2
            c0 = blk * CPB
            for t in range(L):
                k.op("pe", lambda g, t=t: g.matmul(zps[s][:, t, :], lhsT=Bin[:, g_, t, :], rhs=uv[:, c0:c0 + CPB, t], start=True, stop=True),
                     ins=[bw, bu_], outs=[bzps[s]])
            evac(zin[s][:].rearrange("p (c l) -> p l c", l=L), zps[s][:], bzps[s], bzin[s])
            k.op("dve", lambda g: g.tensor_tensor_scan(out=zf[s][:], data0=Ag[:], data1=zin[s][:], initial=0.0, op0=ALU.mult, op1=ALU.add),
                 ins=[bAg, bzin[s]], outs=[bzf[s]])
            k.op("pool", lambda g: g.tensor_copy(out=zbf[:, blk * NBLK:(blk + 1) * NBLK], in_=zf[s][:]), ins=[bzf[s]], outs=[bzbf])
            p = blk % 2
            k.op("pe", lambda g, p=p: g.matmul(pps[p][:, 0:CPB], lhsT=ErT[:, g_, :], rhs=zf[s][:].rearrange("p (c l) -> p c l", l=L)[:, :, L - 1],
                                               start=True, stop=True), ins=[bw, bzf[s]], outs=[bpps[p]])
            k.op("dve", lambda g, p=p: g.tensor_copy(out=Ssc[0][:, c0:c0 + CPB], in_=pps[p][:, 0:CPB]), ins=[bpps[p]], outs=[bS[0]])
        for lv in range(NLEV):
            d = 1 << lv
            src, dst = Ssc[lv % 2], Ssc[(lv + 1) % 2]
            bsrc, bdst = bS[lv % 2], bS[(lv + 1) % 2]
            n = NC - d
            k.op("pool", lambda g: g.tensor_copy(out=dst[:, 0:d], in_=src[:, 0:d]), ins=[bsrc], outs=[bdst])
            for j0 in range(0, n, 512):
                nn = min(512, n - j0)
                p = (j0 // 512) % 2
                k.op("pe", lambda g, p=p: g.matmul(pps[p][:, 0:nn], lhsT=MkT[:, g_ % 2, lv, :], rhs=src[:, j0:j0 + nn], start=True, stop=True),
                     ins=[bw, bsrc], outs=[bpps[p]])
                k.op("dve", lambda g, p=p: g.tensor_tensor(out=dst[:, d + j0:d + j0 + nn], in0=src[:, d + j0:d + j0 + nn], in1=pps[p][:, 0:nn], op=ALU.add),
                     ins=[bsrc, bpps[p]], outs=[bdst])
        fin = Ssc[NLEV % 2]; bfin = bS[NLEV % 2]
        k.op("act", lambda g: g.activation(out=Sbf[:, 1:NC + 1], in_=fin[:], func=AF.Identity), ins=[bfin], outs=[bSbf])
        for blk in range(NB):
            s = blk % 2
            c0 = blk * CPB
            t0 = blk * NBLK
            for t in range(L):
                k.op("pe", lambda g, t=t: g.matmul(yps[0:16, t, :], lhsT=Cloc[:, g_, t, :], rhs=zbv[:, c0:c0 + CPB, t], start=True, stop=False),
                     ins=[bw, bzbf], outs=[byps])
                k.op("pe", lambda g, t=t: g.matmul(yps[0:16, t, :], lhsT=Ccor[:, g_, t, :], rhs=Sbf[:, c0:c0 + CPB], start=False, stop=True),
                     ins=[bw, bSbf], outs=[byps])
            evac(y1[s][:].rearrange("p (c l) -> p l c", l=L), yps[0:16], byps, by1[s])
            k.op("dve", lambda g: g.scalar_tensor_tensor(out=y1[s][:], in0=u_[:, t0:t0 + NBLK], scalar=dsk[:, gs], in1=y1[s][:], op0=ALU.mult, op1=ALU.add),
                 ins=[bu_, bpar, by1[s]], outs=[by1[s]])
            k.op("act", lambda g: g.activation(out=y2[s][:], in_=y1[s][:], func=AF.Square), ins=[by1[s]], outs=[by2[s]])
            k.op("dve", lambda g: g.tensor_scalar(out=y2[s][:], in0=y2[s][:], scalar1=0.044715, scalar2=1.0, op0=ALU.mult, op1=ALU.add), ins=[by2[s]], outs=[by2[s]])
            k.op("dve", lambda g: g.tensor_tensor(out=y2[s][:], in0=y2[s][:], in1=y1[s][:], op=ALU.mult), ins=[by2[s], by1[s]], outs=[by2[s]])
            k.op("act", lambda g: g.activation(out=y2[s][:], in_=y2[s][:], func=AF.Sigmoid, scale=1.5957691216057308), ins=[by2[s]], outs=[by2[s]])
            k.op("dve", lambda g: g.tensor_tensor(out=yo[s][:], in0=y2[s][:], in1=y1[s][:], op=ALU.mult), ins=[by2[s], by1[s]], outs=[byo[s]])
            k.dma(y_dram[:, g_, t0:t0 + NBLK], yo[s][:], ins=[byo[s]], outs=[by])


def build_S5():
    nc = new_nc()
    u16_d = nc.dram_tensor("u16", [16, 8, T], BF16, kind="ExternalInput").ap()
    lamst_d = nc.dram_tensor("lamst", [128, 3, 8], F32, kind="ExternalInput").ap()
    bst_d = nc.dram_tensor("bst", [128, 8, 16], F32, kind="ExternalInput").ap()
    cst_d = nc.dram_tensor("cst", [128, 8, 16], F32, kind="ExternalInput").ap()
    dsk_d = nc.dram_tensor("dsk", [16, 8], F32, kind="ExternalInput").ap()
    yd = nc.dram_tensor("ys5", [16, 8, T], BF16, kind="ExternalOutput").ap()
    with contextlib.ExitStack() as st:
        k = K(nc, st)
        by = k.buf()
        emit_s5(k, u16_d, lamst_d, bst_d, cst_d, dsk_d, yd, by)
        k.finish([by])
    return nc


def s5_host_inputs(c, u, lam_re, lam_im, log_dt, b_re, b_im, c_re, c_im, d_skip):
    bf = ml_dtypes.bfloat16
    G = slice(8 * c, 8 * c + 8)
    u16 = u[:, 128 * c:128 * (c + 1)].reshape(T, 8, 16).transpose(2, 1, 0).astype(bf)
    st2 = lambda a: np.concatenate([a, a], axis=0)
    lamst = np.stack([st2(lam_re[G].T), st2(lam_im[G].T), np.tile(log_dt[G][None, :], (128, 1))], axis=1).astype(np.float32)
    bst = np.concatenate([b_re[G].transpose(1, 0, 2), b_im[G].transpose(1, 0, 2)], axis=0).astype(np.float32)
    cst = np.concatenate([c_re[G].transpose(2, 0, 1), c_im[G].transpose(2, 0, 1)], axis=0).astype(np.float32)
    dsk = d_skip[128 * c:128 * (c + 1)].reshape(8, 16).T.astype(np.float32)
    return {"u16": np.ascontiguousarray(u16), "lamst": np.ascontiguousarray(lamst), "bst": np.ascontiguousarray(bst),
            "cst": np.ascontiguousarray(cst), "dsk": np.ascontiguousarray(dsk)}


def build_ADA():
    NCOL = 2 * 6 * D // NCORES
    nc = new_nc()
    cT = nc.dram_tensor("cT", [128, 16], F32, kind="ExternalInput").ap()
    w = nc.dram_tensor("w", [D, NCOL], F32, kind="ExternalInput").ap()
    b = nc.dram_tensor("b", [1, NCOL], F32, kind="ExternalInput").ap()
    o = nc.dram_tensor("mod", [1, NCOL], F32, kind="ExternalOutput").ap()
    w_v = w.rearrange("(kc p) n -> p kc n", p=128)
    with contextlib.ExitStack() as st:
        k = K(nc, st)
        cs = k.sb("cs", [128, 16], F32); bcs = k.buf()
        bs = k.sb("bs", [1, NCOL], F32); bbs = k.buf()
        os_ = k.sb("os", [1, NCOL], F32); bos = k.buf()
        wt = [k.sb("wt%d" % i, [128, 16, 512], F32) for i in range(2)]; bwt = k.bufs(2)
        ps = [k.ps("ps%d" % i, [128, 512]) for i in range(2)]; bps = k.bufs(2)
        bo = k.buf()
        k.dma(cs[:], cT, outs=[bcs]); k.dma(bs[:], b, outs=[bbs])
        k.op("act", lambda g: g.activation(out=cs[:], in_=cs[:], func=AF.Silu), ins=[bcs], outs=[bcs])
        for j in range(NCOL // 512):
            s = j % 2
            k.dma(wt[s][:], w_v[:, :, j * 512:(j + 1) * 512], outs=[bwt[s]])
            for kc in range(16):
                k.op("pe", lambda g, kc=kc: g.matmul(ps[s][0:1, :], lhsT=cs[:, kc:kc + 1], rhs=wt[s][:, kc, :], start=(kc == 0), stop=(kc == 15)),
                     ins=[bcs, bwt[s]], outs=[bps[s]])
            k.op("dve", lambda g: g.tensor_tensor(out=os_[:, j * 512:(j + 1) * 512], in0=ps[s][0:1, :], in1=bs[:, j * 512:(j + 1) * 512], op=ALU.add),
                 ins=[bps[s], bbs], outs=[bos])
        k.dma(o, os_[:], ins=[bos], outs=[bo])
        k.finish([bo])
    return nc


def build_C(last):
    NTOK = 2048
    NT = 512
    nc = new_nc()
    xT = nc.dram_tensor("xT", [D, NTOK], F32, kind="ExternalInput").ap()
    pgT = nc.dram_tensor("pgT", [3 * D, NTOK], F32, kind="ExternalInput").ap()
    yT = nc.dram_tensor("yT", [3072, NTOK], BF16, kind="ExternalInput").ap()
    vecs = nc.dram_tensor("vecs", [128, 8, 16], F32, kind="ExternalInput").ap()
    wglu = nc.dram_tensor("wglu", [1024, 1024], F32, kind="ExternalInput").ap()
    pw = [nc.dram_tensor(n, [1024, D], F32, kind="ExternalInput").ap() for n in ("psb", "ps5", "pml")]
    wout = nc.dram_tensor("wout", [D, D], F32, kind="ExternalInput").ap()
    wgr = nc.dram_tensor("wgr", [D, 36], F32, kind="ExternalInput").ap()
    bgr = nc.dram_tensor("bgr", [128, 36], F32, kind="ExternalInput").ap()
    w1 = nc.dram_tensor("w1", [32, D, 256], F32, kind="ExternalInput").ap()
    w3 = nc.dram_tensor("w3", [32, D, 256], F32, kind="ExternalInput").ap()
    w2 = nc.dram_tensor("w2", [32, 256, D], F32, kind="ExternalInput").ap()
    xnT = nc.dram_tensor("xnT", [D, NTOK], F32, kind="ExternalOutput").ap()
    if last:
        outT = nc.dram_tensor("outT", [D, NTOK], F32, kind="ExternalOutput").ap()
    xT_v = xT.rearrange("(kc p) t -> p kc t", p=128)
    xnT_v = xnT.rearrange("(kc p) t -> p kc t", p=128)
    yT_v = yT.rearrange("(kc p) t -> p kc t", p=128)
    pg_v = pgT.rearrange("(br n p) t -> p br n t", br=3, p=128)
    wglu_v = wglu.rearrange("(kc p) n -> p kc n", p=128)
    pw_v = [a.rearrange("(kc p) n -> p kc n", p=128) for a in pw]
    wout_v = wout.rearrange("(kc p) n -> p kc n", p=128)
    wgr_v = wgr.rearrange("(kc p) n -> p kc n", p=128)
    w1_v = w1.rearrange("e (kc p) f -> p e kc f", p=128)
    w3_v = w3.rearrange("e (kc p) f -> p e kc f", p=128)
    w2_v = w2.rearrange("e (f p) n -> p e f n", p=128)
    with contextlib.ExitStack() as st:
        k = K(nc, st)
        identf = k.sb("identf", [128, 128], F32); onesf = k.sb("onesf", [128, 128], F32); ones_bf = k.sb("ones_bf", [128, 128], BF16)
        bc = k.buf()
        k.op("pool", lambda g: g.memset(onesf[:], 1.0), outs=[bc])
        k.op("pool", lambda g: g.memset(ones_bf[:], 1.0), ins=[bc], outs=[bc])
        k.op("pool", lambda g: g.affine_select(out=identf[:], in_=onesf[:], pattern=[[-1, 128]], compare_op=ALU.is_equal,
                                               fill=0.0, base=0, channel_multiplier=1), ins=[bc], outs=[bc])
        vin = k.sb("vin", [128, 8, 16], F32); bvin = k.buf()
        vec = k.sb("vec", [128, 2, 16], F32); bvec = k.buf()
        vecf = k.sb("vecf", [128, 2, 16], F32); bvecf = k.buf()
        wgrs = k.sb("wgrs", [128, 16, 36], F32); bgrs = k.sb("bgrs", [128, 36], F32); bwgr = k.buf()
        k.dma(vin[:], vecs, outs=[bvin]); k.dma(wgrs[:], wgr_v, outs=[bwgr]); k.dma(bgrs[:], bgr, outs=[bwgr])
        k.op("dve", lambda g: g.scalar_tensor_tensor(out=vec[:, 0, :], in0=vin[:, 2, :], scalar=1.0, in1=vin[:, 1, :], op0=ALU.add, op1=ALU.mult), ins=[bvin], outs=[bvec])
        k.op("dve", lambda g: g.tensor_copy(out=vec[:, 1, :], in_=vin[:, 3, :]), ins=[bvin, bvec], outs=[bvec])
        k.op("dve", lambda g: g.tensor_copy(out=vecf[:, 0, :], in_=vin[:, 5, :]), ins=[bvin], outs=[bvecf])
        k.op("dve", lambda g: g.memset(vecf[:, 1, :], 0.0), ins=[bvecf], outs=[bvecf])
        xt = k.sb("xt", [128, 16, NT], F32); bx = k.buf()
        yt = k.sb("yt", [128, 24, NT], BF16); byt = k.buf()
        ysg = k.sb("ysg", [128, 8, NT], BF16); bysg = k.buf()
        mh = k.sb("mh", [128, 16, NT], BF16); bmh = k.buf()
        hid = k.sb("hid", [128, 32, NT], BF16); bhid = k.buf()
        sq = hid[:, 0:16, :]
        wst = [k.sb("wst%d" % i, [128, 4096], F32) for i in range(2)]; bwst = k.bufs(2)
        wbf = [k.sb("wbf%d" % i, [128, 4096], BF16) for i in range(2)]; bwbf = k.bufs(2)
        pgt = [k.sb("pgt%d" % i, [128, 3, NT], F32) for i in range(1)] * 2; bpgt = [k.buf()] * 2
        sg = [k.sb("sg%d" % i, [128, NT], F32) for i in range(3)]; bsg = k.bufs(3)
        t1 = k.sb("t1", [128, NT], F32); t2 = k.sb("t2", [128, NT], F32); bt1 = k.buf(); bt2 = k.buf()
        rstd = k.sb("rstd", [128, NT], F32); brstd = k.buf()
        tmp = k.sb("tmp", [128, 2, NT], F32); btmp = k.bufs(2)
        h2f = [k.sb("h2f%d" % i, [128, NT], F32) for i in range(2)]; bh2f = k.bufs(2)
        gT = k.sb("gT", [32, NT], F32); bgT = k.buf()
        gsel = [k.sb("gsel%d" % i, [32, NT], F32) for i in range(2)]; bgsel = k.bufs(2)
        gbs = [k.sb("gbs%d" % i, [128, NT], F32) for i in range(2)]; bgbs = k.bufs(2)
        rt = k.sb("rt", [128, 16, 36], F32); brt = k.buf()
        P = [k.ps("P%d" % i, [128, 512]) for i in range(8)]; bP = k.bufs(8)
        ACC = [0, 1, 2, 7]
        bout = k.buf()
        wcnt = [0]

        def wtile(view, shape):
            i = wcnt[0] % 2
            wcnt[0] += 1
            a, b = shape
            sv = wst[i][:, 0:a * b].rearrange("p (a b) -> p a b", b=b)
            bv = wbf[i][:, 0:a * b].rearrange("p (a b) -> p a b", b=b)
            k.dma(sv, view, outs=[bwst[i]])
            k.op("pool", lambda g: g.tensor_copy(out=bv, in_=sv), ins=[bwst[i]], outs=[bwbf[i]])
            return bv, bwbf[i]

        acnt = [0]

        def accbank():
            i = ACC[acnt[0] % 4]
            acnt[0] += 1
            return P[i], bP[i]

        for tt in range(NTOK // NT):
            tsl = slice(tt * NT, (tt + 1) * NT)
            k.dma(xt[:], xT_v[:, :, tsl], outs=[bx])
            k.dma(yt[:], yT_v[:, :, tsl], outs=[byt])
            for n in range(8):
                wv, bwv = wtile(wglu_v[:, :, n * 128:(n + 1) * 128], (8, 128))
                ps, bps = accbank()
                for kc in range(8):
                    k.op("pe", lambda g, kc=kc: g.matmul(ps[:], lhsT=wv[:, kc, :], rhs=yt[:, 8 + kc, :], start=(kc == 0), stop=(kc == 7)), ins=[bwv, byt], outs=[bps])
                k.op("act", lambda g: g.activation(out=sg[0][:], in_=ps[:], func=AF.Sigmoid), ins=[bps], outs=[bsg[0]])
                k.op("dve", lambda g: g.tensor_tensor(out=ysg[:, n, :], in0=yt[:, 8 + n, :], in1=sg[0][:], op=ALU.mult), ins=[byt, bsg[0]], outs=[bysg])
            for n in range(16):
                pgs = pgt[n % 2]; bpgs = bpgt[n % 2]
                k.dma(pgs[:], pg_v[:, :, n, tsl], outs=[bpgs])
                banks = []
                for br in range(3):
                    wv, bwv = wtile(pw_v[br][:, :, n * 128:(n + 1) * 128], (8, 128))
                    ps, bps = accbank()
                    for kc in range(8):
                        rhs = ysg[:, kc, :] if br == 1 else yt[:, br * 8 + kc, :]
                        k.op("pe", lambda g, kc=kc, rhs=rhs: g.matmul(ps[:], lhsT=wv[:, kc, :], rhs=rhs, start=(kc == 0), stop=(kc == 7)),
                             ins=[bwv, byt, bysg], outs=[bps])
                    k.op("act", lambda g, br=br: g.activation(out=sg[br][:], in_=pgs[:, br, :], func=AF.Sigmoid), ins=[bpgs], outs=[bsg[br]])
                    banks.append((ps, bps))
                k.op("dve", lambda g: g.tensor_tensor(out=t1[:], in0=banks[0][0][:], in1=sg[0][:], op=ALU.mult), ins=[banks[0][1], bsg[0]], outs=[bt1])
                k.op("dve", lambda g: g.tensor_tensor(out=t2[:], in0=banks[1][0][:], in1=sg[1][:], op=ALU.mult), ins=[banks[1][1], bsg[1]], outs=[bt2])
                k.op("pool", lambda g: g.tensor_tensor(out=t1[:], in0=t1[:], in1=t2[:], op=ALU.add), ins=[bt1, bt2], outs=[bt1])
                k.op("dve", lambda g: g.tensor_tensor(out=t2[:], in0=banks[2][0][:], in1=sg[2][:], op=ALU.mult), ins=[banks[2][1], bsg[2], bt1], outs=[bt2])
                k.op("pool", lambda g: g.tensor_tensor(out=mh[:, n, :], in0=t1[:], in1=t2[:], op=ALU.add), ins=[bt1, bt2], outs=[bmh])
            for n in range(16):
                wv, bwv = wtile(wout_v[:, :, n * 128:(n + 1) * 128], (16, 128))
                ps, bps = accbank()
                for kc in range(16):
                    k.op("pe", lambda g, kc=kc: g.matmul(ps[:], lhsT=wv[:, kc, :], rhs=mh[:, kc, :], start=(kc == 0), stop=(kc == 15)), ins=[bwv, bmh], outs=[bps])
                k.op("dve", lambda g: g.scalar_tensor_tensor(out=xt[:, n, :], in0=ps[:], scalar=vin[:, 0, n:n + 1], in1=xt[:, n, :], op0=ALU.mult, op1=ALU.add),
                     ins=[bps, bvin, bx], outs=[bx])
            k.op("act", lambda g: g.activation(out=sq, in_=xt[:], func=AF.Square), ins=[bx], outs=[bhid])
            for kc in range(16):
                k.op("pe", lambda g, kc=kc: g.matmul(P[6][:], lhsT=ones_bf[:], rhs=sq[:, kc, :], start=(kc == 0), stop=(kc == 15)), ins=[bc, bhid], outs=[bP[6]])
            k.op("act", lambda g: g.activation(out=rstd[:], in_=P[6][:], func=AF.Sqrt, scale=1.0 / D, bias=EPS), ins=[bP[6]], outs=[brstd])
            k.op("dve", lambda g: g.reciprocal(out=rstd[:], in_=rstd[:]), ins=[brstd], outs=[brstd])
            for kc in range(16):
                i = kc % 2
                k.op("dve", lambda g, kc=kc: g.tensor_tensor(out=tmp[:, i, :], in0=xt[:, kc, :], in1=rstd[:], op=ALU.mult), ins=[bx, brstd], outs=[btmp[i]])
                k.op("act", lambda g, kc=kc: g.activation(out=h2f[i][:], in_=tmp[:, i, :], func=AF.Identity, scale=vec[:, 0, kc:kc + 1], bias=vec[:, 1, kc:kc + 1]),
                     ins=[btmp[i], bvec], outs=[bh2f[i]])
                k.op("pool", lambda g, kc=kc: g.tensor_copy(out=mh[:, kc, :], in_=h2f[i][:]), ins=[bh2f[i]], outs=[bmh])
                for j in range(4):
                    k.op("pe", lambda g, kc=kc, j=j: g.matmul(P[4][:, j * 36:(j + 1) * 36], lhsT=h2f[i][:, j * 128:(j + 1) * 128], rhs=wgrs[:, kc, :],
                                                             start=(kc == 0 and j == 0), stop=(kc == 15), skip_group_check=True), ins=[bh2f[i], bwgr], outs=[bP[4]])
            R = lambda a, b_: rt[:, a, 0:b_]
            def dv(fn, extra=()):
                k.op("dve", fn, ins=[brt, bP[4], bwgr] + list(extra), outs=[brt])
            for j in range(4):
                lg = rt[:, 0, :]
                dv(lambda g: g.tensor_tensor(out=lg, in0=P[4][:, j * 36:(j + 1) * 36], in1=bgrs[:], op=ALU.add))
                dv(lambda g: g.tensor_reduce(out=R(1, 1), in_=lg[:, 0:4], axis=AX.X, op=ALU.max))
                dv(lambda g: g.tensor_scalar(out=R(2, 4), in0=lg[:, 0:4], scalar1=R(1, 1), scalar2=None, op0=ALU.is_equal))
                dv(lambda g: g.tensor_scalar(out=R(3, 1), in0=R(1, 1), scalar1=-1.0, scalar2=None, op0=ALU.mult))
                k.op("act", lambda g: g.activation(out=R(4, 4), in_=lg[:, 0:4], func=AF.Exp, bias=R(3, 1)), ins=[brt], outs=[brt])
                dv(lambda g: g.tensor_reduce(out=R(5, 1), in_=R(4, 4), axis=AX.X, op=ALU.add))
                dv(lambda g: g.reciprocal(out=R(5, 1), in_=R(5, 1)))
                dv(lambda g: g.tensor_scalar(out=R(6, 8), in0=lg[:, 4:12], scalar1=rt[:, 2, 0:1], scalar2=None, op0=ALU.mult))
                for gi in range(1, 4):
                    dv(lambda g, gi=gi: g.scalar_tensor_tensor(out=R(6, 8), in0=lg[:, 4 + 8 * gi:12 + 8 * gi], scalar=rt[:, 2, gi:gi + 1], in1=R(6, 8),
                                                               op0=ALU.mult, op1=ALU.add))
                dv(lambda g: g.tensor_reduce(out=R(7, 1), in_=R(6, 8), axis=AX.X, op=ALU.max))
                dv(lambda g: g.tensor_scalar(out=R(8, 8), in0=R(6, 8), scalar1=R(7, 1), scalar2=None, op0=ALU.is_equal))
                dv(lambda g: g.scalar_tensor_tensor(out=R(9, 8), in0=R(8, 8), scalar=-1e30, in1=R(6, 8), op0=ALU.mult, op1=ALU.add))
                dv(lambda g: g.tensor_reduce(out=R(10, 1), in_=R(9, 8), axis=AX.X, op=ALU.max))
                dv(lambda g: g.tensor_scalar(out=R(11, 8), in0=R(9, 8), scalar1=R(10, 1), scalar2=None, op0=ALU.is_equal))
                dv(lambda g: g.tensor_tensor(out=R(12, 1), in0=R(10, 1), in1=R(7, 1), op=ALU.subtract))
                k.op("act", lambda g: g.activation(out=R(12, 1), in_=R(12, 1), func=AF.Exp), ins=[brt], outs=[brt])
                dv(lambda g: g.tensor_scalar(out=R(13, 1), in0=R(12, 1), scalar1=1.0, scalar2=None, op0=ALU.add))
                dv(lambda g: g.reciprocal(out=R(13, 1), in_=R(13, 1)))
                dv(lambda g: g.tensor_tensor(out=R(13, 1), in0=R(13, 1), in1=R(5, 1), op=ALU.mult))
                dv(lambda g: g.tensor_tensor(out=R(14, 1), in0=R(13, 1), in1=R(12, 1), op=ALU.mult))
                dv(lambda g: g.tensor_scalar(out=R(15, 8), in0=R(8, 8), scalar1=R(13, 1), scalar2=None, op0=ALU.mult))
                dv(lambda g: g.scalar_tensor_tensor(out=R(15, 8), in0=R(11, 8), scalar=R(14, 1), in1=R(15, 8), op0=ALU.mult, op1=ALU.add))
                gts = rt[:, 1, 4:36]
                for gi in range(4):
                    dv(lambda g, gi=gi: g.tensor_scalar(out=gts[:, gi * 8:(gi + 1) * 8], in0=R(15, 8), scalar1=rt[:, 2, gi:gi + 1], scalar2=None, op0=ALU.mult))
                k.op("pe", lambda g: g.transpose(P[5][0:32, 0:128], gts, identf[:]), ins=[brt, bc], outs=[bP[5]])
                k.op("act", lambda g, j=j: g.activation(out=gT[:, j * 128:(j + 1) * 128], in_=P[5][0:32, 0:128], func=AF.Identity), ins=[bP[5]], outs=[bgT])
            for half in range(2):
                for el in range(16):
                    e = half * 16 + el
                    gsl = gbs[e % 2]; bgsl = bgbs[e % 2]
                    k.op("pool", lambda g, e=e: g.tensor_scalar(out=gsel[e % 2][:], in0=gT[:], scalar1=identf[0:32, e:e + 1], scalar2=None, op0=ALU.mult),
                         ins=[bc, bgT], outs=[bgsel[e % 2]])
                    k.op("pe", lambda g, e=e: g.matmul(P[3][:], lhsT=onesf[0:32, :], rhs=gsel[e % 2][:], start=True, stop=True), ins=[bc, bgsel[e % 2]], outs=[bP[3]])
                    k.op("act", lambda g: g.activation(out=gsl[:], in_=P[3][:], func=AF.Identity), ins=[bP[3]], outs=[bgsl])
                    w1v, bw1 = wtile(w1_v[:, e, :, :], (16, 256))
                    w3v, bw3 = wtile(w3_v[:, e, :, :], (16, 256))
                    for f in range(2):
                        pa, bpa = accbank()
                        pb, bpb = accbank()
                        for kc in range(16):
                            k.op("pe", lambda g, kc=kc: g.matmul(pa[:], lhsT=w1v[:, kc, f * 128:(f + 1) * 128], rhs=mh[:, kc, :], start=(kc == 0), stop=(kc == 15)),
                                 ins=[bw1, bmh], outs=[bpa])
                        for kc in range(16):
                            k.op("pe", lambda g, kc=kc: g.matmul(pb[:], lhsT=w3v[:, kc, f * 128:(f + 1) * 128], rhs=mh[:, kc, :], start=(kc == 0), stop=(kc == 15)),
                                 ins=[bw3, bmh], outs=[bpb])
                        k.op("act", lambda g: g.activation(out=t1[:], in_=pa[:], func=AF.Silu), ins=[bpa], outs=[bt1])
                        k.op("dve", lambda g: g.tensor_tensor(out=t2[:], in0=pb[:], in1=t1[:], op=ALU.mult), ins=[bpb, bt1], outs=[bt2])
                        k.op("pool", lambda g, el=el, f=f: g.tensor_tensor(out=hid[:, el * 2 + f, :], in0=t2[:], in1=gsl[:], op=ALU.mult), ins=[bt2, bgsl], outs=[bhid])
                for n in range(16):
                    wv, bwv = wtile(w2_v[:, half * 16:(half + 1) * 16, :, n * 128:(n + 1) * 128].rearrange("p e f n -> p (e f) n"), (32, 128))
                    ps, bps = accbank()
                    for kk in range(32):
                        k.op("pe", lambda g, kk=kk: g.matmul(ps[:], lhsT=wv[:, kk, :], rhs=hid[:, kk, :], start=(kk == 0), stop=(kk == 31)), ins=[bwv, bhid], outs=[bps])
                    k.op("dve", lambda g: g.scalar_tensor_tensor(out=xt[:, n, :], in0=ps[:], scalar=vin[:, 4, n:n + 1], in1=xt[:, n, :], op0=ALU.mult, op1=ALU.add),
                         ins=[bps, bvin, bx], outs=[bx])
            k.dma(xnT_v[:, :, tsl], xt[:], ins=[bx], outs=[bout])
            if last:
                k.op("act", lambda g: g.activation(out=sq, in_=xt[:], func=AF.Square), ins=[bx], outs=[bhid])
                for kc in range(16):
                    k.op("pe", lambda g, kc=kc: g.matmul(P[6][:], lhsT=ones_bf[:], rhs=sq[:, kc, :], start=(kc == 0), stop=(kc == 15)), ins=[bc, bhid], outs=[bP[6]])
                k.op("act", lambda g: g.activation(out=rstd[:], in_=P[6][:], func=AF.Sqrt, scale=1.0 / D, bias=EPS), ins=[bP[6]], outs=[brstd])
                k.op("dve", lambda g: g.reciprocal(out=rstd[:], in_=rstd[:]), ins=[brstd], outs=[brstd])
                for kc in range(16):
                    i = kc % 2
                    k.op("dve", lambda g, kc=kc: g.tensor_tensor(out=tmp[:, i, :], in0=xt[:, kc, :], in1=rstd[:], op=ALU.mult), ins=[bx, brstd], outs=[btmp[i]])
                    k.op("act", lambda g, kc=kc: g.activation(out=h2f[i][:], in_=tmp[:, i, :], func=AF.Identity, scale=vecf[:, 0, kc:kc + 1], bias=vecf[:, 1, kc:kc + 1]),
                         ins=[btmp[i], bvecf], outs=[bh2f[i]])
                    k.dma(outT[kc * 128:(kc + 1) * 128, tsl], h2f[i][:], ins=[bh2f[i]], outs=[bout])
        k.finish([bout])
    return nc


def build_C2(last):
    NTOK = 2048
    NT = 512
    nc = new_nc()
    xT = nc.dram_tensor("xT", [D, NTOK], F32, kind="ExternalInput").ap()
    pgT = nc.dram_tensor("pgT", [3 * D, NTOK], F32, kind="ExternalInput").ap()
    yT = nc.dram_tensor("yT", [3072, NTOK], BF16, kind="ExternalInput").ap()
    vecs = nc.dram_tensor("vecs", [128, 8, 16], F32, kind="ExternalInput").ap()
    wglu = nc.dram_tensor("wglu", [1024, 1024], F32, kind="ExternalInput").ap()
    pw = [nc.dram_tensor(n, [1024, D], F32, kind="ExternalInput").ap() for n in ("psb", "ps5", "pml")]
    wout = nc.dram_tensor("wout", [D, D], F32, kind="ExternalInput").ap()
    wgr = nc.dram_tensor("wgr", [D, 36], F32, kind="ExternalInput").ap()
    bgr = nc.dram_tensor("bgr", [128, 36], F32, kind="ExternalInput").ap()
    w1 = nc.dram_tensor("w1", [32, D, 256], F32, kind="ExternalInput").ap()
    w3 = nc.dram_tensor("w3", [32, D, 256], F32, kind="ExternalInput").ap()
    w2 = nc.dram_tensor("w2", [32, 256, D], F32, kind="ExternalInput").ap()
    xnT = nc.dram_tensor("xnT", [D, NTOK], F32, kind="ExternalOutput").ap()
    if last:
        outT = nc.dram_tensor("outT", [D, NTOK], F32, kind="ExternalOutput").ap()
    xT_v = xT.rearrange("(kc p) t -> p kc t", p=128)
    xnT_v = xnT.rearrange("(kc p) t -> p kc t", p=128)
    yT_v = yT.rearrange("(kc p) t -> p kc t", p=128)
    pg_v = pgT.rearrange("(br n p) t -> p br n t", br=3, p=128)
    wglu_v = wglu.rearrange("(kc p) n -> p kc n", p=128)
    pw_v = [a.rearrange("(kc p) n -> p kc n", p=128) for a in pw]
    wout_v = wout.rearrange("(kc p) n -> p kc n", p=128)
    wgr_v = wgr.rearrange("(kc p) n -> p kc n", p=128)
    w1_v = w1.rearrange("e (kc p) f -> p e kc f", p=128)
    w3_v = w3.rearrange("e (kc p) f -> p e kc f", p=128)
    w2_v = w2.rearrange("e (f p) n -> p e f n", p=128)
    xmid_d = nc.dram_tensor("xmid_d", [D, NTOK], F32).ap()
    xmid_v = xmid_d.rearrange("(kc p) t -> p kc t", p=128)
    with contextlib.ExitStack() as st:
        k = K(nc, st)
        identf = k.sb("identf", [128, 128], F32); onesf = k.sb("onesf", [128, 128], F32); ones_bf = k.sb("ones_bf", [128, 128], BF16)
        bc = k.buf()
        k.op("pool", lambda g: g.memset(onesf[:], 1.0), outs=[bc])
        k.op("pool", lambda g: g.memset(ones_bf[:], 1.0), ins=[bc], outs=[bc])
        k.op("pool", lambda g: g.affine_select(out=identf[:], in_=onesf[:], pattern=[[-1, 128]], compare_op=ALU.is_equal,
                                               fill=0.0, base=0, channel_multiplier=1), ins=[bc], outs=[bc])
        vin = k.sb("vin", [128, 8, 16], F32); bvin = k.buf()
        vec = k.sb("vec", [128, 2, 16], F32); bvec = k.buf()
        vecf = k.sb("vecf", [128, 2, 16], F32); bvecf = k.buf()
        wgrs = k.sb("wgrs", [128, 16, 36], F32); bgrs = k.sb("bgrs", [128, 36], F32); bwgr = k.buf()
        k.dma(vin[:], vecs, outs=[bvin]); k.dma(wgrs[:], wgr_v, outs=[bwgr]); k.dma(bgrs[:], bgr, outs=[bwgr])
        k.op("dve", lambda g: g.scalar_tensor_tensor(out=vec[:, 0, :], in0=vin[:, 2, :], scalar=1.0, in1=vin[:, 1, :], op0=ALU.add, op1=ALU.mult), ins=[bvin], outs=[bvec])
        k.op("dve", lambda g: g.tensor_copy(out=vec[:, 1, :], in_=vin[:, 3, :]), ins=[bvin, bvec], outs=[bvec])
        k.op("dve", lambda g: g.tensor_copy(out=vecf[:, 0, :], in_=vin[:, 5, :]), ins=[bvin], outs=[bvecf])
        k.op("dve", lambda g: g.memset(vecf[:, 1, :], 0.0), ins=[bvecf], outs=[bvecf])
        mh2 = k.sb("mh2", [128, 16, NTOK], BF16); bmh2 = k.bufs(NTOK // NT)
        gT = k.sb("gT", [32, NTOK], F32); bgT = k.bufs(NTOK // NT)
        P = [k.ps("P%d" % i, [128, 512]) for i in range(8)]; bP = k.bufs(8)
        ACC = [0, 1, 2, 7]
        bout = k.buf()
        bxm = [[k.buf() for _ in range(NTOK // NT)] for _ in range(16)]
        wcnt = [0]
        ccnt = [0]
        acnt = [0]

        def accbank():
            i = ACC[acnt[0] % 4]
            acnt[0] += 1
            return P[i], bP[i]

        outer = k.stack
        with contextlib.ExitStack() as s1:
            k.stack = s1
            xt = k.sb("xt", [128, 16, NT], F32); bx = k.buf()
            yt = k.sb("yt", [128, 24, NT], BF16); byt = k.buf()
            sq = yt[:, 0:16, :]
            ysg = k.sb("ysg", [128, 8, NT], BF16); bysg = k.buf()
            mh = k.sb("mh", [128, 16, NT], BF16); bmh = k.buf()
            NW = 2
            wst = [k.sb("wst%d" % i, [128, 2048], F32) for i in range(NW)]; bwst = k.bufs(NW)
            wbf = [k.sb("wbf%d" % i, [128, 2048], BF16) for i in range(NW)]; bwbf = k.bufs(NW)
            pgt = [k.sb("pgt%d" % i, [128, 3, NT], F32) for i in range(1)] * 2; bpgt = [k.buf()] * 2
            sg = [k.sb("sg%d" % i, [128, NT], F32) for i in range(3)]; bsg = k.bufs(3)
            t1 = k.sb("t1", [128, NT], F32); t2 = k.sb("t2", [128, NT], F32); bt1 = k.buf(); bt2 = k.buf()
            rstd = k.sb("rstd", [128, NT], F32); brstd = k.buf()
            tmp = [t1, t2]; btmp = [bt1, bt2]
            h2f = [k.sb("h2f%d" % i, [128, NT], F32) for i in range(2)]; bh2f = k.bufs(2)
            rt = k.sb("rt", [128, 16, 36], F32); brt = k.buf()

            def wtile(view, shape):
                i = wcnt[0] % NW
                wcnt[0] += 1
                a, b = shape
                sv = wst[i][:, 0:a * b].rearrange("p (a b) -> p a b", b=b)
                bv = wbf[i][:, 0:a * b].rearrange("p (a b) -> p a b", b=b)
                k.dma(sv, view, outs=[bwst[i]])
                ce = ("act", "dve", "act", "pool")[ccnt[0] % 4]
                ccnt[0] += 1
                if ce == "act":
                    k.op("act", lambda g: g.activation(out=bv, in_=sv, func=AF.Identity), ins=[bwst[i]], outs=[bwbf[i]])
                else:
                    k.op(ce, lambda g: g.tensor_copy(out=bv, in_=sv), ins=[bwst[i]], outs=[bwbf[i]])
                return bv, bwbf[i]

            for tt in range(NTOK // NT):
                tsl = slice(tt * NT, (tt + 1) * NT)
                k.dma(xt[:], xT_v[:, :, tsl], outs=[bx])
                k.dma(yt[:], yT_v[:, :, tsl], outs=[byt])
                for n in range(8):
                    wv, bwv = wtile(wglu_v[:, :, n * 128:(n + 1) * 128], (8, 128))
                    ps, bps = accbank()
                    for kc in range(8):
                        k.op("pe", lambda g, kc=kc: g.matmul(ps[:], lhsT=wv[:, kc, :], rhs=yt[:, 8 + kc, :], start=(kc == 0), stop=(kc == 7)), ins=[bwv, byt], outs=[bps])
                    k.op("act", lambda g: g.activation(out=sg[0][:], in_=ps[:], func=AF.Sigmoid), ins=[bps], outs=[bsg[0]])
                    k.op("dve", lambda g: g.tensor_tensor(out=ysg[:, n, :], in0=yt[:, 8 + n, :], in1=sg[0][:], op=ALU.mult), ins=[byt, bsg[0]], outs=[bysg])
                for n in range(16):
                    pgs = pgt[n % 2]; bpgs = bpgt[n % 2]
                    k.dma(pgs[:], pg_v[:, :, n, tsl], outs=[bpgs])
                    banks = []
                    for br in range(3):
                        wv, bwv = wtile(pw_v[br][:, :, n * 128:(n + 1) * 128], (8, 128))
                        ps, bps = accbank()
                        for kc in range(8):
                            rhs = ysg[:, kc, :] if br == 1 else yt[:, br * 8 + kc, :]
                            k.op("pe", lambda g, kc=kc, rhs=rhs: g.matmul(ps[:], lhsT=wv[:, kc, :], rhs=rhs, start=(kc == 0), stop=(kc == 7)),
                                 ins=[bwv, byt, bysg], outs=[bps])
                        k.op("act", lambda g, br=br: g.activation(out=sg[br][:], in_=pgs[:, br, :], func=AF.Sigmoid), ins=[bpgs], outs=[bsg[br]])
                        banks.append((ps, bps))
                    k.op("dve", lambda g: g.tensor_tensor(out=t1[:], in0=banks[0][0][:], in1=sg[0][:], op=ALU.mult), ins=[banks[0][1], bsg[0]], outs=[bt1])
                    k.op("dve", lambda g: g.tensor_tensor(out=t2[:], in0=banks[1][0][:], in1=sg[1][:], op=ALU.mult), ins=[banks[1][1], bsg[1]], outs=[bt2])
                    k.op("pool", lambda g: g.tensor_tensor(out=t1[:], in0=t1[:], in1=t2[:], op=ALU.add), ins=[bt1, bt2], outs=[bt1])
                    k.op("dve", lambda g: g.tensor_tensor(out=t2[:], in0=banks[2][0][:], in1=sg[2][:], op=ALU.mult), ins=[banks[2][1], bsg[2], bt1], outs=[bt2])
                    k.op("pool", lambda g: g.tensor_tensor(out=mh[:, n, :], in0=t1[:], in1=t2[:], op=ALU.add), ins=[bt1, bt2], outs=[bmh])
                for n in range(16):
                    wv, bwv = wtile(wout_v[:, :, n * 128:(n + 1) * 128], (16, 128))
                    ps, bps = accbank()
                    for kc in range(16):
                        k.op("pe", lambda g, kc=kc: g.matmul(ps[:], lhsT=wv[:, kc, :], rhs=mh[:, kc, :], start=(kc == 0), stop=(kc == 15)), ins=[bwv, bmh], outs=[bps])
                    k.op("dve", lambda g: g.scalar_tensor_tensor(out=xt[:, n, :], in0=ps[:], scalar=vin[:, 0, n:n + 1], in1=xt[:, n, :], op0=ALU.mult, op1=ALU.add),
                         ins=[bps, bvin, bx], outs=[bx])
                k.op("act", lambda g: g.activation(out=sq, in_=xt[:], func=AF.Square), ins=[bx], outs=[byt])
                for kc in range(16):
                    k.op("pe", lambda g, kc=kc: g.matmul(P[6][:], lhsT=ones_bf[:], rhs=sq[:, kc, :], start=(kc == 0), stop=(kc == 15)), ins=[bc, byt], outs=[bP[6]])
                k.op("act", lambda g: g.activation(out=rstd[:], in_=P[6][:], func=AF.Sqrt, scale=1.0 / D, bias=EPS), ins=[bP[6]], outs=[brstd])
                k.op("dve", lambda g: g.reciprocal(out=rstd[:], in_=rstd[:]), ins=[brstd], outs=[brstd])
                for kc in range(16):
                    i = kc % 2
                    k.op("dve", lambda g, kc=kc: g.tensor_tensor(out=tmp[i][:], in0=xt[:, kc, :], in1=rstd[:], op=ALU.mult), ins=[bx, brstd], outs=[btmp[i]])
                    k.op("act", lambda g, kc=kc: g.activation(out=h2f[i][:], in_=tmp[i][:], func=AF.Identity, scale=vec[:, 0, kc:kc + 1], bias=vec[:, 1, kc:kc + 1]),
                         ins=[btmp[i], bvec], outs=[bh2f[i]])
                    k.op("pool", lambda g, kc=kc: g.tensor_copy(out=mh2[:, kc, tsl], in_=h2f[i][:]), ins=[bh2f[i]], outs=[bmh2[tt]])
                    for j in range(4):
                        k.op("pe", lambda g, kc=kc, j=j: g.matmul(P[4][:, j * 36:(j + 1) * 36], lhsT=h2f[i][:, j * 128:(j + 1) * 128], rhs=wgrs[:, kc, :],
                                                                 start=(kc == 0 and j == 0), stop=(kc == 15), skip_group_check=True), ins=[bh2f[i], bwgr], outs=[bP[4]])
                R = lambda a, b_: rt[:, a, 0:b_]
                def dv(fn, extra=()):
                    k.op("dve", fn, ins=[brt, bP[4], bwgr] + list(extra), outs=[brt])
                for j in range(4):
                    lg = rt[:, 0, :]
                    dv(lambda g: g.tensor_tensor(out=lg, in0=P[4][:, j * 36:(j + 1) * 36], in1=bgrs[:], op=ALU.add))
                    dv(lambda g: g.tensor_reduce(out=R(1, 1), in_=lg[:, 0:4], axis=AX.X, op=ALU.max))
                    dv(lambda g: g.tensor_scalar(out=R(2, 4), in0=lg[:, 0:4], scalar1=R(1, 1), scalar2=None, op0=ALU.is_equal))
                    dv(lambda g: g.tensor_scalar(out=R(3, 1), in0=R(1, 1), scalar1=-1.0, scalar2=None, op0=ALU.mult))
                    k.op("act", lambda g: g.activation(out=R(4, 4), in_=lg[:, 0:4], func=AF.Exp, bias=R(3, 1)), ins=[brt], outs=[brt])
                    dv(lambda g: g.tensor_reduce(out=R(5, 1), in_=R(4, 4), axis=AX.X, op=ALU.add))
                    dv(lambda g: g.reciprocal(out=R(5, 1), in_=R(5, 1)))
                    dv(lambda g: g.tensor_scalar(out=R(6, 8), in0=lg[:, 4:12], scalar1=rt[:, 2, 0:1], scalar2=None, op0=ALU.mult))
                    for gi in range(1, 4):
                        dv(lambda g, gi=gi: g.scalar_tensor_tensor(out=R(6, 8), in0=lg[:, 4 + 8 * gi:12 + 8 * gi], scalar=rt[:, 2, gi:gi + 1], in1=R(6, 8),
                                                                   op0=ALU.mult, op1=ALU.add))
                    dv(lambda g: g.tensor_reduce(out=R(7, 1), in_=R(6, 8), axis=AX.X, op=ALU.max))
                    dv(lambda g: g.tensor_scalar(out=R(8, 8), in0=R(6, 8), scalar1=R(7, 1), scalar2=None, op0=ALU.is_equal))
                    dv(lambda g: g.scalar_tensor_tensor(out=R(9, 8), in0=R(8, 8), scalar=-1e30, in1=R(6, 8), op0=ALU.mult, op1=ALU.add))
                    dv(lambda g: g.tensor_reduce(out=R(10, 1), in_=R(9, 8), axis=AX.X, op=ALU.max))
                    dv(lambda g: g.tensor_scalar(out=R(11, 8), in0=R(9, 8), scalar1=R(10, 1), scalar2=None, op0=ALU.is_equal))
                    dv(lambda g: g.tensor_tensor(out=R(12, 1), in0=R(10, 1), in1=R(7, 1), op=ALU.subtract))
                    k.op("act", lambda g: g.activation(out=R(12, 1), in_=R(12, 1), func=AF.Exp), ins=[brt], outs=[brt])
                    dv(lambda g: g.tensor_scalar(out=R(13, 1), in0=R(12, 1), scalar1=1.0, scalar2=None, op0=ALU.add))
                    dv(lambda g: g.reciprocal(out=R(13, 1), in_=R(13, 1)))
                    dv(lambda g: g.tensor_tensor(out=R(13, 1), in0=R(13, 1), in1=R(5, 1), op=ALU.mult))
                    dv(lambda g: g.tensor_tensor(out=R(14, 1), in0=R(13, 1), in1=R(12, 1), op=ALU.mult))
                    dv(lambda g: g.tensor_scalar(out=R(15, 8), in0=R(8, 8), scalar1=R(13, 1), scalar2=None, op0=ALU.mult))
                    dv(lambda g: g.scalar_tensor_tensor(out=R(15, 8), in0=R(11, 8), scalar=R(14, 1), in1=R(15, 8), op0=ALU.mult, op1=ALU.add))
                    gts = rt[:, 1, 4:36]
                    for gi in range(4):
                        dv(lambda g, gi=gi: g.tensor_scalar(out=gts[:, gi * 8:(gi + 1) * 8], in0=R(15, 8), scalar1=rt[:, 2, gi:gi + 1], scalar2=None, op0=ALU.mult))
                    k.op("pe", lambda g: g.transpose(P[5][0:32, 0:128], gts, identf[:]), ins=[brt, bc], outs=[bP[5]])
                    k.op("act", lambda g, j=j: g.activation(out=gT[:, tt * NT + j * 128:tt * NT + (j + 1) * 128], in_=P[5][0:32, 0:128], func=AF.Identity), ins=[bP[5]], outs=[bgT[tt]])
                k.dma(xmid_v[:, :, tsl], xt[:], ins=[bx], outs=[bxm[n][tt] for n in range(16)])
            k.barrier()
        k.stack = outer
        with contextlib.ExitStack() as s2:
            k.stack = s2
            hid = k.sb("hid", [128, 16, NTOK], BF16); bhid = k.bufs(NTOK // NT)
            wst = [k.sb("wstb%d" % i, [128, 4096], F32) for i in range(2)]; bwst = k.bufs(2)
            wbf = [k.sb("wbfb%d" % i, [128, 4096], BF16) for i in range(2)]; bwbf = k.bufs(2)
            gsel = [k.sb("gsel%d" % i, [32, NT], F32) for i in range(2)]; bgsel = k.bufs(2)
            gbs = [k.sb("gbs%d" % i, [128, NT], F32) for i in range(2)]; bgbs = k.bufs(2)
            t1 = k.sb("t1b", [128, NT], F32); t2 = k.sb("t2b", [128, NT], F32); bt1 = k.buf(); bt2 = k.buf()
            NX = 3
            xs = [k.sb("xs%d" % i, [128, NT], F32) for i in range(NX)]; bxs = k.bufs(NX)
            xcnt = [0]
            wcnt[0] = 0

            def wtile2(view, shape):
                i = wcnt[0] % 2
                wcnt[0] += 1
                a, b = shape
                sv = wst[i][:, 0:a * b].rearrange("p (a b) -> p a b", b=b)
                bv = wbf[i][:, 0:a * b].rearrange("p (a b) -> p a b", b=b)
                k.dma(sv, view, outs=[bwst[i]])
                ce = ("act", "dve", "act", "pool")[ccnt[0] % 4]
                ccnt[0] += 1
                if ce == "act":
                    k.op("act", lambda g: g.activation(out=bv, in_=sv, func=AF.Identity), ins=[bwst[i]], outs=[bwbf[i]])
                else:
                    k.op(ce, lambda g: g.tensor_copy(out=bv, in_=sv), ins=[bwst[i]], outs=[bwbf[i]])
                return bv, bwbf[i]

            NG = 4
            for grp in range(NG):
                for el in range(8):
                    e = grp * 8 + el
                    w1v, bw1 = wtile2(w1_v[:, e, :, :], (16, 256))
                    w3v, bw3 = wtile2(w3_v[:, e, :, :], (16, 256))
                    for tt in range(NTOK // NT):
                        tsl = slice(tt * NT, (tt + 1) * NT)
                        gi = (e * 4 + tt) % 2
                        gsl = gbs[gi]; bgsl = bgbs[gi]
                        k.op("dve", lambda g, e=e, gi=gi, tsl=tsl: g.tensor_scalar(out=gsel[gi][:], in0=gT[:, tsl], scalar1=identf[0:32, e:e + 1], scalar2=None, op0=ALU.mult),
                             ins=[bc, bgT[tt]], outs=[bgsel[gi]])
                        k.op("pe", lambda g, gi=gi: g.matmul(P[3][:], lhsT=onesf[0:32, :], rhs=gsel[gi][:], start=True, stop=True), ins=[bc, bgsel[gi]], outs=[bP[3]])
                        k.op("act", lambda g, gsl=gsl: g.activation(out=gsl[:], in_=P[3][:], func=AF.Identity), ins=[bP[3]], outs=[bgsl])
                        for f in range(2):
                            pa, bpa = accbank()
                            pb, bpb = accbank()
                            for kc in range(16):
                                k.op("pe", lambda g, kc=kc, pa=pa, f=f, tsl=tsl: g.matmul(pa[:], lhsT=w1v[:, kc, f * 128:(f + 1) * 128], rhs=mh2[:, kc, tsl], start=(kc == 0), stop=(kc == 15)),
                                     ins=[bw1, bmh2[tt]], outs=[bpa])
                            for kc in range(16):
                                k.op("pe", lambda g, kc=kc, pb=pb, f=f, tsl=tsl: g.matmul(pb[:], lhsT=w3v[:, kc, f * 128:(f + 1) * 128], rhs=mh2[:, kc, tsl], start=(kc == 0), stop=(kc == 15)),
                                     ins=[bw3, bmh2[tt]], outs=[bpb])
                            k.op("act", lambda g, pa=pa: g.activation(out=t1[:], in_=pa[:], func=AF.Silu), ins=[bpa], outs=[bt1])
                            k.op("dve", lambda g, pb=pb: g.tensor_tensor(out=t2[:], in0=pb[:], in1=t1[:], op=ALU.mult), ins=[bpb, bt1], outs=[bt2])
                            k.op("pool", lambda g, el=el, f=f, tsl=tsl, gsl=gsl: g.tensor_tensor(out=hid[:, el * 2 + f, tsl], in0=t2[:], in1=gsl[:], op=ALU.mult),
                                 ins=[bt2, bgsl], outs=[bhid[tt]])
                for n in range(16):
                    wv, bwv = wtile2(w2_v[:, grp * 8:(grp + 1) * 8, :, n * 128:(n + 1) * 128].rearrange("p e f n -> p (e f) n"), (16, 128))
                    for tt in range(NTOK // NT):
                        tsl = slice(tt * NT, (tt + 1) * NT)
                        xi = xcnt[0] % NX
                        xcnt[0] += 1
                        k.dma(xs[xi][:], xmid_d[n * 128:(n + 1) * 128, tsl], ins=[bxm[n][tt]], outs=[bxs[xi]])
                        ps, bps = accbank()
                        for kk in range(16):
                            k.op("pe", lambda g, kk=kk, ps=ps, tsl=tsl: g.matmul(ps[:], lhsT=wv[:, kk, :], rhs=hid[:, kk, tsl], start=(kk == 0), stop=(kk == 15)), ins=[bwv, bhid[tt]], outs=[bps])
                        k.op("dve", lambda g, ps=ps, xi=xi, n=n: g.scalar_tensor_tensor(out=xs[xi][:], in0=ps[:], scalar=vin[:, 4, n:n + 1], in1=xs[xi][:], op0=ALU.mult, op1=ALU.add),
                             ins=[bps, bvin, bxs[xi]], outs=[bxs[xi]])
                        if grp < NG - 1 or last:
                            k.dma(xmid_d[n * 128:(n + 1) * 128, tsl], xs[xi][:], ins=[bxs[xi]], outs=[bxm[n][tt]])
                        if grp == NG - 1:
                            k.dma(xnT[n * 128:(n + 1) * 128, tsl], xs[xi][:], ins=[bxs[xi]], outs=[bout])
            k.barrier()
        k.stack = outer
        if last:
            with contextlib.ExitStack() as s3:
                k.stack = s3
                xt = k.sb("xt3", [128, 16, NT], F32); bx = k.buf()
                sq = k.sb("sq3", [128, 16, NT], BF16); bsq = k.buf()
                rstd = k.sb("rstd3", [128, NT], F32); brstd = k.buf()
                tmp = k.sb("tmp3", [128, 2, NT], F32); btmp = k.bufs(2)
                h2f = [k.sb("o3_%d" % i, [128, NT], F32) for i in range(2)]; bh2f = k.bufs(2)
                for tt in range(NTOK // NT):
                    tsl = slice(tt * NT, (tt + 1) * NT)
                    k.dma(xt[:], xmid_v[:, :, tsl], ins=[bxm[n][tt] for n in range(16)], outs=[bx])
                    k.op("act", lambda g: g.activation(out=sq[:], in_=xt[:], func=AF.Square), ins=[bx], outs=[bsq])
                    for kc in range(16):
                        k.op("pe", lambda g, kc=kc: g.matmul(P[6][:], lhsT=ones_bf[:], rhs=sq[:, kc, :], start=(kc == 0), stop=(kc == 15)), ins=[bc, bsq], outs=[bP[6]])
                    k.op("act", lambda g: g.activation(out=rstd[:], in_=P[6][:], func=AF.Sqrt, scale=1.0 / D, bias=EPS), ins=[bP[6]], outs=[brstd])
                    k.op("dve", lambda g: g.reciprocal(out=rstd[:], in_=rstd[:]), ins=[brstd], outs=[brstd])
                    for kc in range(16):
                        i = kc % 2
                        k.op("dve", lambda g, kc=kc, i=i: g.tensor_tensor(out=tmp[:, i, :], in0=xt[:, kc, :], in1=rstd[:], op=ALU.mult), ins=[bx, brstd], outs=[btmp[i]])
                        k.op("act", lambda g, kc=kc, i=i: g.activation(out=h2f[i][:], in_=tmp[:, i, :], func=AF.Identity, scale=vecf[:, 0, kc:kc + 1], bias=vecf[:, 1, kc:kc + 1]),
                             ins=[btmp[i], bvecf], outs=[bh2f[i]])
                        k.dma(outT[kc * 128:(kc + 1) * 128, tsl], h2f[i][:], ins=[bh2f[i]], outs=[bout])
                k.barrier()
            k.stack = outer
        k.finish([bout])
    return nc


_NC_CACHE = {}


def _get_nc(name, builder, *a):
    key = (name,) + a
    if key not in _NC_CACHE:
        _NC_CACHE[key] = builder(*a)
    return _NC_CACHE[key]


def _run(nc, in_maps):
    res = run_bass_kernel_spmd(nc, in_maps, core_ids=list(range(len(in_maps))))
    return res.results


def _pk(v):
    return np.ascontiguousarray(np.asarray(v, np.float32).reshape(16, 128).T)


def _c_inputs(c, l, xT_c, pg_c, yT_c, mod_l, inp):
    shift1, scale1, gate1, shift2, scale2, gate2 = np.split(mod_l, 6)
    vecs = np.stack([_pk(gate1), _pk(inp['norm_moe_g'][l]), _pk(scale2), _pk(shift2), _pk(gate2), _pk(inp['final_g']),
                     _pk(gate2), _pk(gate2)], axis=1)
    wgr = np.concatenate([inp['moe_w_group'][l], inp['moe_w_router'][l]], axis=1)
    bgr = np.tile(np.concatenate([inp['moe_b_group'][l], inp['moe_b_router'][l]])[None, :], (128, 1))
    return {"xT": xT_c, "pgT": pg_c, "yT": yT_c, "vecs": np.ascontiguousarray(vecs.astype(np.float32)),
            "wglu": np.ascontiguousarray(inp['s5_w_glu'][l]), "psb": np.ascontiguousarray(inp['p_sb'][l]),
            "ps5": np.ascontiguousarray(inp['p_s5'][l]), "pml": np.ascontiguousarray(inp['p_ml'][l]),
            "wout": np.ascontiguousarray(inp['w_out'][l]), "wgr": np.ascontiguousarray(wgr.astype(np.float32)),
            "bgr": np.ascontiguousarray(bgr.astype(np.float32)), "w1": np.ascontiguousarray(inp['moe_w1'][l]),
            "w3": np.ascontiguousarray(inp['moe_w3'][l]), "w2": np.ascontiguousarray(inp['moe_w2'][l])}


def kernel(**inp):
    inp = {k_: np.asarray(v) for k_, v in inp.items()}
    bf = ml_dtypes.bfloat16
    x = inp['x'][0]
    TS = T // NCORES
    wcat = np.concatenate([inp['w_ada'][0], inp['w_ada'][1]], axis=1)
    bcat = np.concatenate([inp['b_ada'][0], inp['b_ada'][1]])
    ncol = wcat.shape[1] // NCORES
    cT = _pk(inp['c'][0])
    r = _run(_get_nc("ada", build_ADA), [{"cT": cT, "w": np.ascontiguousarray(wcat[:, c * ncol:(c + 1) * ncol]),
                                          "b": np.ascontiguousarray(bcat[None, c * ncol:(c + 1) * ncol])} for c in range(NCORES)])
    mod = np.concatenate([np.asarray(r[c]["mod"])[0] for c in range(NCORES)])
    del wcat
    xT = [np.ascontiguousarray(x[c * TS:(c + 1) * TS].T) for c in range(NCORES)]
    out = None
    for l in range(DEPTH):
        mod_l = mod[l * 6 * D:(l + 1) * 6 * D]
        shift1, scale1 = mod_l[0:D], mod_l[D:2 * D]
        vecsA = np.ascontiguousarray(np.stack([_pk(inp['norm_mix_g'][l]), _pk(scale1), _pk(shift1)], axis=1))
        wA = np.ascontiguousarray(inp['w_in'][l])
        r = _run(_get_nc("A", build_A), [{"xT": xT[c], "vecs": vecsA, "w": wA} for c in range(NCORES)])
        pa = np.concatenate([np.asarray(r[c]["pa"]).T for c in range(NCORES)], axis=0)
        pif = np.concatenate([np.asarray(r[c]["pif"]).T for c in range(NCORES)], axis=0)
        pg = [np.asarray(r[c]["pg"]) for c in range(NCORES)]
        del r
        ims = [{"qT": np.ascontiguousarray(pa[:, c * 128:(c + 1) * 128].T), "kT": np.ascontiguousarray(pa[:, 1024 + c * 128:1024 + (c + 1) * 128].T),
                "v": np.ascontiguousarray(pa[:, 2048 + c * 128:2048 + (c + 1) * 128])} for c in range(NCORES)]
        r = _run(_get_nc("SB", build_SB), ims)
        ysb = np.concatenate([np.asarray(r[c]["ysb"]).T for c in range(NCORES)], axis=1)
        P5 = [inp[k_][l] for k_ in ('s5_lam_re', 's5_lam_im', 's5_log_dt', 's5_b_re', 's5_b_im', 's5_c_re', 's5_c_im', 's5_d')]
        r = _run(_get_nc("S5", build_S5), [s5_host_inputs(c, pa[:, 3072:4096], *P5) for c in range(NCORES)])
        ys5 = np.concatenate([np.asarray(r[c]["ys5"]).transpose(2, 1, 0).reshape(T, 128) for c in range(NCORES)], axis=1)
        r = _run(_get_nc("ML", build_ML), [ml_host_inputs(c, l, pa[:, 4096:5120], pa[:, 5120:6144], pa[:, 6144:7168], pa[:, 7168:8192],
                                                          pif[:, 0:4], pif[:, 4:8], inp['ml_conv'][l], inp['ml_i_bias'][l], inp['ml_f_bias'][l])
                                           for c in range(NCORES)])
        yml = np.concatenate([np.asarray(r[c]["yml"]).T for c in range(NCORES)], axis=1)
        yall = np.concatenate([ysb, ys5, yml], axis=1)
        del pa, ysb, ys5, yml
        last = (l == DEPTH - 1)
        ims = [_c_inputs(c, l, xT[c], pg[c], np.ascontiguousarray(yall[c * TS:(c + 1) * TS].T), mod_l, inp) for c in range(NCORES)]
        r = _run(_get_nc("C", build_C2, last), ims)
        xT = [np.asarray(r[c]["xnT"]) for c in range(NCORES)]
        if last:
            out = np.concatenate([np.asarray(r[c]["outT"]).T for c in range(NCORES)], axis=0)
        del r, ims
    return np.ascontiguousarray(out[None].astype(np.float32))
```

```python
import contextlib
import numpy as np
import ml_dtypes
import concourse.bass as bass
import concourse.mybir as mybir
from concourse.bass_utils import run_bass_kernel_spmd

F32 = mybir.dt.float32
BF16 = mybir.dt.bfloat16
AF = mybir.ActivationFunctionType
ALU = mybir.AluOpType
AX = mybir.AxisListType

NCORES = 8
D = 2048
T = 16384
DEPTH = 2
IN_COLS = 14344
EPS = 1e-6


class Buf:
    __slots__ = ("name", "w", "r")

    def __init__(self, name):
        self.name = name
        self.w = None
        self.r = {}


class K:
    NDMA = 12

    def __init__(self, nc, stack):
        self.nc = nc
        self.stack = stack
        self.eng = {"pe": nc.tensor, "act": nc.scalar, "dve": nc.vector, "pool": nc.gpsimd, "sp": nc.sync}
        self.sem = {}
        self.cnt = {}
        for e in self.eng:
            self.sem[e] = stack.enter_context(nc.semaphore("s_" + e))
            self.cnt[e] = 0
        self.dsem = [stack.enter_context(nc.semaphore("s_dma%d" % i)) for i in range(self.NDMA)]
        self.dcnt = [0] * self.NDMA
        self.dnext = 0
        self.waited = {e: {} for e in self.eng}
        self.nbuf = 0

    def sb(self, name, shape, dt):
        return self.stack.enter_context(self.nc.sbuf_tensor(name, list(shape), dt))

    def ps(self, name, shape, dt=F32):
        return self.stack.enter_context(self.nc.psum_tensor(name, list(shape), dt))

    def buf(self, name=None):
        self.nbuf += 1
        return Buf(name or "b%d" % self.nbuf)

    def bufs(self, n, name="b"):
        return [self.buf("%s%d" % (name, i)) for i in range(n)]

    def _semof(self, key):
        if isinstance(key, int):
            return self.dsem[key]
        return self.sem[key]

    def _collect(self, e, ins, outs):
        need = {}

        def add(tok):
            if tok is None:
                return
            k, v = tok
            if need.get(k, 0) < v:
                need[k] = v

        for b in ins:
            add(b.w)
        for b in outs:
            add(b.w)
            for k, v in b.r.items():
                if k == e:
                    continue
                add((k, v))
        return need

    def _emit_waits(self, e, need):
        eng = self.eng[e]
        wd = self.waited[e]
        for k, v in need.items():
            if e == "pe" and k == "pe":
                continue
            if wd.get(k, 0) >= v:
                continue
            eng.wait_ge(self._semof(k), v)
            wd[k] = v

    def _finish(self, tok, ins, outs):
        k, v = tok
        for b in ins:
            if b.r.get(k, 0) < v:
                b.r[k] = v
        for b in outs:
            b.w = tok
            b.r = {}

    def op(self, e, fn, ins=(), outs=()):
        need = self._collect(e, ins, outs)
        self._emit_waits(e, need)
        inst = fn(self.eng[e])
        self.cnt[e] += 1
        inst.then_inc(self.sem[e], 1)
        tok = (e, self.cnt[e])
        self._finish(tok, ins, outs)
        return tok

    def dma(self, out_ap, in_ap, ins=(), outs=(), q="sp", **kw):
        i = self.dnext
        self.dnext = (self.dnext + 1) % self.NDMA
        need = self._collect(q, ins, outs)
        if self.dcnt[i] > 0:
            need[i] = max(need.get(i, 0), self.dcnt[i])
        self._emit_waits(q, need)
        inst = self.eng[q].dma_start(out=out_ap, in_=in_ap, **kw)
        self.dcnt[i] += 16
        inst.then_inc(self.dsem[i], 16)
        tok = (i, self.dcnt[i])
        self._finish(tok, ins, outs)
        return tok

    def cc(self, kind, out_ap, in_ap, ins=(), outs=(), groups=None):
        q = "pool"
        i = self.dnext
        self.dnext = (self.dnext + 1) % self.NDMA
        need = self._collect(q, ins, outs)
        if self.dcnt[i] > 0:
            need[i] = max(need.get(i, 0), self.dcnt[i])
        self._emit_waits(q, need)
        inst = self.nc.gpsimd.collective_compute(kind, ALU.bypass, replica_groups=groups or [list(range(NCORES))],
                                                 ins=[in_ap], outs=[out_ap])
        self.dcnt[i] += 16
        inst.then_inc(self.dsem[i], 16)
        tok = (i, self.dcnt[i])
        self._finish(tok, ins, outs)
        return tok

    def barrier(self):
        need = {}
        for e in ("pe", "act", "dve", "pool", "sp"):
            if self.cnt[e]:
                need[e] = self.cnt[e]
        for i in range(self.NDMA):
            if self.dcnt[i]:
                need[i] = self.dcnt[i]
        need.pop("sp", None)
        for e in ("pe", "act", "dve", "pool", "sp"):
            self._emit_waits(e, dict(need))

    def finish(self, out_bufs):
        need = {}
        for b in out_bufs:
            if b.w is not None:
                k, v = b.w
                need[k] = max(need.get(k, 0), v)
        for e in ("pe", "act", "dve", "pool"):
            if self.cnt[e]:
                need[e] = self.cnt[e]
        for i in range(self.NDMA):
            if self.dcnt[i]:
                need[i] = self.dcnt[i]
        self._emit_waits("sp", need)


def new_nc():
    return bass.Bass("TRN2", target_bir_lowering=False)


def _evac(k, idx, out_ap, in_ap, ins, outs):
    if idx % 2 == 0:
        return k.op("act", lambda g: g.activation(out=out_ap, in_=in_ap, func=AF.Identity), ins=ins, outs=outs)
    return k.op("dve", lambda g: g.tensor_copy(out=out_ap, in_=in_ap), ins=ins, outs=outs)


def emit_modnorm(k, xt, bx, hT_ap_fn, bh, vec, bvec, ones_bf, bones, sq, bsq, psb, bpsb, rstd, brstd, tmp, btmp, NT=512):
    k.op("act", lambda g: g.activation(out=sq[:], in_=xt[:], func=AF.Square), ins=[bx], outs=[bsq])
    for kc in range(16):
        k.op("pe", lambda g, kc=kc: g.matmul(psb[:, 0:NT], lhsT=ones_bf[:], rhs=sq[:, kc, :], start=(kc == 0), stop=(kc == 15)),
             ins=[bones, bsq], outs=[bpsb])
    k.op("act", lambda g: g.activation(out=rstd[:], in_=psb[:, 0:NT], func=AF.Sqrt, scale=1.0 / D, bias=EPS), ins=[bpsb], outs=[brstd])
    k.op("dve", lambda g: g.reciprocal(out=rstd[:], in_=rstd[:]), ins=[brstd], outs=[brstd])
    for kc in range(16):
        k.op("dve", lambda g, kc=kc: g.tensor_tensor(out=tmp[:, kc % 2, :], in0=xt[:, kc, :], in1=rstd[:], op=ALU.mult),
             ins=[bx, brstd], outs=[btmp[kc % 2]])
        k.op("act", lambda g, kc=kc: g.activation(out=hT_ap_fn(kc), in_=tmp[:, kc % 2, :], func=AF.Identity,
                                                  scale=vec[:, 0, kc:kc + 1], bias=vec[:, 1, kc:kc + 1]),
             ins=[btmp[kc % 2], bvec], outs=[bh])


def build_A():
    NTOK = 2048
    nc = new_nc()
    xT = nc.dram_tensor("xT", [D, NTOK], F32, kind="ExternalInput").ap()
    vecs = nc.dram_tensor("vecs", [128, 3, 16], F32, kind="ExternalInput").ap()
    w = nc.dram_tensor("w", [D, IN_COLS], F32, kind="ExternalInput").ap()
    pa = nc.dram_tensor("pa", [8192, NTOK], BF16, kind="ExternalOutput").ap()
    pif = nc.dram_tensor("pif", [8, NTOK], F32, kind="ExternalOutput").ap()
    pg = nc.dram_tensor("pg", [6144, NTOK], F32, kind="ExternalOutput").ap()
    xT_v = xT.rearrange("(kc p) t -> p kc t", p=128)
    w_v = w.rearrange("(kc p) n -> p kc n", p=128)
    with contextlib.ExitStack() as st:
        k = K(nc, st)
        xt = k.sb("xt", [128, 16, 512], F32); bx = k.buf()
        sq = k.sb("sq", [128, 16, 512], BF16); bsq = k.buf()
        hT = k.sb("hT", [128, 16, NTOK], BF16); bh = k.bufs(4, "bh")
        vin = k.sb("vin", [128, 3, 16], F32); bvin = k.buf()
        vec = k.sb("vec", [128, 2, 16], F32); bvec = k.buf()
        ones_bf = k.sb("ones_bf", [128, 128], BF16); bones = k.buf()
        rstd = k.sb("rstd", [128, 512], F32); brstd = k.buf()
        tmp = k.sb("tmp", [128, 2, 512], F32); btmp = k.bufs(2, "btmp")
        wst = [k.sb("wst%d" % i, [128, 16, 128], F32) for i in range(2)]; bwst = k.bufs(2, "bwst")
        wbf = [k.sb("wbf%d" % i, [128, 16, 128], BF16) for i in range(2)]; bwbf = k.bufs(2, "bwbf")
        ost = [k.sb("ost%d" % i, [128, NTOK], F32) for i in range(2)]; bost = k.bufs(2, "bost")
        ostb = [k.sb("ostb%d" % i, [128, NTOK], BF16) for i in range(2)]; bostb = k.bufs(2, "bostb")
        ps = [k.ps("ps%d" % i, [128, 512]) for i in range(8)]; bps = k.bufs(8, "bps")
        bout = k.buf("out")

        k.op("pool", lambda g: g.memset(ones_bf[:], 1.0), outs=[bones])
        k.dma(vin[:], vecs, outs=[bvin])
        k.op("dve", lambda g: g.scalar_tensor_tensor(out=vec[:, 0, :], in0=vin[:, 1, :], scalar=1.0, in1=vin[:, 0, :],
                                                      op0=ALU.add, op1=ALU.mult), ins=[bvin], outs=[bvec])
        k.op("dve", lambda g: g.tensor_copy(out=vec[:, 1, :], in_=vin[:, 2, :]), ins=[bvin, bvec], outs=[bvec])
        for tt in range(4):
            k.dma(xt[:], xT_v[:, :, tt * 512:(tt + 1) * 512], outs=[bx])
            emit_modnorm(k, xt, bx, lambda kc, tt=tt: hT[:, kc, tt * 512:(tt + 1) * 512], bh[tt], vec, bvec, ones_bf, bones,
                         sq, bsq, ps[0], bps[0], rstd, brstd, tmp, btmp)
        tiles = [(n0, 128) for n0 in range(0, 8192, 128)] + [(8192, 8)] + [(n0, 128) for n0 in range(8200, IN_COLS, 128)]
        for ti, (n0, ncol) in enumerate(tiles):
            s = ti % 2
            k.dma(wst[s][:, :, 0:ncol], w_v[:, :, n0:n0 + ncol], outs=[bwst[s]])
            eng = ("dve", "act")[ti % 2]
            if eng == "act":
                k.op("act", lambda g, s=s, ncol=ncol: g.activation(out=wbf[s][:, :, 0:ncol], in_=wst[s][:, :, 0:ncol], func=AF.Identity),
                     ins=[bwst[s]], outs=[bwbf[s]])
            else:
                k.op(eng, lambda g, s=s, ncol=ncol: g.tensor_copy(out=wbf[s][:, :, 0:ncol], in_=wst[s][:, :, 0:ncol]),
                     ins=[bwst[s]], outs=[bwbf[s]])
            is_bf = n0 < 8192
            o_t, o_b = (ostb[s], bostb[s]) if is_bf else (ost[s], bost[s])
            for tt in range(4):
                pi = (ti % 2) * 4 + tt
                for kc in range(16):
                    k.op("pe", lambda g, pi=pi, s=s, kc=kc, tt=tt, ncol=ncol: g.matmul(
                        ps[pi][0:ncol, :], lhsT=wbf[s][:, kc, 0:ncol], rhs=hT[:, kc, tt * 512:(tt + 1) * 512],
                        start=(kc == 0), stop=(kc == 15)), ins=[bwbf[s], bh[tt]], outs=[bps[pi]])
                _evac(k, tt, o_t[0:ncol, tt * 512:(tt + 1) * 512], ps[pi][0:ncol, :], [bps[pi]], [o_b])
            if is_bf:
                dst = pa[n0:n0 + ncol, :]
            elif ncol == 8:
                dst = pif[:, :]
            else:
                dst = pg[n0 - 8200:n0 - 8200 + ncol, :]
            k.dma(dst, o_t[0:ncol, :], ins=[o_b], outs=[bout], q="pool")
        k.finish([bout])
    return nc


def emit_sb(k, qT, bq, kT, bk, vS, bv, y_dram, by, tag="sb"):
    NQ = T // 512
    SC = 128.0 ** -0.5
    tri = k.sb(tag + "tri", [128, 128], BF16); omt = k.sb(tag + "omt", [128, 128], BF16); bconst = k.buf()
    onesf = k.sb(tag + "onesf", [128, 512], F32)
    masks = k.sb(tag + "masks", [128, 4, 512], F32)
    k.op("pool", lambda g: g.memset(onesf[:], 1.0), outs=[bconst])
    k.op("pool", lambda g: g.affine_select(out=tri[:], in_=onesf[:, 0:128], pattern=[[-1, 128]], compare_op=ALU.is_ge,
                                           fill=0.0, base=0, channel_multiplier=1), ins=[bconst], outs=[bconst])
    k.op("pool", lambda g: g.affine_select(out=omt[:], in_=onesf[:, 0:128], pattern=[[1, 128]], compare_op=ALU.is_gt,
                                           fill=0.0, base=0, channel_multiplier=-1), ins=[bconst], outs=[bconst])
    for j in range(4):
        k.op("pool", lambda g, j=j: g.affine_select(out=masks[:, j, :], in_=onesf[:], pattern=[[1, 512]], compare_op=ALU.is_gt,
                                                    fill=0.0, base=-128 * j, channel_multiplier=-1), ins=[bconst], outs=[bconst])

    class Stream:
        pass

    streams = []
    for si in range(2):
        s = Stream()
        s.z = k.ps("%sz%d" % (tag, si), [128, 512]); s.bz = k.buf()
        s.p = k.ps("%sp%d" % (tag, si), [128, 512]); s.bp = k.buf()
        s.y = [k.ps("%sy%d_%d" % (tag, si, i), [128, 512]) for i in range(2)]; s.by = k.bufs(2)
        s.u = [k.sb("%su%d_%d" % (tag, si, i), [128, 512], F32) for i in range(2)]; s.bu = k.bufs(2)
        s.sp = [k.sb("%ssp%d_%d" % (tag, si, i), [128, 512], BF16) for i in range(2)]; s.bsp = k.bufs(2)
        s.ec = [k.sb("%sec%d_%d" % (tag, si, i), [128, 512], BF16) for i in range(2)]; s.bec = k.bufs(2)
        s.w = [k.sb("%sw%d_%d" % (tag, si, i), [128, 512], BF16) for i in range(2)]; s.bw = k.bufs(2)
        s.yo = [k.sb("%syo%d_%d" % (tag, si, i), [128, 512], BF16) for i in range(2)]; s.byo = k.bufs(2)
        s.steps = []
        for qt in range(si, NQ, 2):
            nb = 4 * qt + 4
            for i, b in enumerate(range(nb - 1, -1, -1)):
                s.steps.append((qt, b, i == 0, i == nb - 1))
        streams.append(s)

    def st_z(s, n):
        qt, b, first, last = s.steps[n]
        k.op("pe", lambda g: g.matmul(s.z[:], lhsT=kT[:, b * 128:(b + 1) * 128], rhs=qT[:, qt * 512:(qt + 1) * 512],
                                      start=True, stop=True), ins=[bk, bq], outs=[s.bz])

    def st_u(s, n):
        qt, b, first, last = s.steps[n]
        i = n % 2
        k.op("act", lambda g: g.activation(out=s.u[i][:], in_=s.z[:], func=AF.Exp, scale=SC), ins=[s.bz], outs=[s.bu[i]])
        j = b - 4 * qt
        if j >= 0:
            k.op("dve", lambda g: g.tensor_tensor(out=s.u[i][:], in0=s.u[i][:], in1=masks[:, j, :], op=ALU.mult),
                 ins=[s.bu[i], bconst], outs=[s.bu[i]])

    def st_sp(s, n):
        i = n % 2
        k.op("act", lambda g: g.activation(out=s.sp[i][:], in_=s.u[i][:], func=AF.Ln, bias=1.0), ins=[s.bu[i]], outs=[s.bsp[i]])

    def st_tri(s, n):
        qt, b, first, last = s.steps[n]
        i = n % 2
        k.op("pe", lambda g: g.matmul(s.p[:], lhsT=tri[:], rhs=s.sp[i][:], start=first, stop=True, skip_group_check=True),
             ins=[bconst, s.bsp[i]], outs=[s.bp])

    def st_ec(s, n):
        i = n % 2
        k.op("act", lambda g: g.activation(out=s.ec[i][:], in_=s.p[:], func=AF.Exp, scale=-1.0), ins=[s.bp], outs=[s.bec[i]])

    def st_omt(s, n):
        qt, b, first, last = s.steps[n]
        i = n % 2
        if not last:
            k.op("pe", lambda g: g.matmul(s.p[:], lhsT=omt[:], rhs=s.sp[i][:], start=False, stop=True, skip_group_check=True),
                 ins=[bconst, s.bsp[i]], outs=[s.bp])

    def st_w(s, n):
        i = n % 2
        k.op("dve", lambda g: g.tensor_tensor(out=s.w[i][:], in0=s.u[i][:], in1=s.ec[i][:], op=ALU.mult),
             ins=[s.bu[i], s.bec[i]], outs=[s.bw[i]])

    def st_wv(s, n):
        qt, b, first, last = s.steps[n]
        i = n % 2
        yi = (qt // 2) % 2
        k.op("pe", lambda g: g.matmul(s.y[yi][:], lhsT=vS[:, b, :], rhs=s.w[i][:], start=first, stop=last),
             ins=[bv, s.bw[i]], outs=[s.by[yi]])
        if last:
            k.op("act", lambda g: g.activation(out=s.yo[yi][:], in_=s.y[yi][:], func=AF.Identity), ins=[s.by[yi]], outs=[s.byo[yi]])
            k.dma(y_dram[:, qt * 512:(qt + 1) * 512], s.yo[yi][:], ins=[s.byo[yi]], outs=[by], q="pool")

    nmax = max(len(s.steps) for s in streams)
    for s in streams:
        st_z(s, 0)
    for n in range(nmax):
        act = [s for s in streams if n < len(s.steps)]
        for s in act:
            st_u(s, n)
        for s in act:
            st_sp(s, n)
        for s in act:
            st_tri(s, n)
        for s in act:
            if n + 1 < len(s.steps):
                st_z(s, n + 1)
        for s in act:
            st_ec(s, n)
        for s in act:
            st_omt(s, n)
        for s in act:
            st_w(s, n)
        for s in act:
            st_wv(s, n)


def build_SB():
    nc = new_nc()
    qTd = nc.dram_tensor("qT", [128, T], BF16, kind="ExternalInput").ap()
    kTd = nc.dram_tensor("kT", [128, T], BF16, kind="ExternalInput").ap()
    vd = nc.dram_tensor("v", [T, 128], BF16, kind="ExternalInput").ap()
    yd = nc.dram_tensor("ysb", [128, T], BF16, kind="ExternalOutput").ap()
    with contextlib.ExitStack() as st:
        k = K(nc, st)
        qT = k.sb("qTs", [128, T], BF16); bq = k.buf()
        kT = k.sb("kTs", [128, T], BF16); bk = k.buf()
        vS = k.sb("vS", [128, T // 128, 128], BF16); bv = k.buf()
        by = k.buf()
        for i in range(4):
            sl = slice(i * 4096, (i + 1) * 4096)
            k.dma(qT[:, sl], qTd[:, sl], outs=[bq]); k.dma(kT[:, sl], kTd[:, sl], outs=[bk])
            k.dma(vS[:, i * 32:(i + 1) * 32, :], vd.rearrange("(b p) d -> p b d", p=128)[:, i * 32:(i + 1) * 32, :], outs=[bv])
        emit_sb(k, qT, bq, kT, bk, vS, bv, yd, by)
        k.finish([by])
    return nc


def emit_ml(k, mqk_d, mv_d, mo_d, gif_d, cw_d, gb_d, y_dram, by, tag="ml"):
    TB = 2048
    NBLK = T // TB
    CPB = TB // 128
    LN16 = float(np.log(1.0 / 16.0))
    identf = k.sb(tag + "identf", [128, 128], F32); onesf = k.sb(tag + "onesf", [128, 128], F32)
    trile = k.sb(tag + "trile", [128, 128], F32); negmask = k.sb(tag + "negmask", [128, 128], F32)
    onesbf = k.sb(tag + "onesbf", [128, 128], BF16); identbf = k.sb(tag + "identbf", [128, 128], BF16)
    zerosf = k.sb(tag + "zerosf", [128, 128], F32)
    bc = k.buf()
    k.op("pool", lambda g: g.memset(onesf[:], 1.0), outs=[bc])
    k.op("pool", lambda g: g.memset(zerosf[:], 0.0), ins=[bc], outs=[bc])
    k.op("pool", lambda g: g.memset(onesbf[:], 1.0), ins=[bc], outs=[bc])
    k.op("pool", lambda g: g.affine_select(out=identf[:], in_=onesf[:], pattern=[[-1, 128]], compare_op=ALU.is_equal,
                                           fill=0.0, base=0, channel_multiplier=1), ins=[bc], outs=[bc])
    k.op("pool", lambda g: g.tensor_copy(out=identbf[:], in_=identf[:]), ins=[bc], outs=[bc])
    k.op("pool", lambda g: g.affine_select(out=trile[:], in_=onesf[:], pattern=[[1, 128]], compare_op=ALU.is_ge,
                                           fill=0.0, base=0, channel_multiplier=-1), ins=[bc], outs=[bc])
    k.op("pool", lambda g: g.affine_select(out=negmask[:], in_=zerosf[:], pattern=[[1, 128]], compare_op=ALU.is_ge,
                                           fill=-30000.0, base=0, channel_multiplier=-1), ins=[bc], outs=[bc])
    gif = k.sb(tag + "gif", [128, 2, 128], F32); bgif = k.buf()
    cw = k.sb(tag + "cw", [128, 4, 4], F32); bcw = k.buf()
    gb = k.sb(tag + "gb", [128, 2], F32); bgb = k.buf()
    k.dma(gif[:], gif_d, outs=[bgif]); k.dma(cw[:], cw_d, outs=[bcw]); k.dma(gb[:], gb_d, outs=[bgb])
    psA = k.ps(tag + "psA", [128, 512]); bpsA = k.buf()
    psB = k.ps(tag + "psB", [128, 512]); bpsB = k.buf()
    Eps = k.ps(tag + "Eps", [128, 512]); bEps = k.buf()
    Sps = k.ps(tag + "Sps", [128, 512]); bSps = k.buf()
    NDps = k.ps(tag + "NDps", [128, 512]); bND = k.buf()
    ktps = k.ps(tag + "ktps", [128, 1024], BF16); bktps = k.buf()
    dCps = k.ps(tag + "dCps", [128, 2, 256]); bdC = k.buf()
    sc = k.sb(tag + "sc", [128, 4], F32); bsc = k.buf()
    k.op("dve", lambda g: g.tensor_scalar(out=sc[:, 0:1], in0=gb[:, 1:2], scalar1=-1.0, scalar2=None, op0=ALU.mult), ins=[bgb], outs=[bsc])
    k.op("dve", lambda g: g.tensor_scalar(out=sc[:, 1:2], in0=gb[:, 0:1], scalar1=LN16, scalar2=None, op0=ALU.add), ins=[bgb, bsc], outs=[bsc])
    lfneg = k.sb(tag + "lfneg", [128, 128], F32); blf = k.buf()
    k.op("act", lambda g: g.activation(out=lfneg[:], in_=gif[:, 1, :], func=AF.Exp, scale=-1.0, bias=sc[:, 0:1]), ins=[bgif, bsc], outs=[blf])
    k.op("act", lambda g: g.activation(out=lfneg[:], in_=lfneg[:], func=AF.Ln, bias=1.0), ins=[blf], outs=[blf])
    k.op("pe", lambda g: g.matmul(psA[:, 0:128], lhsT=trile[:], rhs=lfneg[:], start=True, stop=True), ins=[bc, blf], outs=[bpsA])
    k.op("pe", lambda g: g.matmul(psB[:, 0:128], lhsT=onesf[:], rhs=lfneg[:], start=True, stop=True), ins=[bc, blf], outs=[bpsB])
    imb = k.sb(tag + "imb", [128, 128], F32); negb = k.sb(tag + "negb", [128, 128], F32)
    wa = k.sb(tag + "wa", [128, 128], F32); ebtot = k.sb(tag + "ebtot", [128, 128], F32); bg = k.buf()
    k.op("dve", lambda g: g.scalar_tensor_tensor(out=imb[:], in0=gif[:, 0, :], scalar=sc[:, 1:2], in1=psA[:, 0:128], op0=ALU.add, op1=ALU.add),
         ins=[bgif, bsc, bpsA], outs=[bg])
    k.op("dve", lambda g: g.tensor_scalar(out=negb[:], in0=psA[:, 0:128], scalar1=-1.0, scalar2=None, op0=ALU.mult), ins=[bpsA, bg], outs=[bg])
    k.op("dve", lambda g: g.tensor_tensor(out=wa[:], in0=imb[:], in1=psB[:, 0:128], op=ALU.subtract), ins=[bg, bpsB], outs=[bg])
    k.op("act", lambda g: g.activation(out=wa[:], in_=wa[:], func=AF.Exp), ins=[bg], outs=[bg])
    k.op("act", lambda g: g.activation(out=ebtot[:], in_=psB[:, 0:128], func=AF.Exp, scale=-1.0), ins=[bpsB, bg], outs=[bg])
    Cst = k.sb(tag + "Cst", [128, 2, 129], F32); bCst = k.buf()
    C0bf = k.sb(tag + "C0bf", [128, 2, 128], BF16); n0bc = k.sb(tag + "n0bc", [128, 2, 128], BF16); bC0 = k.buf()
    k.op("pool", lambda g: g.memset(Cst[:], 0.0), outs=[bCst])
    k.op("pool", lambda g: g.memset(C0bf[:], 0.0), outs=[bC0])
    k.op("pool", lambda g: g.memset(n0bc[:], 0.0), ins=[bC0], outs=[bC0])
    xin = [k.sb("%sxin%d" % (tag, i), [128, 4, TB + 4], BF16) for i in range(2)]; bxin = k.bufs(2)
    acc = k.sb(tag + "acc", [128, TB], F32); bacc = k.buf()
    qk = [k.sb("%sqk%d" % (tag, i), [128, 4, TB], BF16) for i in range(2)]; bqk = k.bufs(2)
    vaug = [k.sb("%svaug%d" % (tag, i), [128, CPB, 129], BF16) for i in range(2)]; bva = k.bufs(2)
    mo = [k.sb("%smo%d" % (tag, i), [128, TB], BF16) for i in range(2)]; bmo = k.bufs(2)
    sigo = [k.sb("%ssigo%d" % (tag, i), [128, TB], F32) for i in range(2)]; bsigo = k.bufs(2)
    yb = [k.sb("%syb%d" % (tag, i), [128, TB], BF16) for i in range(2)]; byb = k.bufs(2)
    diagb = [k.sb("%sdiagb%d" % (tag, i), [128, 128], F32) for i in range(2)]; bdiag = k.bufs(2)
    Dt = k.sb(tag + "Dt", [128, 128], F32); bDt = k.buf()
    ebb = k.sb(tag + "ebb", [128, 128], F32); bebb = k.buf()
    Pt = k.sb(tag + "Pt", [128, 128], BF16); bPt = k.buf()
    qtl = k.sb(tag + "qtl", [128, 2, 128], BF16); bqtl = k.buf()
    kt = k.sb(tag + "kt", [128, 256], BF16); bkt = k.buf()
    dn = k.sb(tag + "dn", [128, 128], F32); bdn = k.buf()
    hh = k.sb(tag + "hh", [128, 128], F32); bhh = k.buf()
    for i in range(2):
        k.op("pool", lambda g, i=i: g.memset(vaug[i][:], 1.0), outs=[bva[i]])
    mqk_v = mqk_d.rearrange("(x p) t -> p x t", p=128)
    mv_v = mv_d.rearrange("(c l) d -> l c d", l=128)
    for blk in range(NBLK):
        s = blk % 2
        t0 = blk * TB
        if blk == 0:
            k.op("pool", lambda g: g.memset(xin[s][:, :, 0:4], 0.0), outs=[bxin[s]])
            k.dma(xin[s][:, :, 4:4 + TB], mqk_v[:, :, 0:TB], outs=[bxin[s]])
        else:
            k.dma(xin[s][:, :, 0:4 + TB], mqk_v[:, :, t0 - 4:t0 + TB], outs=[bxin[s]])
        k.dma(vaug[s][:, :, 0:128], mv_v[:, blk * CPB:(blk + 1) * CPB, :], outs=[bva[s]])
        k.dma(mo[s][:], mo_d[:, t0:t0 + TB], outs=[bmo[s]])
        k.op("act", lambda g: g.activation(out=sigo[s][:], in_=mo[s][:], func=AF.Sigmoid), ins=[bmo[s]], outs=[bsigo[s]])
        for X in range(4):
            k.op("dve", lambda g: g.tensor_scalar(out=acc[:], in0=xin[s][:, X, 4:4 + TB], scalar1=cw[:, X, 3:4], scalar2=None, op0=ALU.mult),
                 ins=[bxin[s], bcw], outs=[bacc])
            for j in (2, 1, 0):
                k.op("dve", lambda g, j=j: g.scalar_tensor_tensor(out=acc[:], in0=xin[s][:, X, 1 + j:1 + j + TB], scalar=cw[:, X, j:j + 1],
                                                                  in1=acc[:], op0=ALU.mult, op1=ALU.add), ins=[bxin[s], bcw, bacc], outs=[bacc])
            k.op("act", lambda g: g.activation(out=qk[s][:, X, :], in_=acc[:], func=AF.Silu), ins=[bacc], outs=[bqk[s]])
        for ci in range(CPB):
            cg = blk * CPB + ci
            csl = slice(ci * 128, (ci + 1) * 128)
            d = cg % 2
            k.op("dve", lambda g: g.tensor_scalar(out=diagb[d][:], in0=identf[:], scalar1=negb[:, cg:cg + 1], scalar2=None, op0=ALU.mult),
                 ins=[bc, bg], outs=[bdiag[d]])
            k.op("pe", lambda g: g.matmul(Eps[:, 0:128], lhsT=onesf[:], rhs=diagb[d][:], start=True, stop=False), ins=[bc, bdiag[d]], outs=[bEps])
            k.op("pe", lambda g: g.matmul(Eps[:, 0:128], lhsT=identf[:], rhs=negmask[:], start=False, stop=True), ins=[bc], outs=[bEps])
            k.op("pe", lambda g: g.matmul(Eps[:, 128:256], lhsT=onesf[:], rhs=diagb[d][:], start=True, stop=True), ins=[bc, bdiag[d]], outs=[bEps])
            k.op("act", lambda g: g.activation(out=Dt[:], in_=Eps[:, 0:128], func=AF.Exp, bias=imb[:, cg:cg + 1]), ins=[bEps, bg], outs=[bDt])
            k.op("act", lambda g: g.activation(out=ebb[:], in_=Eps[:, 128:256], func=AF.Exp), ins=[bEps], outs=[bebb])
            for dk in range(2):
                k.op("pe", lambda g, dk=dk: g.matmul(Sps[:, 0:128], lhsT=qk[s][:, 2 + dk, csl], rhs=qk[s][:, dk, csl], start=(dk == 0), stop=(dk == 1)),
                     ins=[bqk[s]], outs=[bSps])
            k.op("dve", lambda g: g.tensor_tensor(out=Pt[:], in0=Sps[:, 0:128], in1=Dt[:], op=ALU.mult), ins=[bSps, bDt], outs=[bPt])
            for dk in range(2):
                k.op("dve", lambda g, dk=dk: g.tensor_tensor(out=qtl[:, dk, :], in0=qk[s][:, dk, csl], in1=ebb[:], op=ALU.mult),
                     ins=[bqk[s], bebb], outs=[bqtl])
            k.op("pe", lambda g: g.matmul(NDps[:, 0:128], lhsT=vaug[s][:, ci, 0:128], rhs=Pt[:], start=True, stop=False), ins=[bva[s], bPt], outs=[bND])
            for dk in range(2):
                k.op("pe", lambda g, dk=dk: g.matmul(NDps[:, 0:128], lhsT=C0bf[:, dk, :], rhs=qtl[:, dk, :], start=False, stop=(dk == 1)),
                     ins=[bC0, bqtl], outs=[bND])
            k.op("pe", lambda g: g.matmul(NDps[:, 128:256], lhsT=onesbf[:], rhs=Pt[:], start=True, stop=False), ins=[bc, bPt], outs=[bND])
            for dk in range(2):
                k.op("pe", lambda g, dk=dk: g.matmul(NDps[:, 128:256], lhsT=n0bc[:, dk, :], rhs=qtl[:, dk, :], start=False, stop=(dk == 1)),
                     ins=[bC0, bqtl], outs=[bND])
            k.op("act", lambda g: g.activation(out=dn[:], in_=NDps[:, 128:256], func=AF.Abs), ins=[bND], outs=[bdn])
            k.op("dve", lambda g: g.tensor_scalar(out=dn[:], in0=dn[:], scalar1=1.0, scalar2=None, op0=ALU.max), ins=[bdn], outs=[bdn])
            k.op("dve", lambda g: g.reciprocal(out=dn[:], in_=dn[:]), ins=[bdn], outs=[bdn])
            k.op("dve", lambda g: g.tensor_tensor(out=hh[:], in0=NDps[:, 0:128], in1=dn[:], op=ALU.mult), ins=[bND, bdn], outs=[bhh])
            k.op("dve", lambda g: g.tensor_tensor(out=yb[s][:, csl], in0=hh[:], in1=sigo[s][:, csl], op=ALU.mult), ins=[bhh, bsigo[s]], outs=[byb[s]])
            for dk in range(2):
                k.op("pe", lambda g, dk=dk: g.transpose(ktps[:, dk * 128:(dk + 1) * 128], qk[s][:, 2 + dk, csl], identbf[:]),
                     ins=[bqk[s], bc], outs=[bktps])
            k.op("dve", lambda g: g.tensor_scalar(out=kt[:], in0=ktps[:, 0:256], scalar1=wa[:, cg:cg + 1], scalar2=None, op0=ALU.mult),
                 ins=[bktps, bg], outs=[bkt])
            for dk in range(2):
                k.op("pe", lambda g, dk=dk: g.matmul(dCps[:, dk, 0:129], lhsT=kt[:, dk * 128:(dk + 1) * 128], rhs=vaug[s][:, ci, :], start=True, stop=True),
                     ins=[bkt, bva[s]], outs=[bdC])
            for dk in range(2):
                k.op("dve", lambda g, dk=dk: g.scalar_tensor_tensor(out=Cst[:, dk, :], in0=Cst[:, dk, :], scalar=ebtot[:, cg:cg + 1], in1=dCps[:, dk, 0:129],
                                                                    op0=ALU.mult, op1=ALU.add), ins=[bCst, bg, bdC], outs=[bCst])
            k.op("pool", lambda g: g.tensor_copy(out=C0bf[:], in_=Cst[:, :, 0:128]), ins=[bCst], outs=[bC0])
            for dk in range(2):
                k.op("dve", lambda g, dk=dk: g.tensor_scalar(out=n0bc[:, dk, :], in0=onesbf[:], scalar1=Cst[:, dk, 128:129], scalar2=None, op0=ALU.mult),
                     ins=[bc, bCst, bC0], outs=[bC0])
        k.dma(y_dram[:, t0:t0 + TB], yb[s][:], ins=[byb[s]], outs=[by], q="pool")


def build_ML():
    nc = new_nc()
    mqk_d = nc.dram_tensor("mqk", [512, T], BF16, kind="ExternalInput").ap()
    mv_d = nc.dram_tensor("mv", [T, 128], BF16, kind="ExternalInput").ap()
    mo_d = nc.dram_tensor("mo", [128, T], BF16, kind="ExternalInput").ap()
    gif_d = nc.dram_tensor("gif", [128, 2, 128], F32, kind="ExternalInput").ap()
    cw_d = nc.dram_tensor("cw", [128, 4, 4], F32, kind="ExternalInput").ap()
    gb_d = nc.dram_tensor("gb", [128, 2], F32, kind="ExternalInput").ap()
    yd = nc.dram_tensor("yml", [128, T], BF16, kind="ExternalOutput").ap()
    with contextlib.ExitStack() as st:
        k = K(nc, st)
        by = k.buf()
        emit_ml(k, mqk_d, mv_d, mo_d, gif_d, cw_d, gb_d, yd, by)
        k.finish([by])
    return nc


def ml_host_inputs(c, l, mq, mk, mv, mo, mi, mf, ml_conv, ml_i_bias, ml_f_bias):
    bf = ml_dtypes.bfloat16
    hd, vh = c // 2, c % 2
    q = mq[:, hd * 256:(hd + 1) * 256]; kk = mk[:, hd * 256:(hd + 1) * 256]
    mqk = np.concatenate([q.T, kk.T], axis=0).astype(bf)
    v = mv[:, hd * 256 + vh * 128: hd * 256 + (vh + 1) * 128].astype(bf)
    o = mo[:, hd * 256 + vh * 128: hd * 256 + (vh + 1) * 128].T.astype(bf)
    gi = mi[:, hd].reshape(128, 128).T; gf = mf[:, hd].reshape(128, 128).T
    gif = np.stack([gi, gf], axis=1).astype(np.float32)
    cwq = ml_conv[:, hd * 256:(hd + 1) * 256]; cwk = ml_conv[:, 1024 + hd * 256:1024 + (hd + 1) * 256]
    cw = np.stack([cwq[:, 0:128].T, cwq[:, 128:256].T, cwk[:, 0:128].T, cwk[:, 128:256].T], axis=1).astype(np.float32)
    gb = np.tile(np.array([[ml_i_bias[hd], ml_f_bias[hd]]], np.float32), (128, 1))
    return {"mqk": np.ascontiguousarray(mqk), "mv": np.ascontiguousarray(v), "mo": np.ascontiguousarray(o),
            "gif": np.ascontiguousarray(gif), "cw": np.ascontiguousarray(cw), "gb": gb}


def emit_s5(k, u16_d, lamst_d, bst_d, cst_d, dsk_d, y_dram, by, tag="s5"):
    L = 16
    NBLK = 1024
    NB = T // NBLK
    CPB = NBLK // L
    NC = T // L
    NLEV = 10
    PI = float(np.pi)
    identf = k.sb(tag + "identf", [128, 128], F32); onesf = k.sb(tag + "onesf", [128, 128], F32)
    swap = k.sb(tag + "swap", [128, 128], F32); sw2 = k.sb(tag + "sw2", [128, 128], F32)
    sgn = k.sb(tag + "sgn", [128, 1], F32)
    bc = k.buf()
    k.op("pool", lambda g: g.memset(onesf[:], 1.0), outs=[bc])
    k.op("pool", lambda g: g.affine_select(out=identf[:], in_=onesf[:], pattern=[[-1, 128]], compare_op=ALU.is_equal,
                                           fill=0.0, base=0, channel_multiplier=1), ins=[bc], outs=[bc])
    k.op("pool", lambda g: g.affine_select(out=swap[:], in_=onesf[:], pattern=[[-1, 128]], compare_op=ALU.is_equal,
                                           fill=0.0, base=64, channel_multiplier=1), ins=[bc], outs=[bc])
    k.op("pool", lambda g: g.affine_select(out=sw2[:], in_=onesf[:], pattern=[[-1, 128]], compare_op=ALU.is_equal,
                                           fill=0.0, base=-64, channel_multiplier=1), ins=[bc], outs=[bc])
    k.op("pool", lambda g: g.tensor_tensor(out=swap[:], in0=swap[:], in1=sw2[:], op=ALU.add), ins=[bc], outs=[bc])
    k.op("pool", lambda g: g.memset(sgn[0:64, :], 1.0), ins=[bc], outs=[bc])
    k.op("pool", lambda g: g.memset(sgn[64:128, :], -1.0), ins=[bc], outs=[bc])
    pat01 = k.sb(tag + "pat01", [128, NBLK // L, L], F32)
    k.op("pool", lambda g: g.memset(pat01[:], 1.0), ins=[bc], outs=[bc])
    k.op("pool", lambda g: g.memset(pat01[:, :, 0:1], 0.0), ins=[bc], outs=[bc])
    lamst = k.sb(tag + "lamst", [128, 3, 8], F32); bst = k.sb(tag + "bst", [128, 8, 16], F32)
    cst = k.sb(tag + "cst", [128, 8, 16], F32); dsk = k.sb(tag + "dsk", [16, 8], F32)
    bpar = k.buf()
    k.dma(lamst[:], lamst_d, outs=[bpar]); k.dma(bst[:], bst_d, outs=[bpar]); k.dma(cst[:], cst_d, outs=[bpar]); k.dma(dsk[:], dsk_d, outs=[bpar])
    k.op("dve", lambda g: g.tensor_scalar(out=cst[64:128], in0=cst[64:128], scalar1=-1.0, scalar2=None, op0=ALU.mult), ins=[bpar], outs=[bpar])
    NTAB = 24
    tab = k.sb(tag + "tab", [128, NTAB, 8], F32); bt = k.buf()
    tmp = k.sb(tag + "tmpt", [128, 8, 8], F32)
    (I_DT, I_ER, I_ANG, I_SIN, I_COS, I_LR, I_LI, I_KR, I_KI, I_T0, I_T1, I_T2, I_T3) = range(13)
    tv = lambda i: tab[:, i, :]
    dv = lambda fn, **kw: k.op("dve", fn, ins=[bt, bpar, bc], outs=[bt])
    av = lambda fn: k.op("act", fn, ins=[bt, bpar], outs=[bt])
    TT = lambda o, a, b, op: dv(lambda g: g.tensor_tensor(out=o, in0=a, in1=b, op=op))
    TS = lambda o, a, s1, op0, s2=None, op1=None: dv(lambda g: g.tensor_scalar(out=o, in0=a, scalar1=s1, scalar2=s2, op0=op0, **({"op1": op1} if op1 is not None else {})))

    def cmul(o_r, o_i, ar, ai, br, bi, t1, t2):
        TT(t1, ar, br, ALU.mult); TT(t2, ai, bi, ALU.mult); TT(o_r, t1, t2, ALU.subtract)
        TT(t1, ar, bi, ALU.mult); TT(t2, ai, br, ALU.mult); TT(o_i, t1, t2, ALU.add)

    av(lambda g: g.activation(out=tv(I_DT), in_=lamst[:, 2, :], func=AF.Exp))
    TT(tv(I_T0), lamst[:, 0, :], tv(I_DT), ALU.mult)
    av(lambda g: g.activation(out=tv(I_ER), in_=tv(I_T0), func=AF.Exp))
    TT(tv(I_ANG), lamst[:, 1, :], tv(I_DT), ALU.mult)

    def rr(dst, src, shift):
        TS(dst, src, shift, ALU.add)
        for _ in range(8):
            TS(tv(I_T1), dst, PI, ALU.is_gt)
            dv(lambda g: g.scalar_tensor_tensor(out=dst, in0=tv(I_T1), scalar=-2.0 * PI, in1=dst, op0=ALU.mult, op1=ALU.add))
        TS(dst, dst, -PI, ALU.max)
        TS(dst, dst, PI, ALU.min)

    rr(tv(I_T2), tv(I_ANG), 0.0)
    av(lambda g: g.activation(out=tv(I_SIN), in_=tv(I_T2), func=AF.Sin))
    rr(tv(I_T2), tv(I_ANG), PI / 2)
    av(lambda g: g.activation(out=tv(I_COS), in_=tv(I_T2), func=AF.Sin))
    TT(tv(I_LR), tv(I_ER), tv(I_COS), ALU.mult)
    TT(tv(I_LI), tv(I_ER), tv(I_SIN), ALU.mult)
    TS(tv(I_T0), tv(I_LR), -1.0, ALU.add)
    TT(tv(I_T1), lamst[:, 0, :], lamst[:, 0, :], ALU.mult)
    TT(tv(I_T2), lamst[:, 1, :], lamst[:, 1, :], ALU.mult)
    TT(tv(I_T1), tv(I_T1), tv(I_T2), ALU.add)
    dv(lambda g: g.reciprocal(out=tv(I_T1), in_=tv(I_T1)))
    TT(tv(I_T2), tv(I_T0), lamst[:, 0, :], ALU.mult)
    TT(tv(I_T3), tv(I_LI), lamst[:, 1, :], ALU.mult)
    TT(tv(I_T2), tv(I_T2), tv(I_T3), ALU.add)
    TT(tv(I_KR), tv(I_T2), tv(I_T1), ALU.mult)
    TT(tv(I_T2), tv(I_LI), lamst[:, 0, :], ALU.mult)
    TT(tv(I_T3), tv(I_T0), lamst[:, 1, :], ALU.mult)
    TT(tv(I_T2), tv(I_T2), tv(I_T3), ALU.subtract)
    TT(tv(I_KI), tv(I_T2), tv(I_T1), ALU.mult)
    ct = k.sb(tag + "ct", [128, L, 8], F32); stt = k.sb(tag + "stt", [128, L, 8], F32)
    pwr = k.sb(tag + "pwr", [128, L, 8], F32); pwi = k.sb(tag + "pwi", [128, L, 8], F32)
    rhr = k.sb(tag + "rhr", [128, L, 8], F32); rhi = k.sb(tag + "rhi", [128, L, 8], F32)
    mur = k.sb(tag + "mur", [128, NLEV, 8], F32); mui = k.sb(tag + "mui", [128, NLEV, 8], F32)
    nst = k.sb(tag + "nst", [128, L, 8], F32)
    dv(lambda g: g.memset(ct[:, 0, :], 1.0)); dv(lambda g: g.memset(stt[:, 0, :], 0.0))
    dv(lambda g: g.tensor_copy(out=pwr[:, 0, :], in_=tv(I_LR))); dv(lambda g: g.tensor_copy(out=pwi[:, 0, :], in_=tv(I_LI)))
    for t in range(1, L):
        cmul(ct[:, t, :], stt[:, t, :], ct[:, t - 1, :], stt[:, t - 1, :], tv(I_COS), tv(I_SIN), tmp[:, 0, :], tmp[:, 1, :])
        cmul(pwr[:, t, :], pwi[:, t, :], pwr[:, t - 1, :], pwi[:, t - 1, :], tv(I_LR), tv(I_LI), tmp[:, 0, :], tmp[:, 1, :])
    dv(lambda g: g.tensor_scalar(out=nst[:], in0=stt[:], scalar1=-1.0, scalar2=None, op0=ALU.mult))
    for t in range(L):
        cmul(rhr[:, t, :], rhi[:, t, :], ct[:, t, :], nst[:, t, :], tv(I_KR), tv(I_KI), tmp[:, 0, :], tmp[:, 1, :])
    dv(lambda g: g.tensor_copy(out=mur[:, 0, :], in_=pwr[:, L - 1, :])); dv(lambda g: g.tensor_copy(out=mui[:, 0, :], in_=pwi[:, L - 1, :]))
    for lv in range(1, NLEV):
        cmul(mur[:, lv, :], mui[:, lv, :], mur[:, lv - 1, :], mui[:, lv - 1, :], mur[:, lv - 1, :], mui[:, lv - 1, :], tmp[:, 0, :], tmp[:, 1, :])
    rhis = k.sb(tag + "rhis", [128, L, 8], F32); nsts = k.sb(tag + "nsts", [128, L, 8], F32)
    npwis = k.sb(tag + "npwis", [128, L, 8], F32); muis = k.sb(tag + "muis", [128, NLEV, 8], F32)
    sts = k.sb(tag + "sts", [128, 8], F32)
    TS(rhis[:], rhi[:], sgn[:, 0:1], ALU.mult)
    TS(nsts[:], nst[:], sgn[:, 0:1], ALU.mult)
    TS(npwis[:], pwi[:], sgn[:, 0:1], ALU.mult, -1.0, ALU.mult)
    TS(muis[:], mui[:], sgn[:, 0:1], ALU.mult)
    TS(sts[:], stt[:, L - 1, :], sgn[:, 0:1], ALU.mult)
    Bin = k.sb(tag + "Bin", [16, 8, L, 128], BF16); Cloc = k.sb(tag + "Cloc", [128, 8, L, 16], BF16)
    Ccor = k.sb(tag + "Ccor", [128, 8, L, 16], BF16)
    ErT = k.sb(tag + "ErT", [128, 8, 128], F32); MkT = k.sb(tag + "MkT", [128, 2, NLEV, 128], F32)
    bw = k.buf()
    bm = [k.sb("%sbm%d" % (tag, i), [128, 128], F32) for i in range(4)]; bbm = k.bufs(4)
    pps = [k.ps("%spps%d" % (tag, i), [128, 512]) for i in range(2)]; bpps = k.bufs(2)
    cnt = [0]

    def blockmat(a_ap, bs_ap):
        i = cnt[0] % 4
        cnt[0] += 1
        k.op("dve", lambda g: g.tensor_scalar(out=bm[i][:], in0=identf[:], scalar1=a_ap, scalar2=None, op0=ALU.mult), ins=[bc, bt], outs=[bbm[i]])
        k.op("dve", lambda g: g.scalar_tensor_tensor(out=bm[i][:], in0=swap[:], scalar=bs_ap, in1=bm[i][:], op0=ALU.mult, op1=ALU.add),
             ins=[bc, bt, bbm[i]], outs=[bbm[i]])
        return bm[i], bbm[i]

    ev = [0]

    def evac(out_ap, in_ap, bin_, bout):
        ev[0] += 1
        _evac(k, ev[0], out_ap, in_ap, [bin_], [bout])

    def group_weights(g_):
        gs = slice(g_, g_ + 1)
        for t in range(L):
            m, bm_ = blockmat(rhr[:, t, gs], rhis[:, t, gs])
            p = cnt[0] % 2
            k.op("pe", lambda g, m=m, p=p: g.matmul(pps[p][0:16, 0:128], lhsT=bst[:, g_, :], rhs=m[:], start=True, stop=True), ins=[bpar, bm_], outs=[bpps[p]])
            evac(Bin[:, g_, t, :], pps[p][0:16, 0:128], bpps[p], bw)
            m, bm_ = blockmat(ct[:, t, gs], nsts[:, t, gs])
            p = cnt[0] % 2
            k.op("pe", lambda g, m=m, p=p: g.matmul(pps[p][:, 0:16], lhsT=m[:], rhs=cst[:, g_, :], start=True, stop=True), ins=[bpar, bm_], outs=[bpps[p]])
            evac(Cloc[:, g_, t, :], pps[p][:, 0:16], bpps[p], bw)
            m, bm_ = blockmat(pwr[:, t, gs], npwis[:, t, gs])
            p = cnt[0] % 2
            k.op("pe", lambda g, m=m, p=p: g.matmul(pps[p][:, 0:16], lhsT=m[:], rhs=cst[:, g_, :], start=True, stop=True), ins=[bpar, bm_], outs=[bpps[p]])
            evac(Ccor[:, g_, t, :], pps[p][:, 0:16], bpps[p], bw)
    group_weights(0)
    def blockmat_into(out_ap, a_ap, bs_ap):
        k.op("dve", lambda g: g.tensor_scalar(out=out_ap, in0=identf[:], scalar1=a_ap, scalar2=None, op0=ALU.mult), ins=[bc, bt, bw], outs=[bw])
        k.op("dve", lambda g: g.scalar_tensor_tensor(out=out_ap, in0=swap[:], scalar=bs_ap, in1=out_ap, op0=ALU.mult, op1=ALU.add),
             ins=[bc, bt, bw], outs=[bw])
    for g_ in range(8):
        gs = slice(g_, g_ + 1)
        blockmat_into(ErT[:, g_, :], ct[:, L - 1, gs], sts[:, gs])
    ug = [k.sb("%sug%d" % (tag, i), [16, T], BF16) for i in range(1)] * 2; bug = [k.buf()] * 2
    zbf = k.sb(tag + "zbf", [128, T], BF16); bzbf = k.buf()
    Ag = k.sb(tag + "Ag", [128, NBLK], F32); bAg = k.buf()
    zin = [k.sb("%szin%d" % (tag, i), [128, NBLK], F32) for i in range(2)]; bzin = k.bufs(2)
    zf = [k.sb("%szf%d" % (tag, i), [128, NBLK], F32) for i in range(2)]; bzf = k.bufs(2)
    zps = [k.ps("%szps%d" % (tag, i), [128, L, CPB]) for i in range(2)]; bzps = k.bufs(2)
    yps = k.ps(tag + "yps", [128, L, CPB]); byps = k.buf()
    Ssc = [k.sb("%sSsc%d" % (tag, i), [128, NC], F32) for i in range(2)]; bS = k.bufs(2)
    Sbf = k.sb(tag + "Sbf", [128, NC + 1], BF16); bSbf = k.buf()
    y1 = [k.sb("%sy1_%d" % (tag, i), [16, NBLK], F32) for i in range(2)]; by1 = k.bufs(2)
    y2 = [k.sb("%sy2_%d" % (tag, i), [16, NBLK], F32) for i in range(2)]; by2 = k.bufs(2)
    yo = [k.sb("%syo_%d" % (tag, i), [16, NBLK], BF16) for i in range(2)]; byo = k.bufs(2)
    k.op("pool", lambda g: g.memset(Sbf[:, 0:1], 0.0), outs=[bSbf])
    for g_ in range(8):
        gs = slice(g_, g_ + 1)
        u_ = ug[g_ % 2]; bu_ = bug[g_ % 2]
        for i in range(4):
            k.dma(u_[:, i * 4096:(i + 1) * 4096], u16_d[:, g_, i * 4096:(i + 1) * 4096], outs=[bu_])
        k.op("dve", lambda g: g.tensor_scalar(out=Ag[:], in0=pat01[:].rearrange("p c l -> p (c l)"), scalar1=tab[:, I_ER, gs], scalar2=None, op0=ALU.mult),
             ins=[bc, bt], outs=[bAg])
        for lv in range(NLEV):
            blockmat_into(MkT[:, g_ % 2, lv, :], mur[:, lv, gs], muis[:, lv, gs])
        uv = u_[:].rearrange("p (c l) -> p c l", l=L)
        zbv = zbf[:].rearrange("p (c l) -> p c l", l=L)
        for blk in range(NB):
            s = blk % 2
            c0 = blk * CPB
            for t in range(L):
                k.op("pe", lambda g, t=t: g.matmul(zps[s][:, t, :], lhsT=Bin[:, g_, t, :], rhs=uv[:, c0:c0 + CPB, t], start=True, stop=True),
                     ins=[bw, bu_], outs=[bzps[s]])
            evac(zin[s][:].rearrange("p (c l) -> p l c", l=L), zps[s][:], bzps[s], bzin[s])
            k.op("dve", lambda g: g.tensor_tensor_scan(out=zf[s][:], data0=Ag[:], data1=zin[s][:], initial=0.0, op0=ALU.mult, op1=ALU.add),
                 ins=[bAg, bzin[s]], outs=[bzf[s]])
            k.op("pool", lambda g: g.tensor_copy(out=zbf[:, blk * NBLK:(blk + 1) * NBLK], in_=zf[s][:]), ins=[bzf[s]], outs=[bzbf])
            p = blk % 2
            k.op("pe", lambda g, p=p: g.matmul(pps[p][:, 0:CPB], lhsT=ErT[:, g_, :], rhs=zf[s][:].rearrange("p (c l) -> p c l", l=L)[:, :, L - 1],
                                               start=True, stop=True), ins=[bw, bzf[s]], outs=[bpps[p]])
            k.op("dve", lambda g, p=p: g.tensor_copy(out=Ssc[0][:, c0:c0 + CPB], in_=pps[p][:, 0:CPB]), ins=[bpps[p]], outs=[bS[0]])
        if g_ + 1 < 8:
            group_weights(g_ + 1)
        for lv in range(NLEV):
            d = 1 << lv
            src, dst = Ssc[lv % 2], Ssc[(lv + 1) % 2]
            bsrc, bdst = bS[lv % 2], bS[(lv + 1) % 2]
            n = NC - d
            k.op("pool", lambda g: g.tensor_copy(out=dst[:, 0:d], in_=src[:, 0:d]), ins=[bsrc], outs=[bdst])
            for j0 in range(0, n, 512):
                nn = min(512, n - j0)
                p = (j0 // 512) % 2
                k.op("pe", lambda g, p=p: g.matmul(pps[p][:, 0:nn], lhsT=MkT[:, g_ % 2, lv, :], rhs=src[:, j0:j0 + nn], start=True, stop=True),
                     ins=[bw, bsrc], outs=[bpps[p]])
                k.op("dve", lambda g, p=p: g.tensor_tensor(out=dst[:, d + j0:d + j0 + nn], in0=src[:, d + j0:d + j0 + nn], in1=pps[p][:, 0:nn], op=ALU.add),
                     ins=[bsrc, bpps[p]], outs=[bdst])
        fin = Ssc[NLEV % 2]; bfin = bS[NLEV % 2]
        k.op("act", lambda g: g.activation(out=Sbf[:, 1:NC + 1], in_=fin[:], func=AF.Identity), ins=[bfin], outs=[bSbf])
        for blk in range(NB):
            s = blk % 2
            c0 = blk * CPB
            t0 = blk * NBLK
            for t in range(L):
                k.op("pe", lambda g, t=t: g.matmul(yps[0:16, t, :], lhsT=Cloc[:, g_, t, :], rhs=zbv[:, c0:c0 + CPB, t], start=True, stop=False),
                     ins=[bw, bzbf], outs=[byps])
                k.op("pe", lambda g, t=t: g.matmul(yps[0:16, t, :], lhsT=Ccor[:, g_, t, :], rhs=Sbf[:, c0:c0 + CPB], start=False, stop=True),
                     ins=[bw, bSbf], outs=[byps])
            evac(y1[s][:].rearrange("p (c l) -> p l c", l=L), yps[0:16], byps, by1[s])
            k.op("dve", lambda g: g.scalar_tensor_tensor(out=y1[s][:], in0=u_[:, t0:t0 + NBLK], scalar=dsk[:, gs], in1=y1[s][:], op0=ALU.mult, op1=ALU.add),
                 ins=[bu_, bpar, by1[s]], outs=[by1[s]])
            k.op("act", lambda g: g.activation(out=y2[s][:], in_=y1[s][:], func=AF.Square), ins=[by1[s]], outs=[by2[s]])
            k.op("dve", lambda g: g.tensor_scalar(out=y2[s][:], in0=y2[s][:], scalar1=0.044715, scalar2=1.0, op0=ALU.mult, op1=ALU.add), ins=[by2[s]], outs=[by2[s]])
            k.op("dve", lambda g: g.tensor_tensor(out=y2[s][:], in0=y2[s][:], in1=y1[s][:], op=ALU.mult), ins=[by2[s], by1[s]], outs=[by2[s]])
            k.op("act", lambda g: g.activation(out=y2[s][:], in_=y2[s][:], func=AF.Sigmoid, scale=1.5957691216057308), ins=[by2[s]], outs=[by2[s]])
            k.op("dve", lambda g: g.tensor_tensor(out=yo[s][:], in0=y2[s][:], in1=y1[s][:], op=ALU.mult), ins=[by2[s], by1[s]], outs=[byo[s]])
            k.dma(y_dram[:, g_, t0:t0 + NBLK], yo[s][:], ins=[byo[s]], outs=[by])


def build_S5():
    nc = new_nc()
    u16_d = nc.dram_tensor("u16", [16, 8, T], BF16, kind="ExternalInput").ap()
    lamst_d = nc.dram_tensor("lamst", [128, 3, 8], F32, kind="ExternalInput").ap()
    bst_d = nc.dram_tensor("bst", [128, 8, 16], F32, kind="ExternalInput").ap()
    cst_d = nc.dram_tensor("cst", [128, 8, 16], F32, kind="ExternalInput").ap()
    dsk_d = nc.dram_tensor("dsk", [16, 8], F32, kind="ExternalInput").ap()
    yd = nc.dram_tensor("ys5", [16, 8, T], BF16, kind="ExternalOutput").ap()
    with contextlib.ExitStack() as st:
        k = K(nc, st)
        by = k.buf()
        emit_s5(k, u16_d, lamst_d, bst_d, cst_d, dsk_d, yd, by)
        k.finish([by])
    return nc


def s5_host_inputs(c, u, lam_re, lam_im, log_dt, b_re, b_im, c_re, c_im, d_skip):
    bf = ml_dtypes.bfloat16
    G = slice(8 * c, 8 * c + 8)
    u16 = u[:, 128 * c:128 * (c + 1)].reshape(T, 8, 16).transpose(2, 1, 0).astype(bf)
    st2 = lambda a: np.concatenate([a, a], axis=0)
    lamst = np.stack([st2(lam_re[G].T), st2(lam_im[G].T), np.tile(log_dt[G][None, :], (128, 1))], axis=1).astype(np.float32)
    bst = np.concatenate([b_re[G].transpose(1, 0, 2), b_im[G].transpose(1, 0, 2)], axis=0).astype(np.float32)
    cst = np.concatenate([c_re[G].transpose(2, 0, 1), c_im[G].transpose(2, 0, 1)], axis=0).astype(np.float32)
    dsk = d_skip[128 * c:128 * (c + 1)].reshape(8, 16).T.astype(np.float32)
    return {"u16": np.ascontiguousarray(u16), "lamst": np.ascontiguousarray(lamst), "bst": np.ascontiguousarray(bst),
            "cst": np.ascontiguousarray(cst), "dsk": np.ascontiguousarray(dsk)}


def build_ADA():
    NCOL = 2 * 6 * D // NCORES
    nc = new_nc()
    cT = nc.dram_tensor("cT", [128, 16], F32, kind="ExternalInput").ap()
    w = nc.dram_tensor("w", [D, NCOL], F32, kind="ExternalInput").ap()
    b = nc.dram_tensor("b", [1, NCOL], F32, kind="ExternalInput").ap()
    o = nc.dram_tensor("mod", [1, NCOL], F32, kind="ExternalOutput").ap()
    w_v = w.rearrange("(kc p) n -> p kc n", p=128)
    with contextlib.ExitStack() as st:
        k = K(nc, st)
        cs = k.sb("cs", [128, 16], F32); bcs = k.buf()
        bs = k.sb("bs", [1, NCOL], F32); bbs = k.buf()
        os_ = k.sb("os", [1, NCOL], F32); bos = k.buf()
        wt = [k.sb("wt%d" % i, [128, 16, 512], F32) for i in range(2)]; bwt = k.bufs(2)
        ps = [k.ps("ps%d" % i, [128, 512]) for i in range(2)]; bps = k.bufs(2)
        bo = k.buf()
        k.dma(cs[:], cT, outs=[bcs]); k.dma(bs[:], b, outs=[bbs])
        k.op("act", lambda g: g.activation(out=cs[:], in_=cs[:], func=AF.Silu), ins=[bcs], outs=[bcs])
        for j in range(NCOL // 512):
            s = j % 2
            k.dma(wt[s][:], w_v[:, :, j * 512:(j + 1) * 512], outs=[bwt[s]])
            for kc in range(16):
                k.op("pe", lambda g, kc=kc: g.matmul(ps[s][0:1, :], lhsT=cs[:, kc:kc + 1], rhs=wt[s][:, kc, :], start=(kc == 0), stop=(kc == 15)),
                     ins=[bcs, bwt[s]], outs=[bps[s]])
            k.op("dve", lambda g: g.tensor_tensor(out=os_[:, j * 512:(j + 1) * 512], in0=ps[s][0:1, :], in1=bs[:, j * 512:(j + 1) * 512], op=ALU.add),
                 ins=[bps[s], bbs], outs=[bos])
        k.dma(o, os_[:], ins=[bos], outs=[bo])
        k.finish([bo])
    return nc


def build_C(last):
    NTOK = 2048
    NT = 512
    nc = new_nc()
    xT = nc.dram_tensor("xT", [D, NTOK], F32, kind="ExternalInput").ap()
    pgT = nc.dram_tensor("pgT", [3 * D, NTOK], F32, kind="ExternalInput").ap()
    yT = nc.dram_tensor("yT", [3072, NTOK], BF16, kind="ExternalInput").ap()
    vecs = nc.dram_tensor("vecs", [128, 8, 16], F32, kind="ExternalInput").ap()
    wglu = nc.dram_tensor("wglu", [1024, 1024], F32, kind="ExternalInput").ap()
    pw = [nc.dram_tensor(n, [1024, D], F32, kind="ExternalInput").ap() for n in ("psb", "ps5", "pml")]
    wout = nc.dram_tensor("wout", [D, D], F32, kind="ExternalInput").ap()
    wgr = nc.dram_tensor("wgr", [D, 36], F32, kind="ExternalInput").ap()
    bgr = nc.dram_tensor("bgr", [128, 36], F32, kind="ExternalInput").ap()
    w1 = nc.dram_tensor("w1", [32, D, 256], F32, kind="ExternalInput").ap()
    w3 = nc.dram_tensor("w3", [32, D, 256], F32, kind="ExternalInput").ap()
    w2 = nc.dram_tensor("w2", [32, 256, D], F32, kind="ExternalInput").ap()
    xnT = nc.dram_tensor("xnT", [D, NTOK], F32, kind="ExternalOutput").ap()
    if last:
        outT = nc.dram_tensor("outT", [D, NTOK], F32, kind="ExternalOutput").ap()
    xT_v = xT.rearrange("(kc p) t -> p kc t", p=128)
    xnT_v = xnT.rearrange("(kc p) t -> p kc t", p=128)
    yT_v = yT.rearrange("(kc p) t -> p kc t", p=128)
    pg_v = pgT.rearrange("(br n p) t -> p br n t", br=3, p=128)
    wglu_v = wglu.rearrange("(kc p) n -> p kc n", p=128)
    pw_v = [a.rearrange("(kc p) n -> p kc n", p=128) for a in pw]
    wout_v = wout.rearrange("(kc p) n -> p kc n", p=128)
    wgr_v = wgr.rearrange("(kc p) n -> p kc n", p=128)
    w1_v = w1.rearrange("e (kc p) f -> p e kc f", p=128)
    w3_v = w3.rearrange("e (kc p) f -> p e kc f", p=128)
    w2_v = w2.rearrange("e (f p) n -> p e f n", p=128)
    with contextlib.ExitStack() as st:
        k = K(nc, st)
        identf = k.sb("identf", [128, 128], F32); onesf = k.sb("onesf", [128, 128], F32); ones_bf = k.sb("ones_bf", [128, 128], BF16)
        bc = k.buf()
        k.op("pool", lambda g: g.memset(onesf[:], 1.0), outs=[bc])
        k.op("pool", lambda g: g.memset(ones_bf[:], 1.0), ins=[bc], outs=[bc])
        k.op("pool", lambda g: g.affine_select(out=identf[:], in_=onesf[:], pattern=[[-1, 128]], compare_op=ALU.is_equal,
                                               fill=0.0, base=0, channel_multiplier=1), ins=[bc], outs=[bc])
        vin = k.sb("vin", [128, 8, 16], F32); bvin = k.buf()
        vec = k.sb("vec", [128, 2, 16], F32); bvec = k.buf()
        vecf = k.sb("vecf", [128, 2, 16], F32); bvecf = k.buf()
        wgrs = k.sb("wgrs", [128, 16, 36], F32); bgrs = k.sb("bgrs", [128, 36], F32); bwgr = k.buf()
        k.dma(vin[:], vecs, outs=[bvin]); k.dma(wgrs[:], wgr_v, outs=[bwgr]); k.dma(bgrs[:], bgr, outs=[bwgr])
        k.op("dve", lambda g: g.scalar_tensor_tensor(out=vec[:, 0, :], in0=vin[:, 2, :], scalar=1.0, in1=vin[:, 1, :], op0=ALU.add, op1=ALU.mult), ins=[bvin], outs=[bvec])
        k.op("dve", lambda g: g.tensor_copy(out=vec[:, 1, :], in_=vin[:, 3, :]), ins=[bvin, bvec], outs=[bvec])
        k.op("dve", lambda g: g.tensor_copy(out=vecf[:, 0, :], in_=vin[:, 5, :]), ins=[bvin], outs=[bvecf])
        k.op("dve", lambda g: g.memset(vecf[:, 1, :], 0.0), ins=[bvecf], outs=[bvecf])
        xt = k.sb("xt", [128, 16, NT], F32); bx = k.buf()
        yt = k.sb("yt", [128, 24, NT], BF16); byt = k.buf()
        ysg = k.sb("ysg", [128, 8, NT], BF16); bysg = k.buf()
        mh = k.sb("mh", [128, 16, NT], BF16); bmh = k.buf()
        hid = k.sb("hid", [128, 32, NT], BF16); bhid = k.buf()
        sq = hid[:, 0:16, :]
        wst = [k.sb("wst%d" % i, [128, 4096], F32) for i in range(2)]; bwst = k.bufs(2)
        wbf = [k.sb("wbf%d" % i, [128, 4096], BF16) for i in range(2)]; bwbf = k.bufs(2)
        pgt = [k.sb("pgt%d" % i, [128, 3, NT], F32) for i in range(1)] * 2; bpgt = [k.buf()] * 2
        sg = [k.sb("sg%d" % i, [128, NT], F32) for i in range(3)]; bsg = k.bufs(3)
        t1 = k.sb("t1", [128, NT], F32); t2 = k.sb("t2", [128, NT], F32); bt1 = k.buf(); bt2 = k.buf()
        rstd = k.sb("rstd", [128, NT], F32); brstd = k.buf()
        tmp = k.sb("tmp", [128, 2, NT], F32); btmp = k.bufs(2)
        h2f = [k.sb("h2f%d" % i, [128, NT], F32) for i in range(2)]; bh2f = k.bufs(2)
        gT = k.sb("gT", [32, NT], F32); bgT = k.buf()
        gsel = [k.sb("gsel%d" % i, [32, NT], F32) for i in range(2)]; bgsel = k.bufs(2)
        gbs = [k.sb("gbs%d" % i, [128, NT], F32) for i in range(2)]; bgbs = k.bufs(2)
        rt = k.sb("rt", [128, 16, 36], F32); brt = k.buf()
        P = [k.ps("P%d" % i, [128, 512]) for i in range(8)]; bP = k.bufs(8)
        ACC = [0, 1, 2, 7]
        bout = k.buf()
        wcnt = [0]

        def wtile(view, shape):
            i = wcnt[0] % 2
            wcnt[0] += 1
            a, b = shape
            sv = wst[i][:, 0:a * b].rearrange("p (a b) -> p a b", b=b)
            bv = wbf[i][:, 0:a * b].rearrange("p (a b) -> p a b", b=b)
            k.dma(sv, view, outs=[bwst[i]])
            k.op("pool", lambda g: g.tensor_copy(out=bv, in_=sv), ins=[bwst[i]], outs=[bwbf[i]])
            return bv, bwbf[i]

        acnt = [0]

        def accbank():
            i = ACC[acnt[0] % 4]
            acnt[0] += 1
            return P[i], bP[i]

        for tt in range(NTOK // NT):
            tsl = slice(tt * NT, (tt + 1) * NT)
            k.dma(xt[:], xT_v[:, :, tsl], outs=[bx])
            k.dma(yt[:], yT_v[:, :, tsl], outs=[byt])
            for n in range(8):
                wv, bwv = wtile(wglu_v[:, :, n * 128:(n + 1) * 128], (8, 128))
                ps, bps = accbank()
                for kc in range(8):
                    k.op("pe", lambda g, kc=kc: g.matmul(ps[:], lhsT=wv[:, kc, :], rhs=yt[:, 8 + kc, :], start=(kc == 0), stop=(kc == 7)), ins=[bwv, byt], outs=[bps])
                k.op("act", lambda g: g.activation(out=sg[0][:], in_=ps[:], func=AF.Sigmoid), ins=[bps], outs=[bsg[0]])
                k.op("dve", lambda g: g.tensor_tensor(out=ysg[:, n, :], in0=yt[:, 8 + n, :], in1=sg[0][:], op=ALU.mult), ins=[byt, bsg[0]], outs=[bysg])
            for n in range(16):
                pgs = pgt[n % 2]; bpgs = bpgt[n % 2]
                k.dma(pgs[:], pg_v[:, :, n, tsl], outs=[bpgs])
                banks = []
                for br in range(3):
                    wv, bwv = wtile(pw_v[br][:, :, n * 128:(n + 1) * 128], (8, 128))
                    ps, bps = accbank()
                    for kc in range(8):
                        rhs = ysg[:, kc, :] if br == 1 else yt[:, br * 8 + kc, :]
                        k.op("pe", lambda g, kc=kc, rhs=rhs: g.matmul(ps[:], lhsT=wv[:, kc, :], rhs=rhs, start=(kc == 0), stop=(kc == 7)),
                             ins=[bwv, byt, bysg], outs=[bps])
                    k.op("act", lambda g, br=br: g.activation(out=sg[br][:], in_=pgs[:, br, :], func=AF.Sigmoid), ins=[bpgs], outs=[bsg[br]])
                    banks.append((ps, bps))
                k.op("dve", lambda g: g.tensor_tensor(out=t1[:], in0=banks[0][0][:], in1=sg[0][:], op=ALU.mult), ins=[banks[0][1], bsg[0]], outs=[bt1])
                k.op("dve", lambda g: g.tensor_tensor(out=t2[:], in0=banks[1][0][:], in1=sg[1][:], op=ALU.mult), ins=[banks[1][1], bsg[1]], outs=[bt2])
                k.op("pool", lambda g: g.tensor_tensor(out=t1[:], in0=t1[:], in1=t2[:], op=ALU.add), ins=[bt1, bt2], outs=[bt1])
                k.op("dve", lambda g: g.tensor_tensor(out=t2[:], in0=banks[2][0][:], in1=sg[2][:], op=ALU.mult), ins=[banks[2][1], bsg[2], bt1], outs=[bt2])
                k.op("pool", lambda g: g.tensor_tensor(out=mh[:, n, :], in0=t1[:], in1=t2[:], op=ALU.add), ins=[bt1, bt2], outs=[bmh])
            for n in range(16):
                wv, bwv = wtile(wout_v[:, :, n * 128:(n + 1) * 128], (16, 128))
                ps, bps = accbank()
                for kc in range(16):
                    k.op("pe", lambda g, kc=kc: g.matmul(ps[:], lhsT=wv[:, kc, :], rhs=mh[:, kc, :], start=(kc == 0), stop=(kc == 15)), ins=[bwv, bmh], outs=[bps])
                k.op("dve", lambda g: g.scalar_tensor_tensor(out=xt[:, n, :], in0=ps[:], scalar=vin[:, 0, n:n + 1], in1=xt[:, n, :], op0=ALU.mult, op1=ALU.add),
                     ins=[bps, bvin, bx], outs=[bx])
            k.op("act", lambda g: g.activation(out=sq, in_=xt[:], func=AF.Square), ins=[bx], outs=[bhid])
            for kc in range(16):
                k.op("pe", lambda g, kc=kc: g.matmul(P[6][:], lhsT=ones_bf[:], rhs=sq[:, kc, :], start=(kc == 0), stop=(kc == 15)), ins=[bc, bhid], outs=[bP[6]])
            k.op("act", lambda g: g.activation(out=rstd[:], in_=P[6][:], func=AF.Sqrt, scale=1.0 / D, bias=EPS), ins=[bP[6]], outs=[brstd])
            k.op("dve", lambda g: g.reciprocal(out=rstd[:], in_=rstd[:]), ins=[brstd], outs=[brstd])
            for kc in range(16):
                i = kc % 2
                k.op("dve", lambda g, kc=kc: g.tensor_tensor(out=tmp[:, i, :], in0=xt[:, kc, :], in1=rstd[:], op=ALU.mult), ins=[bx, brstd], outs=[btmp[i]])
                k.op("act", lambda g, kc=kc: g.activation(out=h2f[i][:], in_=tmp[:, i, :], func=AF.Identity, scale=vec[:, 0, kc:kc + 1], bias=vec[:, 1, kc:kc + 1]),
                     ins=[btmp[i], bvec], outs=[bh2f[i]])
                k.op("pool", lambda g, kc=kc: g.tensor_copy(out=mh[:, kc, :], in_=h2f[i][:]), ins=[bh2f[i]], outs=[bmh])
                for j in range(4):
                    k.op("pe", lambda g, kc=kc, j=j: g.matmul(P[4][:, j * 36:(j + 1) * 36], lhsT=h2f[i][:, j * 128:(j + 1) * 128], rhs=wgrs[:, kc, :],
                                                             start=(kc == 0 and j == 0), stop=(kc == 15), skip_group_check=True), ins=[bh2f[i], bwgr], outs=[bP[4]])
            R = lambda a, b_: rt[:, a, 0:b_]
            def dv(fn, extra=()):
                k.op("dve", fn, ins=[brt, bP[4], bwgr] + list(extra), outs=[brt])
            for j in range(4):
                lg = rt[:, 0, :]
                dv(lambda g: g.tensor_tensor(out=lg, in0=P[4][:, j * 36:(j + 1) * 36], in1=bgrs[:], op=ALU.add))
                dv(lambda g: g.tensor_reduce(out=R(1, 1), in_=lg[:, 0:4], axis=AX.X, op=ALU.max))
                dv(lambda g: g.tensor_scalar(out=R(2, 4), in0=lg[:, 0:4], scalar1=R(1, 1), scalar2=None, op0=ALU.is_equal))
                dv(lambda g: g.tensor_scalar(out=R(3, 1), in0=R(1, 1), scalar1=-1.0, scalar2=None, op0=ALU.mult))
                k.op("act", lambda g: g.activation(out=R(4, 4), in_=lg[:, 0:4], func=AF.Exp, bias=R(3, 1)), ins=[brt], outs=[brt])
                dv(lambda g: g.tensor_reduce(out=R(5, 1), in_=R(4, 4), axis=AX.X, op=ALU.add))
                dv(lambda g: g.reciprocal(out=R(5, 1), in_=R(5, 1)))
                dv(lambda g: g.tensor_scalar(out=R(6, 8), in0=lg[:, 4:12], scalar1=rt[:, 2, 0:1], scalar2=None, op0=ALU.mult))
                for gi in range(1, 4):
                    dv(lambda g, gi=gi: g.scalar_tensor_tensor(out=R(6, 8), in0=lg[:, 4 + 8 * gi:12 + 8 * gi], scalar=rt[:, 2, gi:gi + 1], in1=R(6, 8),
                                                               op0=ALU.mult, op1=ALU.add))
                dv(lambda g: g.tensor_reduce(out=R(7, 1), in_=R(6, 8), axis=AX.X, op=ALU.max))
                dv(lambda g: g.tensor_scalar(out=R(8, 8), in0=R(6, 8), scalar1=R(7, 1), scalar2=None, op0=ALU.is_equal))
                dv(lambda g: g.scalar_tensor_tensor(out=R(9, 8), in0=R(8, 8), scalar=-1e30, in1=R(6, 8), op0=ALU.mult, op1=ALU.add))
                dv(lambda g: g.tensor_reduce(out=R(10, 1), in_=R(9, 8), axis=AX.X, op=ALU.max))
                dv(lambda g: g.tensor_scalar(out=R(11, 8), in0=R(9, 8), scalar1=R(10, 1), scalar2=None, op0=ALU.is_equal))
                dv(lambda g: g.tensor_tensor(out=R(12, 1), in0=R(10, 1), in1=R(7, 1), op=ALU.subtract))
                k.op("act", lambda g: g.activation(out=R(12, 1), in_=R(12, 1), func=AF.Exp), ins=[brt], outs=[brt])
                dv(lambda g: g.tensor_scalar(out=R(13, 1), in0=R(12, 1), scalar1=1.0, scalar2=None, op0=ALU.add))
                dv(lambda g: g.reciprocal(out=R(13, 1), in_=R(13, 1)))
                dv(lambda g: g.tensor_tensor(out=R(13, 1), in0=R(13, 1), in1=R(5, 1), op=ALU.mult))
                dv(lambda g: g.tensor_tensor(out=R(14, 1), in0=R(13, 1), in1=R(12, 1), op=ALU.mult))
                dv(lambda g: g.tensor_scalar(out=R(15, 8), in0=R(8, 8), scalar1=R(13, 1), scalar2=None, op0=ALU.mult))
                dv(lambda g: g.scalar_tensor_tensor(out=R(15, 8), in0=R(11, 8), scalar=R(14, 1), in1=R(15, 8), op0=ALU.mult, op1=ALU.add))
                gts = rt[:, 1, 4:36]
                for gi in range(4):
                    dv(lambda g, gi=gi: g.tensor_scalar(out=gts[:, gi * 8:(gi + 1) * 8], in0=R(15, 8), scalar1=rt[:, 2, gi:gi + 1], scalar2=None, op0=ALU.mult))
                k.op("pe", lambda g: g.transpose(P[5][0:32, 0:128], gts, identf[:]), ins=[brt, bc], outs=[bP[5]])
                k.op("act", lambda g, j=j: g.activation(out=gT[:, j * 128:(j + 1) * 128], in_=P[5][0:32, 0:128], func=AF.Identity), ins=[bP[5]], outs=[bgT])
            for half in range(2):
                for el in range(16):
                    e = half * 16 + el
                    gsl = gbs[e % 2]; bgsl = bgbs[e % 2]
                    k.op("pool", lambda g, e=e: g.tensor_scalar(out=gsel[e % 2][:], in0=gT[:], scalar1=identf[0:32, e:e + 1], scalar2=None, op0=ALU.mult),
                         ins=[bc, bgT], outs=[bgsel[e % 2]])
                    k.op("pe", lambda g, e=e: g.matmul(P[3][:], lhsT=onesf[0:32, :], rhs=gsel[e % 2][:], start=True, stop=True), ins=[bc, bgsel[e % 2]], outs=[bP[3]])
                    k.op("act", lambda g: g.activation(out=gsl[:], in_=P[3][:], func=AF.Identity), ins=[bP[3]], outs=[bgsl])
                    w1v, bw1 = wtile(w1_v[:, e, :, :], (16, 256))
                    w3v, bw3 = wtile(w3_v[:, e, :, :], (16, 256))
                    for f in range(2):
                        pa, bpa = accbank()
                        pb, bpb = accbank()
                        for kc in range(16):
                            k.op("pe", lambda g, kc=kc: g.matmul(pa[:], lhsT=w1v[:, kc, f * 128:(f + 1) * 128], rhs=mh[:, kc, :], start=(kc == 0), stop=(kc == 15)),
                                 ins=[bw1, bmh], outs=[bpa])
                        for kc in range(16):
                            k.op("pe", lambda g, kc=kc: g.matmul(pb[:], lhsT=w3v[:, kc, f * 128:(f + 1) * 128], rhs=mh[:, kc, :], start=(kc == 0), stop=(kc == 15)),
                                 ins=[bw3, bmh], outs=[bpb])
                        k.op("act", lambda g: g.activation(out=t1[:], in_=pa[:], func=AF.Silu), ins=[bpa], outs=[bt1])
                        k.op("dve", lambda g: g.tensor_tensor(out=t2[:], in0=pb[:], in1=t1[:], op=ALU.mult), ins=[bpb, bt1], outs=[bt2])
                        k.op("pool", lambda g, el=el, f=f: g.tensor_tensor(out=hid[:, el * 2 + f, :], in0=t2[:], in1=gsl[:], op=ALU.mult), ins=[bt2, bgsl], outs=[bhid])
                for n in range(16):
                    wv, bwv = wtile(w2_v[:, half * 16:(half + 1) * 16, :, n * 128:(n + 1) * 128].rearrange("p e f n -> p (e f) n"), (32, 128))
                    ps, bps = accbank()
                    for kk in range(32):
                        k.op("pe", lambda g, kk=kk: g.matmul(ps[:], lhsT=wv[:, kk, :], rhs=hid[:, kk, :], start=(kk == 0), stop=(kk == 31)), ins=[bwv, bhid], outs=[bps])
                    k.op("dve", lambda g: g.scalar_tensor_tensor(out=xt[:, n, :], in0=ps[:], scalar=vin[:, 4, n:n + 1], in1=xt[:, n, :], op0=ALU.mult, op1=ALU.add),
                         ins=[bps, bvin, bx], outs=[bx])
            k.dma(xnT_v[:, :, tsl], xt[:], ins=[bx], outs=[bout])
            if last:
                k.op("act", lambda g: g.activation(out=sq, in_=xt[:], func=AF.Square), ins=[bx], outs=[bhid])
                for kc in range(16):
                    k.op("pe", lambda g, kc=kc: g.matmul(P[6][:], lhsT=ones_bf[:], rhs=sq[:, kc, :], start=(kc == 0), stop=(kc == 15)), ins=[bc, bhid], outs=[bP[6]])
                k.op("act", lambda g: g.activation(out=rstd[:], in_=P[6][:], func=AF.Sqrt, scale=1.0 / D, bias=EPS), ins=[bP[6]], outs=[brstd])
                k.op("dve", lambda g: g.reciprocal(out=rstd[:], in_=rstd[:]), ins=[brstd], outs=[brstd])
                for kc in range(16):
                    i = kc % 2
                    k.op("dve", lambda g, kc=kc: g.tensor_tensor(out=tmp[:, i, :], in0=xt[:, kc, :], in1=rstd[:], op=ALU.mult), ins=[bx, brstd], outs=[btmp[i]])
                    k.op("act", lambda g, kc=kc: g.activation(out=h2f[i][:], in_=tmp[:, i, :], func=AF.Identity, scale=vecf[:, 0, kc:kc + 1], bias=vecf[:, 1, kc:kc + 1]),
                         ins=[btmp[i], bvecf], outs=[bh2f[i]])
                    k.dma(outT[kc * 128:(kc + 1) * 128, tsl], h2f[i][:], ins=[bh2f[i]], outs=[bout])
        k.finish([bout])
    return nc


def build_C2(last):
    NTOK = 2048
    NT = 512
    nc = new_nc()
    xT = nc.dram_tensor("xT", [D, NTOK], F32, kind="ExternalInput").ap()
    pgT = nc.dram_tensor("pgT", [3 * D, NTOK], F32, kind="ExternalInput").ap()
    yT = nc.dram_tensor("yT", [3072, NTOK], BF16, kind="ExternalInput").ap()
    vecs = nc.dram_tensor("vecs", [128, 8, 16], F32, kind="ExternalInput").ap()
    wglu = nc.dram_tensor("wglu", [1024, 1024], F32, kind="ExternalInput").ap()
    pw = [nc.dram_tensor(n, [1024, D], F32, kind="ExternalInput").ap() for n in ("psb", "ps5", "pml")]
    wout = nc.dram_tensor("wout", [D, D], F32, kind="ExternalInput").ap()
    wgr = nc.dram_tensor("wgr", [D, 36], F32, kind="ExternalInput").ap()
    bgr = nc.dram_tensor("bgr", [128, 36], F32, kind="ExternalInput").ap()
    w1 = nc.dram_tensor("w1", [32, D, 256], F32, kind="ExternalInput").ap()
    w3 = nc.dram_tensor("w3", [32, D, 256], F32, kind="ExternalInput").ap()
    w2 = nc.dram_tensor("w2", [32, 256, D], F32, kind="ExternalInput").ap()
    xnT = nc.dram_tensor("xnT", [D, NTOK], F32, kind="ExternalOutput").ap()
    if last:
        outT = nc.dram_tensor("outT", [D, NTOK], F32, kind="ExternalOutput").ap()
    xT_v = xT.rearrange("(kc p) t -> p kc t", p=128)
    xnT_v = xnT.rearrange("(kc p) t -> p kc t", p=128)
    yT_v = yT.rearrange("(kc p) t -> p kc t", p=128)
    pg_v = pgT.rearrange("(br n p) t -> p br n t", br=3, p=128)
    wglu_v = wglu.rearrange("(kc p) n -> p kc n", p=128)
    pw_v = [a.rearrange("(kc p) n -> p kc n", p=128) for a in pw]
    wout_v = wout.rearrange("(kc p) n -> p kc n", p=128)
    wgr_v = wgr.rearrange("(kc p) n -> p kc n", p=128)
    w1_v = w1.rearrange("e (kc p) f -> p e kc f", p=128)
    w3_v = w3.rearrange("e (kc p) f -> p e kc f", p=128)
    w2_v = w2.rearrange("e (f p) n -> p e f n", p=128)
    xmid_d = nc.dram_tensor("xmid_d", [D, NTOK], F32).ap()
    xmid_v = xmid_d.rearrange("(kc p) t -> p kc t", p=128)
    with contextlib.ExitStack() as st:
        k = K(nc, st)
        identf = k.sb("identf", [128, 128], F32); onesf = k.sb("onesf", [128, 128], F32); ones_bf = k.sb("ones_bf", [128, 128], BF16)
        bc = k.buf()
        k.op("pool", lambda g: g.memset(onesf[:], 1.0), outs=[bc])
        k.op("pool", lambda g: g.memset(ones_bf[:], 1.0), ins=[bc], outs=[bc])
        k.op("pool", lambda g: g.affine_select(out=identf[:], in_=onesf[:], pattern=[[-1, 128]], compare_op=ALU.is_equal,
                                               fill=0.0, base=0, channel_multiplier=1), ins=[bc], outs=[bc])
        vin = k.sb("vin", [128, 8, 16], F32); bvin = k.buf()
        vec = k.sb("vec", [128, 2, 16], F32); bvec = k.buf()
        vecf = k.sb("vecf", [128, 2, 16], F32); bvecf = k.buf()
        wgrs = k.sb("wgrs", [128, 16, 36], F32); bgrs = k.sb("bgrs", [128, 36], F32); bwgr = k.buf()
        k.dma(vin[:], vecs, outs=[bvin]); k.dma(wgrs[:], wgr_v, outs=[bwgr]); k.dma(bgrs[:], bgr, outs=[bwgr])
        k.op("dve", lambda g: g.scalar_tensor_tensor(out=vec[:, 0, :], in0=vin[:, 2, :], scalar=1.0, in1=vin[:, 1, :], op0=ALU.add, op1=ALU.mult), ins=[bvin], outs=[bvec])
        k.op("dve", lambda g: g.tensor_copy(out=vec[:, 1, :], in_=vin[:, 3, :]), ins=[bvin, bvec], outs=[bvec])
        k.op("dve", lambda g: g.tensor_copy(out=vecf[:, 0, :], in_=vin[:, 5, :]), ins=[bvin], outs=[bvecf])
        k.op("dve", lambda g: g.memset(vecf[:, 1, :], 0.0), ins=[bvecf], outs=[bvecf])
        mh2 = k.sb("mh2", [128, 16, NTOK], BF16); bmh2 = k.bufs(NTOK // NT)
        gT = k.sb("gT", [32, NTOK], F32); bgT = k.bufs(NTOK // NT)
        P = [k.ps("P%d" % i, [128, 512]) for i in range(8)]; bP = k.bufs(8)
        ACC = [0, 1, 2, 7]
        bout = k.buf()
        bxm = [[k.buf() for _ in range(NTOK // NT)] for _ in range(16)]
        wcnt = [0]
        ccnt = [0]
        acnt = [0]

        def accbank():
            i = ACC[acnt[0] % 4]
            acnt[0] += 1
            return P[i], bP[i]

        outer = k.stack
        with contextlib.ExitStack() as s1:
            k.stack = s1
            xt = k.sb("xt", [128, 16, NT], F32); bx = k.buf()
            yt = k.sb("yt", [128, 24, NT], BF16); byt = k.buf()
            sq = yt[:, 0:16, :]
            ysg = k.sb("ysg", [128, 8, NT], BF16); bysg = k.buf()
            mh = k.sb("mh", [128, 16, NT], BF16); bmh = k.buf()
            NW = 2
            wst = [k.sb("wst%d" % i, [128, 2048], F32) for i in range(NW)]; bwst = k.bufs(NW)
            wbf = [k.sb("wbf%d" % i, [128, 2048], BF16) for i in range(NW)]; bwbf = k.bufs(NW)
            pgt = [k.sb("pgt%d" % i, [128, 3, NT], F32) for i in range(1)] * 2; bpgt = [k.buf()] * 2
            sg = [k.sb("sg%d" % i, [128, NT], F32) for i in range(3)]; bsg = k.bufs(3)
            t1 = k.sb("t1", [128, NT], F32); t2 = k.sb("t2", [128, NT], F32); bt1 = k.buf(); bt2 = k.buf()
            rstd = k.sb("rstd", [128, NT], F32); brstd = k.buf()
            tmp = [t1, t2]; btmp = [bt1, bt2]
            h2f = [k.sb("h2f%d" % i, [128, NT], F32) for i in range(2)]; bh2f = k.bufs(2)
            rt = k.sb("rt", [128, 16, 36], F32); brt = k.buf()

            def wtile(view, shape):
                i = wcnt[0] % NW
                wcnt[0] += 1
                a, b = shape
                sv = wst[i][:, 0:a * b].rearrange("p (a b) -> p a b", b=b)
                bv = wbf[i][:, 0:a * b].rearrange("p (a b) -> p a b", b=b)
                k.dma(sv, view, outs=[bwst[i]])
                ce = ("dve", "act")[ccnt[0] % 2]
                ccnt[0] += 1
                if ce == "act":
                    k.op("act", lambda g: g.activation(out=bv, in_=sv, func=AF.Identity), ins=[bwst[i]], outs=[bwbf[i]])
                else:
                    k.op(ce, lambda g: g.tensor_copy(out=bv, in_=sv), ins=[bwst[i]], outs=[bwbf[i]])
                return bv, bwbf[i]

            for tt in range(NTOK // NT):
                tsl = slice(tt * NT, (tt + 1) * NT)
                k.dma(xt[:], xT_v[:, :, tsl], outs=[bx])
                k.dma(yt[:], yT_v[:, :, tsl], outs=[byt])
                for n in range(8):
                    wv, bwv = wtile(wglu_v[:, :, n * 128:(n + 1) * 128], (8, 128))
                    ps, bps = accbank()
                    for kc in range(8):
                        k.op("pe", lambda g, kc=kc: g.matmul(ps[:], lhsT=wv[:, kc, :], rhs=yt[:, 8 + kc, :], start=(kc == 0), stop=(kc == 7)), ins=[bwv, byt], outs=[bps])
                    k.op("act", lambda g: g.activation(out=sg[0][:], in_=ps[:], func=AF.Sigmoid), ins=[bps], outs=[bsg[0]])
                    k.op("dve", lambda g: g.tensor_tensor(out=ysg[:, n, :], in0=yt[:, 8 + n, :], in1=sg[0][:], op=ALU.mult), ins=[byt, bsg[0]], outs=[bysg])
                for n in range(16):
                    pgs = pgt[n % 2]; bpgs = bpgt[n % 2]
                    k.dma(pgs[:], pg_v[:, :, n, tsl], outs=[bpgs])
                    banks = []
                    for br in range(3):
                        wv, bwv = wtile(pw_v[br][:, :, n * 128:(n + 1) * 128], (8, 128))
                        ps, bps = accbank()
                        for kc in range(8):
                            rhs = ysg[:, kc, :] if br == 1 else yt[:, br * 8 + kc, :]
                            k.op("pe", lambda g, kc=kc, rhs=rhs: g.matmul(ps[:], lhsT=wv[:, kc, :], rhs=rhs, start=(kc == 0), stop=(kc == 7)),
                                 ins=[bwv, byt, bysg], outs=[bps])
                        k.op("act", lambda g, br=br: g.activation(out=sg[br][:], in_=pgs[:, br, :], func=AF.Sigmoid), ins=[bpgs], outs=[bsg[br]])
                        banks.append((ps, bps))
                    k.op("dve", lambda g: g.tensor_tensor(out=t1[:], in0=banks[0][0][:], in1=sg[0][:], op=ALU.mult), ins=[banks[0][1], bsg[0]], outs=[bt1])
                    k.op("dve", lambda g: g.tensor_tensor(out=t2[:], in0=banks[1][0][:], in1=sg[1][:], op=ALU.mult), ins=[banks[1][1], bsg[1]], outs=[bt2])
                    k.op("pool", lambda g: g.tensor_tensor(out=t1[:], in0=t1[:], in1=t2[:], op=ALU.add), ins=[bt1, bt2], outs=[bt1])
                    k.op("dve", lambda g: g.tensor_tensor(out=t2[:], in0=banks[2][0][:], in1=sg[2][:], op=ALU.mult), ins=[banks[2][1], bsg[2], bt1], outs=[bt2])
                    k.op("pool", lambda g: g.tensor_tensor(out=mh[:, n, :], in0=t1[:], in1=t2[:], op=ALU.add), ins=[bt1, bt2], outs=[bmh])
                for n in range(16):
                    wv, bwv = wtile(wout_v[:, :, n * 128:(n + 1) * 128], (16, 128))
                    ps, bps = accbank()
                    for kc in range(16):
                        k.op("pe", lambda g, kc=kc: g.matmul(ps[:], lhsT=wv[:, kc, :], rhs=mh[:, kc, :], start=(kc == 0), stop=(kc == 15)), ins=[bwv, bmh], outs=[bps])
                    k.op("dve", lambda g: g.scalar_tensor_tensor(out=xt[:, n, :], in0=ps[:], scalar=vin[:, 0, n:n + 1], in1=xt[:, n, :], op0=ALU.mult, op1=ALU.add),
                         ins=[bps, bvin, bx], outs=[bx])
                k.op("act", lambda g: g.activation(out=sq, in_=xt[:], func=AF.Square), ins=[bx], outs=[byt])
                for kc in range(16):
                    k.op("pe", lambda g, kc=kc: g.matmul(P[6][:], lhsT=ones_bf[:], rhs=sq[:, kc, :], start=(kc == 0), stop=(kc == 15)), ins=[bc, byt], outs=[bP[6]])
                k.op("act", lambda g: g.activation(out=rstd[:], in_=P[6][:], func=AF.Sqrt, scale=1.0 / D, bias=EPS), ins=[bP[6]], outs=[brstd])
                k.op("dve", lambda g: g.reciprocal(out=rstd[:], in_=rstd[:]), ins=[brstd], outs=[brstd])
                for kc in range(16):
                    i = kc % 2
                    k.op("dve", lambda g, kc=kc: g.tensor_tensor(out=tmp[i][:], in0=xt[:, kc, :], in1=rstd[:], op=ALU.mult), ins=[bx, brstd], outs=[btmp[i]])
                    k.op("act", lambda g, kc=kc: g.activation(out=h2f[i][:], in_=tmp[i][:], func=AF.Identity, scale=vec[:, 0, kc:kc + 1], bias=vec[:, 1, kc:kc + 1]),
                         ins=[btmp[i], bvec], outs=[bh2f[i]])
                    k.op("pool", lambda g, kc=kc: g.tensor_copy(out=mh2[:, kc, tsl], in_=h2f[i][:]), ins=[bh2f[i]], outs=[bmh2[tt]])
                    for j in range(4):
                        k.op("pe", lambda g, kc=kc, j=j: g.matmul(P[4][:, j * 36:(j + 1) * 36], lhsT=h2f[i][:, j * 128:(j + 1) * 128], rhs=wgrs[:, kc, :],
                                                                 start=(kc == 0 and j == 0), stop=(kc == 15), skip_group_check=True), ins=[bh2f[i], bwgr], outs=[bP[4]])
                R = lambda a, b_: rt[:, a, 0:b_]
                def dv(fn, extra=()):
                    k.op("dve", fn, ins=[brt, bP[4], bwgr] + list(extra), outs=[brt])
                for j in range(4):
                    lg = rt[:, 0, :]
                    dv(lambda g: g.tensor_tensor(out=lg, in0=P[4][:, j * 36:(j + 1) * 36], in1=bgrs[:], op=ALU.add))
                    dv(lambda g: g.tensor_reduce(out=R(1, 1), in_=lg[:, 0:4], axis=AX.X, op=ALU.max))
                    dv(lambda g: g.tensor_scalar(out=R(2, 4), in0=lg[:, 0:4], scalar1=R(1, 1), scalar2=None, op0=ALU.is_equal))
                    dv(lambda g: g.tensor_scalar(out=R(3, 1), in0=R(1, 1), scalar1=-1.0, scalar2=None, op0=ALU.mult))
                    k.op("act", lambda g: g.activation(out=R(4, 4), in_=lg[:, 0:4], func=AF.Exp, bias=R(3, 1)), ins=[brt], outs=[brt])
                    dv(lambda g: g.tensor_reduce(out=R(5, 1), in_=R(4, 4), axis=AX.X, op=ALU.add))
                    dv(lambda g: g.reciprocal(out=R(5, 1), in_=R(5, 1)))
                    dv(lambda g: g.tensor_scalar(out=R(6, 8), in0=lg[:, 4:12], scalar1=rt[:, 2, 0:1], scalar2=None, op0=ALU.mult))
                    for gi in range(1, 4):
                        dv(lambda g, gi=gi: g.scalar_tensor_tensor(out=R(6, 8), in0=lg[:, 4 + 8 * gi:12 + 8 * gi], scalar=rt[:, 2, gi:gi + 1], in1=R(6, 8),
                                                                   op0=ALU.mult, op1=ALU.add))
                    dv(lambda g: g.tensor_reduce(out=R(7, 1), in_=R(6, 8), axis=AX.X, op=ALU.max))
                    dv(lambda g: g.tensor_scalar(out=R(8, 8), in0=R(6, 8), scalar1=R(7, 1), scalar2=None, op0=ALU.is_equal))
                    dv(lambda g: g.scalar_tensor_tensor(out=R(9, 8), in0=R(8, 8), scalar=-1e30, in1=R(6, 8), op0=ALU.mult, op1=ALU.add))
                    dv(lambda g: g.tensor_reduce(out=R(10, 1), in_=R(9, 8), axis=AX.X, op=ALU.max))
                    dv(lambda g: g.tensor_scalar(out=R(11, 8), in0=R(9, 8), scalar1=R(10, 1), scalar2=None, op0=ALU.is_equal))
                    dv(lambda g: g.tensor_tensor(out=R(12, 1), in0=R(10, 1), in1=R(7, 1), op=ALU.subtract))
                    k.op("act", lambda g: g.activation(out=R(12, 1), in_=R(12, 1), func=AF.Exp), ins=[brt], outs=[brt])
                    dv(lambda g: g.tensor_scalar(out=R(13, 1), in0=R(12, 1), scalar1=1.0, scalar2=None, op0=ALU.add))
                    dv(lambda g: g.reciprocal(out=R(13, 1), in_=R(13, 1)))
                    dv(lambda g: g.tensor_tensor(out=R(13, 1), in0=R(13, 1), in1=R(5, 1), op=ALU.mult))
                    dv(lambda g: g.tensor_tensor(out=R(14, 1), in0=R(13, 1), in1=R(12, 1), op=ALU.mult))
                    dv(lambda g: g.tensor_scalar(out=R(15, 8), in0=R(8, 8), scalar1=R(13, 1), scalar2=None, op0=ALU.mult))
                    dv(lambda g: g.scalar_tensor_tensor(out=R(15, 8), in0=R(11, 8), scalar=R(14, 1), in1=R(15, 8), op0=ALU.mult, op1=ALU.add))
                    gts = rt[:, 1, 4:36]
                    for gi in range(4):
                        dv(lambda g, gi=gi: g.tensor_scalar(out=gts[:, gi * 8:(gi + 1) * 8], in0=R(15, 8), scalar1=rt[:, 2, gi:gi + 1], scalar2=None, op0=ALU.mult))
                    k.op("pe", lambda g: g.transpose(P[5][0:32, 0:128], gts, identf[:]), ins=[brt, bc], outs=[bP[5]])
                    k.op("act", lambda g, j=j: g.activation(out=gT[:, tt * NT + j * 128:tt * NT + (j + 1) * 128], in_=P[5][0:32, 0:128], func=AF.Identity), ins=[bP[5]], outs=[bgT[tt]])
                k.dma(xmid_v[:, :, tsl], xt[:], ins=[bx], outs=[bxm[n][tt] for n in range(16)], q="pool")
            k.barrier()
        k.stack = outer
        with contextlib.ExitStack() as s2:
            k.stack = s2
            hid = k.sb("hid", [128, 16, NTOK], BF16); bhid = k.bufs(NTOK // NT)
            wst = [k.sb("wstb%d" % i, [128, 4096], F32) for i in range(2)]; bwst = k.bufs(2)
            wbf = [k.sb("wbfb%d" % i, [128, 4096], BF16) for i in range(2)]; bwbf = k.bufs(2)
            gsel = [k.sb("gsel%d" % i, [32, NT], F32) for i in range(2)]; bgsel = k.bufs(2)
            gbs = [k.sb("gbs%d" % i, [128, NT], F32) for i in range(2)]; bgbs = k.bufs(2)
            t1 = k.sb("t1b", [128, NT], F32); t2 = k.sb("t2b", [128, NT], F32); bt1 = k.buf(); bt2 = k.buf()
            NX = 3
            xs = [k.sb("xs%d" % i, [128, NT], F32) for i in range(NX)]; bxs = k.bufs(NX)
            xcnt = [0]
            wcnt[0] = 0

            def wtile2(view, shape):
                i = wcnt[0] % 2
                wcnt[0] += 1
                a, b = shape
                sv = wst[i][:, 0:a * b].rearrange("p (a b) -> p a b", b=b)
                bv = wbf[i][:, 0:a * b].rearrange("p (a b) -> p a b", b=b)
                k.dma(sv, view, outs=[bwst[i]])
                ce = "dve"
                ccnt[0] += 1
                if ce == "act":
                    k.op("act", lambda g: g.activation(out=bv, in_=sv, func=AF.Identity), ins=[bwst[i]], outs=[bwbf[i]])
                else:
                    k.op(ce, lambda g: g.tensor_copy(out=bv, in_=sv), ins=[bwst[i]], outs=[bwbf[i]])
                return bv, bwbf[i]

            NG = 4
            for grp in range(NG):
                for el in range(8):
                    e = grp * 8 + el
                    w1v, bw1 = wtile2(w1_v[:, e, :, :], (16, 256))
                    w3v, bw3 = wtile2(w3_v[:, e, :, :], (16, 256))
                    for tt in range(NTOK // NT):
                        tsl = slice(tt * NT, (tt + 1) * NT)
                        gi = (e * 4 + tt) % 2
                        gsl = gbs[gi]; bgsl = bgbs[gi]
                        k.op("dve", lambda g, e=e, gi=gi, tsl=tsl: g.tensor_scalar(out=gsel[gi][:], in0=gT[:, tsl], scalar1=identf[0:32, e:e + 1], scalar2=None, op0=ALU.mult),
                             ins=[bc, bgT[tt]], outs=[bgsel[gi]])
                        k.op("pe", lambda g, gi=gi: g.matmul(P[3][:], lhsT=onesf[0:32, :], rhs=gsel[gi][:], start=True, stop=True), ins=[bc, bgsel[gi]], outs=[bP[3]])
                        k.op("act", lambda g, gsl=gsl: g.activation(out=gsl[:], in_=P[3][:], func=AF.Identity), ins=[bP[3]], outs=[bgsl])
                        for f in range(2):
                            pa, bpa = accbank()
                            pb, bpb = accbank()
                            for kc in range(16):
                                k.op("pe", lambda g, kc=kc, pa=pa, f=f, tsl=tsl: g.matmul(pa[:], lhsT=w1v[:, kc, f * 128:(f + 1) * 128], rhs=mh2[:, kc, tsl], start=(kc == 0), stop=(kc == 15)),
                                     ins=[bw1, bmh2[tt]], outs=[bpa])
                            for kc in range(16):
                                k.op("pe", lambda g, kc=kc, pb=pb, f=f, tsl=tsl: g.matmul(pb[:], lhsT=w3v[:, kc, f * 128:(f + 1) * 128], rhs=mh2[:, kc, tsl], start=(kc == 0), stop=(kc == 15)),
                                     ins=[bw3, bmh2[tt]], outs=[bpb])
                            k.op("act", lambda g, pa=pa: g.activation(out=t1[:], in_=pa[:], func=AF.Silu), ins=[bpa], outs=[bt1])
                            k.op("dve", lambda g, pb=pb: g.tensor_tensor(out=t2[:], in0=pb[:], in1=t1[:], op=ALU.mult), ins=[bpb, bt1], outs=[bt2])
                            k.op("pool", lambda g, el=el, f=f, tsl=tsl, gsl=gsl: g.tensor_tensor(out=hid[:, el * 2 + f, tsl], in0=t2[:], in1=gsl[:], op=ALU.mult),
                                 ins=[bt2, bgsl], outs=[bhid[tt]])
                for n in range(16):
                    wv, bwv = wtile2(w2_v[:, grp * 8:(grp + 1) * 8, :, n * 128:(n + 1) * 128].rearrange("p e f n -> p (e f) n"), (16, 128))
                    for tt in range(NTOK // NT):
                        tsl = slice(tt * NT, (tt + 1) * NT)
                        xi = xcnt[0] % NX
                        xcnt[0] += 1
                        k.dma(xs[xi][:], xmid_d[n * 128:(n + 1) * 128, tsl], ins=[bxm[n][tt]], outs=[bxs[xi]])
                        ps, bps = accbank()
                        for kk in range(16):
                            k.op("pe", lambda g, kk=kk, ps=ps, tsl=tsl: g.matmul(ps[:], lhsT=wv[:, kk, :], rhs=hid[:, kk, tsl], start=(kk == 0), stop=(kk == 15)), ins=[bwv, bhid[tt]], outs=[bps])
                        k.op("dve", lambda g, ps=ps, xi=xi, n=n: g.scalar_tensor_tensor(out=xs[xi][:], in0=ps[:], scalar=vin[:, 4, n:n + 1], in1=xs[xi][:], op0=ALU.mult, op1=ALU.add),
                             ins=[bps, bvin, bxs[xi]], outs=[bxs[xi]])
                        if grp < NG - 1 or last:
                            k.dma(xmid_d[n * 128:(n + 1) * 128, tsl], xs[xi][:], ins=[bxs[xi]], outs=[bxm[n][tt]], q="pool")
                        if grp == NG - 1:
                            k.dma(xnT[n * 128:(n + 1) * 128, tsl], xs[xi][:], ins=[bxs[xi]], outs=[bout], q="pool")
            k.barrier()
        k.stack = outer
        if last:
            with contextlib.ExitStack() as s3:
                k.stack = s3
                xt = k.sb("xt3", [128, 16, NT], F32); bx = k.buf()
                sq = k.sb("sq3", [128, 16, NT], BF16); bsq = k.buf()
                rstd = k.sb("rstd3", [128, NT], F32); brstd = k.buf()
                tmp = k.sb("tmp3", [128, 2, NT], F32); btmp = k.bufs(2)
                h2f = [k.sb("o3_%d" % i, [128, NT], F32) for i in range(2)]; bh2f = k.bufs(2)
                for tt in range(NTOK // NT):
                    tsl = slice(tt * NT, (tt + 1) * NT)
                    k.dma(xt[:], xmid_v[:, :, tsl], ins=[bxm[n][tt] for n in range(16)], outs=[bx])
                    k.op("act", lambda g: g.activation(out=sq[:], in_=xt[:], func=AF.Square), ins=[bx], outs=[bsq])
                    for kc in range(16):
                        k.op("pe", lambda g, kc=kc: g.matmul(P[6][:], lhsT=ones_bf[:], rhs=sq[:, kc, :], start=(kc == 0), stop=(kc == 15)), ins=[bc, bsq], outs=[bP[6]])
                    k.op("act", lambda g: g.activation(out=rstd[:], in_=P[6][:], func=AF.Sqrt, scale=1.0 / D, bias=EPS), ins=[bP[6]], outs=[brstd])
                    k.op("dve", lambda g: g.reciprocal(out=rstd[:], in_=rstd[:]), ins=[brstd], outs=[brstd])
                    for kc in range(16):
                        i = kc % 2
                        k.op("dve", lambda g, kc=kc, i=i: g.tensor_tensor(out=tmp[:, i, :], in0=xt[:, kc, :], in1=rstd[:], op=ALU.mult), ins=[bx, brstd], outs=[btmp[i]])
                        k.op("act", lambda g, kc=kc, i=i: g.activation(out=h2f[i][:], in_=tmp[:, i, :], func=AF.Identity, scale=vecf[:, 0, kc:kc + 1], bias=vecf[:, 1, kc:kc + 1]),
                             ins=[btmp[i], bvecf], outs=[bh2f[i]])
                        k.dma(outT[kc * 128:(kc + 1) * 128, tsl], h2f[i][:], ins=[bh2f[i]], outs=[bout], q="pool")
                k.barrier()
            k.stack = outer
        k.finish([bout])
    return nc


_NC_CACHE = {}


def _get_nc(name, builder, *a):
    key = (name,) + a
    if key not in _NC_CACHE:
        _NC_CACHE[key] = builder(*a)
    return _NC_CACHE[key]


def _run(nc, in_maps):
    res = run_bass_kernel_spmd(nc, in_maps, core_ids=list(range(len(in_maps))))
    return res.results


def _pk(v):
    return np.ascontiguousarray(np.asarray(v, np.float32).reshape(16, 128).T)


def _c_inputs(c, l, xT_c, pg_c, yT_c, mod_l, inp):
    shift1, scale1, gate1, shift2, scale2, gate2 = np.split(mod_l, 6)
    vecs = np.stack([_pk(gate1), _pk(inp['norm_moe_g'][l]), _pk(scale2), _pk(shift2), _pk(gate2), _pk(inp['final_g']),
                     _pk(gate2), _pk(gate2)], axis=1)
    wgr = np.concatenate([inp['moe_w_group'][l], inp['moe_w_router'][l]], axis=1)
    bgr = np.tile(np.concatenate([inp['moe_b_group'][l], inp['moe_b_router'][l]])[None, :], (128, 1))
    return {"xT": xT_c, "pgT": pg_c, "yT": yT_c, "vecs": np.ascontiguousarray(vecs.astype(np.float32)),
            "wglu": np.ascontiguousarray(inp['s5_w_glu'][l]), "psb": np.ascontiguousarray(inp['p_sb'][l]),
            "ps5": np.ascontiguousarray(inp['p_s5'][l]), "pml": np.ascontiguousarray(inp['p_ml'][l]),
            "wout": np.ascontiguousarray(inp['w_out'][l]), "wgr": np.ascontiguousarray(wgr.astype(np.float32)),
            "bgr": np.ascontiguousarray(bgr.astype(np.float32)), "w1": np.ascontiguousarray(inp['moe_w1'][l]),
            "w3": np.ascontiguousarray(inp['moe_w3'][l]), "w2": np.ascontiguousarray(inp['moe_w2'][l])}


def kernel(**inp):
    inp = {k_: np.asarray(v) for k_, v in inp.items()}
    bf = ml_dtypes.bfloat16
    x = inp['x'][0]
    TS = T // NCORES
    wcat = np.concatenate([inp['w_ada'][0], inp['w_ada'][1]], axis=1)
    bcat = np.concatenate([inp['b_ada'][0], inp['b_ada'][1]])
    ncol = wcat.shape[1] // NCORES
    cT = _pk(inp['c'][0])
    r = _run(_get_nc("ada", build_ADA), [{"cT": cT, "w": np.ascontiguousarray(wcat[:, c * ncol:(c + 1) * ncol]),
                                          "b": np.ascontiguousarray(bcat[None, c * ncol:(c + 1) * ncol])} for c in range(NCORES)])
    mod = np.concatenate([np.asarray(r[c]["mod"])[0] for c in range(NCORES)])
    del wcat
    xT = [np.ascontiguousarray(x[c * TS:(c + 1) * TS].T) for c in range(NCORES)]
    out = None
    for l in range(DEPTH):
        mod_l = mod[l * 6 * D:(l + 1) * 6 * D]
        shift1, scale1 = mod_l[0:D], mod_l[D:2 * D]
        vecsA = np.ascontiguousarray(np.stack([_pk(inp['norm_mix_g'][l]), _pk(scale1), _pk(shift1)], axis=1))
        wA = np.ascontiguousarray(inp['w_in'][l])
        r = _run(_get_nc("A", build_A), [{"xT": xT[c], "vecs": vecsA, "w": wA} for c in range(NCORES)])
        pa = np.concatenate([np.asarray(r[c]["pa"]).T for c in range(NCORES)], axis=0)
        pif = np.concatenate([np.asarray(r[c]["pif"]).T for c in range(NCORES)], axis=0)
        pg = [np.asarray(r[c]["pg"]) for c in range(NCORES)]
        del r
        ims = [{"qT": np.ascontiguousarray(pa[:, c * 128:(c + 1) * 128].T), "kT": np.ascontiguousarray(pa[:, 1024 + c * 128:1024 + (c + 1) * 128].T),
                "v": np.ascontiguousarray(pa[:, 2048 + c * 128:2048 + (c + 1) * 128])} for c in range(NCORES)]
        r = _run(_get_nc("SB", build_SB), ims)
        ysb = np.concatenate([np.asarray(r[c]["ysb"]).T for c in range(NCORES)], axis=1)
        P5 = [inp[k_][l] for k_ in ('s5_lam_re', 's5_lam_im', 's5_log_dt', 's5_b_re', 's5_b_im', 's5_c_re', 's5_c_im', 's5_d')]
        r = _run(_get_nc("S5", build_S5), [s5_host_inputs(c, pa[:, 3072:4096], *P5) for c in range(NCORES)])
        ys5 = np.concatenate([np.asarray(r[c]["ys5"]).transpose(2, 1, 0).reshape(T, 128) for c in range(NCORES)], axis=1)
        r = _run(_get_nc("ML", build_ML), [ml_host_inputs(c, l, pa[:, 4096:5120], pa[:, 5120:6144], pa[:, 6144:7168], pa[:, 7168:8192],
                                                          pif[:, 0:4], pif[:, 4:8], inp['ml_conv'][l], inp['ml_i_bias'][l], inp['ml_f_bias'][l])
                                           for c in range(NCORES)])
        yml = np.concatenate([np.asarray(r[c]["yml"]).T for c in range(NCORES)], axis=1)
        yall = np.concatenate([ysb, ys5, yml], axis=1)
        del pa, ysb, ys5, yml
        last = (l == DEPTH - 1)
        ims = [_c_inputs(c, l, xT[c], pg[c], np.ascontiguousarray(yall[c * TS:(c + 1) * TS].T), mod_l, inp) for c in range(NCORES)]
        r = _run(_get_nc("C", build_C2, last), ims)
        xT = [np.asarray(r[c]["xnT"]) for c in range(NCORES)]
        if last:
            out = np.concatenate([np.asarray(r[c]["outT"]).T for c in range(NCORES)], axis=0)
        del r, ims
    return np.ascontiguousarray(out[None].astype(np.float32))
```

```python
import contextlib
import numpy as np
import ml_dtypes
import concourse.bass as bass
import concourse.mybir as mybir
from concourse.bass_utils import run_bass_kernel_spmd

F32 = mybir.dt.float32
BF16 = mybir.dt.bfloat16
AF = mybir.ActivationFunctionType
ALU = mybir.AluOpType
AX = mybir.AxisListType

NCORES = 8
D = 2048
T = 16384
DEPTH = 2
IN_COLS = 14344
EPS = 1e-6


class Buf:
    __slots__ = ("name", "w", "r")

    def __init__(self, name):
        self.name = name
        self.w = None
        self.r = {}


class K:
    NDMA = 12

    def __init__(self, nc, stack):
        self.nc = nc
        self.stack = stack
        self.eng = {"pe": nc.tensor, "act": nc.scalar, "dve": nc.vector, "pool": nc.gpsimd, "sp": nc.sync}
        self.sem = {}
        self.cnt = {}
        for e in self.eng:
            self.sem[e] = stack.enter_context(nc.semaphore("s_" + e))
            self.cnt[e] = 0
        self.dsem = [stack.enter_context(nc.semaphore("s_dma%d" % i)) for i in range(self.NDMA)]
        self.dcnt = [0] * self.NDMA
        self.dnext = 0
        self.waited = {e: {} for e in self.eng}
        self.nbuf = 0

    def sb(self, name, shape, dt):
        return self.stack.enter_context(self.nc.sbuf_tensor(name, list(shape), dt))

    def ps(self, name, shape, dt=F32):
        return self.stack.enter_context(self.nc.psum_tensor(name, list(shape), dt))

    def buf(self, name=None):
        self.nbuf += 1
        return Buf(name or "b%d" % self.nbuf)

    def bufs(self, n, name="b"):
        return [self.buf("%s%d" % (name, i)) for i in range(n)]

    def _semof(self, key):
        if isinstance(key, int):
            return self.dsem[key]
        return self.sem[key]

    def _collect(self, e, ins, outs):
        need = {}

        def add(tok):
            if tok is None:
                return
            k, v = tok
            if need.get(k, 0) < v:
                need[k] = v

        for b in ins:
            add(b.w)
        for b in outs:
            add(b.w)
            for k, v in b.r.items():
                if k == e:
                    continue
                add((k, v))
        return need

    def _emit_waits(self, e, need):
        eng = self.eng[e]
        wd = self.waited[e]
        for k, v in need.items():
            if e == "pe" and k == "pe":
                continue
            if wd.get(k, 0) >= v:
                continue
            eng.wait_ge(self._semof(k), v)
            wd[k] = v

    def _finish(self, tok, ins, outs):
        k, v = tok
        for b in ins:
            if b.r.get(k, 0) < v:
                b.r[k] = v
        for b in outs:
            b.w = tok
            b.r = {}

    def op(self, e, fn, ins=(), outs=()):
        need = self._collect(e, ins, outs)
        self._emit_waits(e, need)
        inst = fn(self.eng[e])
        self.cnt[e] += 1
        inst.then_inc(self.sem[e], 1)
        tok = (e, self.cnt[e])
        self._finish(tok, ins, outs)
        return tok

    def dma(self, out_ap, in_ap, ins=(), outs=(), q="sp", **kw):
        i = self.dnext
        self.dnext = (self.dnext + 1) % self.NDMA
        need = self._collect(q, ins, outs)
        if self.dcnt[i] > 0:
            need[i] = max(need.get(i, 0), self.dcnt[i])
        self._emit_waits(q, need)
        inst = self.eng[q].dma_start(out=out_ap, in_=in_ap, **kw)
        self.dcnt[i] += 16
        inst.then_inc(self.dsem[i], 16)
        tok = (i, self.dcnt[i])
        self._finish(tok, ins, outs)
        return tok

    def cc(self, kind, out_ap, in_ap, ins=(), outs=(), groups=None):
        q = "pool"
        i = self.dnext
        self.dnext = (self.dnext + 1) % self.NDMA
        need = self._collect(q, ins, outs)
        if self.dcnt[i] > 0:
            need[i] = max(need.get(i, 0), self.dcnt[i])
        self._emit_waits(q, need)
        inst = self.nc.gpsimd.collective_compute(kind, ALU.bypass, replica_groups=groups or [list(range(NCORES))],
                                                 ins=[in_ap], outs=[out_ap])
        self.dcnt[i] += 16
        inst.then_inc(self.dsem[i], 16)
        tok = (i, self.dcnt[i])
        self._finish(tok, ins, outs)
        return tok

    def barrier(self):
        need = {}
        for e in ("pe", "act", "dve", "pool", "sp"):
            if self.cnt[e]:
                need[e] = self.cnt[e]
        for i in range(self.NDMA):
            if self.dcnt[i]:
                need[i] = self.dcnt[i]
        need.pop("sp", None)
        for e in ("pe", "act", "dve", "pool", "sp"):
            self._emit_waits(e, dict(need))

    def finish(self, out_bufs):
        need = {}
        for b in out_bufs:
            if b.w is not None:
                k, v = b.w
                need[k] = max(need.get(k, 0), v)
        for e in ("pe", "act", "dve", "pool"):
            if self.cnt[e]:
                need[e] = self.cnt[e]
        for i in range(self.NDMA):
            if self.dcnt[i]:
                need[i] = self.dcnt[i]
        self._emit_waits("sp", need)


def new_nc():
    return bass.Bass("TRN2", target_bir_lowering=False)


def _evac(k, idx, out_ap, in_ap, ins, outs):
    if idx % 2 == 0:
        return k.op("act", lambda g: g.activation(out=out_ap, in_=in_ap, func=AF.Identity), ins=ins, outs=outs)
    return k.op("dve", lambda g: g.tensor_copy(out=out_ap, in_=in_ap), ins=ins, outs=outs)


def emit_modnorm(k, xt, bx, hT_ap_fn, bh, vec, bvec, ones_bf, bones, sq, bsq, psb, bpsb, rstd, brstd, tmp, btmp, NT=512):
    k.op("act", lambda g: g.activation(out=sq[:], in_=xt[:], func=AF.Square), ins=[bx], outs=[bsq])
    for kc in range(16):
        k.op("pe", lambda g, kc=kc: g.matmul(psb[:, 0:NT], lhsT=ones_bf[:], rhs=sq[:, kc, :], start=(kc == 0), stop=(kc == 15)),
             ins=[bones, bsq], outs=[bpsb])
    k.op("act", lambda g: g.activation(out=rstd[:], in_=psb[:, 0:NT], func=AF.Sqrt, scale=1.0 / D, bias=EPS), ins=[bpsb], outs=[brstd])
    k.op("dve", lambda g: g.reciprocal(out=rstd[:], in_=rstd[:]), ins=[brstd], outs=[brstd])
    for kc in range(16):
        k.op("dve", lambda g, kc=kc: g.tensor_tensor(out=tmp[:, kc % 2, :], in0=xt[:, kc, :], in1=rstd[:], op=ALU.mult),
             ins=[bx, brstd], outs=[btmp[kc % 2]])
        k.op("act", lambda g, kc=kc: g.activation(out=hT_ap_fn(kc), in_=tmp[:, kc % 2, :], func=AF.Identity,
                                                  scale=vec[:, 0, kc:kc + 1], bias=vec[:, 1, kc:kc + 1]),
             ins=[btmp[kc % 2], bvec], outs=[bh])


def emit_A(k, xT_v, vecs, w_v, pa, pif, pg, bout, xins=lambda tt: (), pfx="A_"):
    NTOK = 2048
    xt = k.sb(pfx + "xt", [128, 16, 512], F32); bx = k.buf()
    sq = k.sb(pfx + "sq", [128, 16, 512], BF16); bsq = k.buf()
    hT = k.sb(pfx + "hT", [128, 16, NTOK], BF16); bh = k.bufs(4, "bh")
    vin = k.sb(pfx + "vin", [128, 3, 16], F32); bvin = k.buf()
    vec = k.sb(pfx + "vec", [128, 2, 16], F32); bvec = k.buf()
    ones_bf = k.sb(pfx + "ones_bf", [128, 128], BF16); bones = k.buf()
    rstd = k.sb(pfx + "rstd", [128, 512], F32); brstd = k.buf()
    tmp = k.sb(pfx + "tmp", [128, 2, 512], F32); btmp = k.bufs(2, "btmp")
    wst = [k.sb(pfx + "wst%d" % i, [128, 16, 128], F32) for i in range(2)]; bwst = k.bufs(2, "bwst")
    wbf = [k.sb(pfx + "wbf%d" % i, [128, 16, 128], BF16) for i in range(2)]; bwbf = k.bufs(2, "bwbf")
    ost = [k.sb(pfx + "ost%d" % i, [128, NTOK], F32) for i in range(2)]; bost = k.bufs(2, "bost")
    ostb = [k.sb(pfx + "ostb%d" % i, [128, NTOK], BF16) for i in range(2)]; bostb = k.bufs(2, "bostb")
    ps = [k.ps(pfx + "ps%d" % i, [128, 512]) for i in range(8)]; bps = k.bufs(8, "bps")

    k.op("pool", lambda g: g.memset(ones_bf[:], 1.0), outs=[bones])
    k.dma(vin[:], vecs, outs=[bvin])
    k.op("dve", lambda g: g.scalar_tensor_tensor(out=vec[:, 0, :], in0=vin[:, 1, :], scalar=1.0, in1=vin[:, 0, :],
                                                  op0=ALU.add, op1=ALU.mult), ins=[bvin], outs=[bvec])
    k.op("dve", lambda g: g.tensor_copy(out=vec[:, 1, :], in_=vin[:, 2, :]), ins=[bvin, bvec], outs=[bvec])
    for tt in range(4):
        k.dma(xt[:], xT_v[:, :, tt * 512:(tt + 1) * 512], ins=list(xins(tt)), outs=[bx])
        emit_modnorm(k, xt, bx, lambda kc, tt=tt: hT[:, kc, tt * 512:(tt + 1) * 512], bh[tt], vec, bvec, ones_bf, bones,
                     sq, bsq, ps[0], bps[0], rstd, brstd, tmp, btmp)
    tiles = [(n0, 128) for n0 in range(0, 8192, 128)] + [(8192, 8)] + [(n0, 128) for n0 in range(8200, IN_COLS, 128)]
    for ti, (n0, ncol) in enumerate(tiles):
        s = ti % 2
        k.dma(wst[s][:, :, 0:ncol], w_v[:, :, n0:n0 + ncol], outs=[bwst[s]])
        eng = ("dve", "act")[ti % 2]
        if eng == "act":
            k.op("act", lambda g, s=s, ncol=ncol: g.activation(out=wbf[s][:, :, 0:ncol], in_=wst[s][:, :, 0:ncol], func=AF.Identity),
                 ins=[bwst[s]], outs=[bwbf[s]])
        else:
            k.op(eng, lambda g, s=s, ncol=ncol: g.tensor_copy(out=wbf[s][:, :, 0:ncol], in_=wst[s][:, :, 0:ncol]),
                 ins=[bwst[s]], outs=[bwbf[s]])
        is_bf = n0 < 8192
        o_t, o_b = (ostb[s], bostb[s]) if is_bf else (ost[s], bost[s])
        for tt in range(4):
            pi = (ti % 2) * 4 + tt
            for kc in range(16):
                k.op("pe", lambda g, pi=pi, s=s, kc=kc, tt=tt, ncol=ncol: g.matmul(
                    ps[pi][0:ncol, :], lhsT=wbf[s][:, kc, 0:ncol], rhs=hT[:, kc, tt * 512:(tt + 1) * 512],
                    start=(kc == 0), stop=(kc == 15)), ins=[bwbf[s], bh[tt]], outs=[bps[pi]])
            _evac(k, tt, o_t[0:ncol, tt * 512:(tt + 1) * 512], ps[pi][0:ncol, :], [bps[pi]], [o_b])
        if is_bf:
            dst = pa[n0:n0 + ncol, :]
        elif ncol == 8:
            dst = pif[:, :]
        else:
            dst = pg[n0 - 8200:n0 - 8200 + ncol, :]
        k.dma(dst, o_t[0:ncol, :], ins=[o_b], outs=[bout], q="pool")


def build_A():
    NTOK = 2048
    nc = new_nc()
    xT = nc.dram_tensor("xT", [D, NTOK], F32, kind="ExternalInput").ap()
    vecs = nc.dram_tensor("vecs", [128, 3, 16], F32, kind="ExternalInput").ap()
    w = nc.dram_tensor("w", [D, IN_COLS], F32, kind="ExternalInput").ap()
    pa = nc.dram_tensor("pa", [8192, NTOK], BF16, kind="ExternalOutput").ap()
    pif = nc.dram_tensor("pif", [8, NTOK], F32, kind="ExternalOutput").ap()
    pg = nc.dram_tensor("pg", [6144, NTOK], F32, kind="ExternalOutput").ap()
    with contextlib.ExitStack() as st:
        k = K(nc, st)
        bout = k.buf("out")
        emit_A(k, xT.rearrange("(kc p) t -> p kc t", p=128), vecs, w.rearrange("(kc p) n -> p kc n", p=128), pa, pif, pg, bout)
        k.finish([bout])
    return nc


def emit_sb(k, qT, bq, kT, bk, vS, bv, y_dram, by, tag="sb"):
    NQ = T // 512
    SC = 128.0 ** -0.5
    tri = k.sb(tag + "tri", [128, 128], BF16); omt = k.sb(tag + "omt", [128, 128], BF16); bconst = k.buf()
    onesf = k.sb(tag + "onesf", [128, 512], F32)
    masks = k.sb(tag + "masks", [128, 4, 512], F32)
    k.op("pool", lambda g: g.memset(onesf[:], 1.0), outs=[bconst])
    k.op("pool", lambda g: g.affine_select(out=tri[:], in_=onesf[:, 0:128], pattern=[[-1, 128]], compare_op=ALU.is_ge,
                                           fill=0.0, base=0, channel_multiplier=1), ins=[bconst], outs=[bconst])
    k.op("pool", lambda g: g.affine_select(out=omt[:], in_=onesf[:, 0:128], pattern=[[1, 128]], compare_op=ALU.is_gt,
                                           fill=0.0, base=0, channel_multiplier=-1), ins=[bconst], outs=[bconst])
    for j in range(4):
        k.op("pool", lambda g, j=j: g.affine_select(out=masks[:, j, :], in_=onesf[:], pattern=[[1, 512]], compare_op=ALU.is_gt,
                                                    fill=0.0, base=-128 * j, channel_multiplier=-1), ins=[bconst], outs=[bconst])

    class Stream:
        pass

    streams = []
    for si in range(2):
        s = Stream()
        s.z = k.ps("%sz%d" % (tag, si), [128, 512]); s.bz = k.buf()
        s.p = k.ps("%sp%d" % (tag, si), [128, 512]); s.bp = k.buf()
        s.y = [k.ps("%sy%d_%d" % (tag, si, i), [128, 512]) for i in range(2)]; s.by = k.bufs(2)
        s.u = [k.sb("%su%d_%d" % (tag, si, i), [128, 512], F32) for i in range(2)]; s.bu = k.bufs(2)
        s.sp = [k.sb("%ssp%d_%d" % (tag, si, i), [128, 512], BF16) for i in range(2)]; s.bsp = k.bufs(2)
        s.ec = [k.sb("%sec%d_%d" % (tag, si, i), [128, 512], BF16) for i in range(2)]; s.bec = k.bufs(2)
        s.w = [k.sb("%sw%d_%d" % (tag, si, i), [128, 512], BF16) for i in range(2)]; s.bw = k.bufs(2)
        s.yo = [k.sb("%syo%d_%d" % (tag, si, i), [128, 512], BF16) for i in range(2)]; s.byo = k.bufs(2)
        s.steps = []
        for qt in range(si, NQ, 2):
            nb = 4 * qt + 4
            for i, b in enumerate(range(nb - 1, -1, -1)):
                s.steps.append((qt, b, i == 0, i == nb - 1))
        streams.append(s)

    def st_z(s, n):
        qt, b, first, last = s.steps[n]
        k.op("pe", lambda g: g.matmul(s.z[:], lhsT=kT[:, b * 128:(b + 1) * 128], rhs=qT[:, qt * 512:(qt + 1) * 512],
                                      start=True, stop=True), ins=[bk, bq], outs=[s.bz])

    def st_u(s, n):
        qt, b, first, last = s.steps[n]
        i = n % 2
        k.op("act", lambda g: g.activation(out=s.u[i][:], in_=s.z[:], func=AF.Exp, scale=SC), ins=[s.bz], outs=[s.bu[i]])
        j = b - 4 * qt
        if j >= 0:
            k.op("dve", lambda g: g.tensor_tensor(out=s.u[i][:], in0=s.u[i][:], in1=masks[:, j, :], op=ALU.mult),
                 ins=[s.bu[i], bconst], outs=[s.bu[i]])

    def st_sp(s, n):
        i = n % 2
        k.op("act", lambda g: g.activation(out=s.sp[i][:], in_=s.u[i][:], func=AF.Ln, bias=1.0), ins=[s.bu[i]], outs=[s.bsp[i]])

    def st_tri(s, n):
        qt, b, first, last = s.steps[n]
        i = n % 2
        k.op("pe", lambda g: g.matmul(s.p[:], lhsT=tri[:], rhs=s.sp[i][:], start=first, stop=True, skip_group_check=True),
             ins=[bconst, s.bsp[i]], outs=[s.bp])

    def st_ec(s, n):
        i = n % 2
        k.op("act", lambda g: g.activation(out=s.ec[i][:], in_=s.p[:], func=AF.Exp, scale=-1.0), ins=[s.bp], outs=[s.bec[i]])

    def st_omt(s, n):
        qt, b, first, last = s.steps[n]
        i = n % 2
        if not last:
            k.op("pe", lambda g: g.matmul(s.p[:], lhsT=omt[:], rhs=s.sp[i][:], start=False, stop=True, skip_group_check=True),
                 ins=[bconst, s.bsp[i]], outs=[s.bp])

    def st_w(s, n):
        i = n % 2
        k.op("dve", lambda g: g.tensor_tensor(out=s.w[i][:], in0=s.u[i][:], in1=s.ec[i][:], op=ALU.mult),
             ins=[s.bu[i], s.bec[i]], outs=[s.bw[i]])

    def st_wv(s, n):
        qt, b, first, last = s.steps[n]
        i = n % 2
        yi = (qt // 2) % 2
        k.op("pe", lambda g: g.matmul(s.y[yi][:], lhsT=vS[:, b, :], rhs=s.w[i][:], start=first, stop=last),
             ins=[bv, s.bw[i]], outs=[s.by[yi]])
        if last:
            k.op("act", lambda g: g.activation(out=s.yo[yi][:], in_=s.y[yi][:], func=AF.Identity), ins=[s.by[yi]], outs=[s.byo[yi]])
            k.dma(y_dram[:, qt * 512:(qt + 1) * 512], s.yo[yi][:], ins=[s.byo[yi]], outs=[by], q="pool")

    nmax = max(len(s.steps) for s in streams)
    for s in streams:
        st_z(s, 0)
    for n in range(nmax):
        act = [s for s in streams if n < len(s.steps)]
        for s in act:
            st_u(s, n)
        for s in act:
            st_sp(s, n)
        for s in act:
            st_tri(s, n)
        for s in act:
            if n + 1 < len(s.steps):
                st_z(s, n + 1)
        for s in act:
            st_ec(s, n)
        for s in act:
            st_omt(s, n)
        for s in act:
            st_w(s, n)
        for s in act:
            st_wv(s, n)


def build_SB():
    nc = new_nc()
    qTd = nc.dram_tensor("qT", [128, T], BF16, kind="ExternalInput").ap()
    kTd = nc.dram_tensor("kT", [128, T], BF16, kind="ExternalInput").ap()
    vd = nc.dram_tensor("v", [T, 128], BF16, kind="ExternalInput").ap()
    yd = nc.dram_tensor("ysb", [128, T], BF16, kind="ExternalOutput").ap()
    with contextlib.ExitStack() as st:
        k = K(nc, st)
        qT = k.sb("qTs", [128, T], BF16); bq = k.buf()
        kT = k.sb("kTs", [128, T], BF16); bk = k.buf()
        vS = k.sb("vS", [128, T // 128, 128], BF16); bv = k.buf()
        by = k.buf()
        for i in range(4):
            sl = slice(i * 4096, (i + 1) * 4096)
            k.dma(qT[:, sl], qTd[:, sl], outs=[bq]); k.dma(kT[:, sl], kTd[:, sl], outs=[bk])
            k.dma(vS[:, i * 32:(i + 1) * 32, :], vd.rearrange("(b p) d -> p b d", p=128)[:, i * 32:(i + 1) * 32, :], outs=[bv])
        emit_sb(k, qT, bq, kT, bk, vS, bv, yd, by)
        k.finish([by])
    return nc


def emit_ml(k, mqk_d, mv_d, mo_d, gif_d, cw_d, gb_d, y_dram, by, tag="ml"):
    TB = 2048
    NBLK = T // TB
    CPB = TB // 128
    LN16 = float(np.log(1.0 / 16.0))
    identf = k.sb(tag + "identf", [128, 128], F32); onesf = k.sb(tag + "onesf", [128, 128], F32)
    trile = k.sb(tag + "trile", [128, 128], F32); negmask = k.sb(tag + "negmask", [128, 128], F32)
    onesbf = k.sb(tag + "onesbf", [128, 128], BF16); identbf = k.sb(tag + "identbf", [128, 128], BF16)
    zerosf = k.sb(tag + "zerosf", [128, 128], F32)
    bc = k.buf()
    k.op("pool", lambda g: g.memset(onesf[:], 1.0), outs=[bc])
    k.op("pool", lambda g: g.memset(zerosf[:], 0.0), ins=[bc], outs=[bc])
    k.op("pool", lambda g: g.memset(onesbf[:], 1.0), ins=[bc], outs=[bc])
    k.op("pool", lambda g: g.affine_select(out=identf[:], in_=onesf[:], pattern=[[-1, 128]], compare_op=ALU.is_equal,
                                           fill=0.0, base=0, channel_multiplier=1), ins=[bc], outs=[bc])
    k.op("pool", lambda g: g.tensor_copy(out=identbf[:], in_=identf[:]), ins=[bc], outs=[bc])
    k.op("pool", lambda g: g.affine_select(out=trile[:], in_=onesf[:], pattern=[[1, 128]], compare_op=ALU.is_ge,
                                           fill=0.0, base=0, channel_multiplier=-1), ins=[bc], outs=[bc])
    k.op("pool", lambda g: g.affine_select(out=negmask[:], in_=zerosf[:], pattern=[[1, 128]], compare_op=ALU.is_ge,
                                           fill=-30000.0, base=0, channel_multiplier=-1), ins=[bc], outs=[bc])
    gif = k.sb(tag + "gif", [128, 2, 128], F32); bgif = k.buf()
    cw = k.sb(tag + "cw", [128, 4, 4], F32); bcw = k.buf()
    gb = k.sb(tag + "gb", [128, 2], F32); bgb = k.buf()
    k.dma(gif[:], gif_d, outs=[bgif]); k.dma(cw[:], cw_d, outs=[bcw]); k.dma(gb[:], gb_d, outs=[bgb])
    psA = k.ps(tag + "psA", [128, 512]); bpsA = k.buf()
    psB = k.ps(tag + "psB", [128, 512]); bpsB = k.buf()
    Eps = k.ps(tag + "Eps", [128, 512]); bEps = k.buf()
    Sps = k.ps(tag + "Sps", [128, 512]); bSps = k.buf()
    NDps = k.ps(tag + "NDps", [128, 512]); bND = k.buf()
    ktps = k.ps(tag + "ktps", [128, 1024], BF16); bktps = k.buf()
    dCps = k.ps(tag + "dCps", [128, 2, 256]); bdC = k.buf()
    sc = k.sb(tag + "sc", [128, 4], F32); bsc = k.buf()
    k.op("dve", lambda g: g.tensor_scalar(out=sc[:, 0:1], in0=gb[:, 1:2], scalar1=-1.0, scalar2=None, op0=ALU.mult), ins=[bgb], outs=[bsc])
    k.op("dve", lambda g: g.tensor_scalar(out=sc[:, 1:2], in0=gb[:, 0:1], scalar1=LN16, scalar2=None, op0=ALU.add), ins=[bgb, bsc], outs=[bsc])
    lfneg = k.sb(tag + "lfneg", [128, 128], F32); blf = k.buf()
    k.op("act", lambda g: g.activation(out=lfneg[:], in_=gif[:, 1, :], func=AF.Exp, scale=-1.0, bias=sc[:, 0:1]), ins=[bgif, bsc], outs=[blf])
    k.op("act", lambda g: g.activation(out=lfneg[:], in_=lfneg[:], func=AF.Ln, bias=1.0), ins=[blf], outs=[blf])
    k.op("pe", lambda g: g.matmul(psA[:, 0:128], lhsT=trile[:], rhs=lfneg[:], start=True, stop=True), ins=[bc, blf], outs=[bpsA])
    k.op("pe", lambda g: g.matmul(psB[:, 0:128], lhsT=onesf[:], rhs=lfneg[:], start=True, stop=True), ins=[bc, blf], outs=[bpsB])
    imb = k.sb(tag + "imb", [128, 128], F32); negb = k.sb(tag + "negb", [128, 128], F32)
    wa = k.sb(tag + "wa", [128, 128], F32); ebtot = k.sb(tag + "ebtot", [128, 128], F32); bg = k.buf()
    k.op("dve", lambda g: g.scalar_tensor_tensor(out=imb[:], in0=gif[:, 0, :], scalar=sc[:, 1:2], in1=psA[:, 0:128], op0=ALU.add, op1=ALU.add),
         ins=[bgif, bsc, bpsA], outs=[bg])
    k.op("dve", lambda g: g.tensor_scalar(out=negb[:], in0=psA[:, 0:128], scalar1=-1.0, scalar2=None, op0=ALU.mult), ins=[bpsA, bg], outs=[bg])
    k.op("dve", lambda g: g.tensor_tensor(out=wa[:], in0=imb[:], in1=psB[:, 0:128], op=ALU.subtract), ins=[bg, bpsB], outs=[bg])
    k.op("act", lambda g: g.activation(out=wa[:], in_=wa[:], func=AF.Exp), ins=[bg], outs=[bg])
    k.op("act", lambda g: g.activation(out=ebtot[:], in_=psB[:, 0:128], func=AF.Exp, scale=-1.0), ins=[bpsB, bg], outs=[bg])
    Cst = k.sb(tag + "Cst", [128, 2, 129], F32); bCst = k.buf()
    C0bf = k.sb(tag + "C0bf", [128, 2, 128], BF16); n0bc = k.sb(tag + "n0bc", [128, 2, 128], BF16); bC0 = k.buf()
    k.op("pool", lambda g: g.memset(Cst[:], 0.0), outs=[bCst])
    k.op("pool", lambda g: g.memset(C0bf[:], 0.0), outs=[bC0])
    k.op("pool", lambda g: g.memset(n0bc[:], 0.0), ins=[bC0], outs=[bC0])
    xin = [k.sb("%sxin%d" % (tag, i), [128, 4, TB + 4], BF16) for i in range(2)]; bxin = k.bufs(2)
    acc = k.sb(tag + "acc", [128, TB], F32); bacc = k.buf()
    qk = [k.sb("%sqk%d" % (tag, i), [128, 4, TB], BF16) for i in range(2)]; bqk = k.bufs(2)
    vaug = [k.sb("%svaug%d" % (tag, i), [128, CPB, 129], BF16) for i in range(2)]; bva = k.bufs(2)
    mo = [k.sb("%smo%d" % (tag, i), [128, TB], BF16) for i in range(2)]; bmo = k.bufs(2)
    sigo = [k.sb("%ssigo%d" % (tag, i), [128, TB], F32) for i in range(2)]; bsigo = k.bufs(2)
    yb = [k.sb("%syb%d" % (tag, i), [128, TB], BF16) for i in range(2)]; byb = k.bufs(2)
    diagb = [k.sb("%sdiagb%d" % (tag, i), [128, 128], F32) for i in range(2)]; bdiag = k.bufs(2)
    Dt = k.sb(tag + "Dt", [128, 128], F32); bDt = k.buf()
    ebb = k.sb(tag + "ebb", [128, 128], F32); bebb = k.buf()
    Pt = k.sb(tag + "Pt", [128, 128], BF16); bPt = k.buf()
    qtl = k.sb(tag + "qtl", [128, 2, 128], BF16); bqtl = k.buf()
    kt = k.sb(tag + "kt", [128, 256], BF16); bkt = k.buf()
    dn = k.sb(tag + "dn", [128, 128], F32); bdn = k.buf()
    hh = k.sb(tag + "hh", [128, 128], F32); bhh = k.buf()
    for i in range(2):
        k.op("pool", lambda g, i=i: g.memset(vaug[i][:], 1.0), outs=[bva[i]])
    mqk_v = mqk_d.rearrange("(x p) t -> p x t", p=128)
    mv_v = mv_d.rearrange("(c l) d -> l c d", l=128)
    for blk in range(NBLK):
        s = blk % 2
        t0 = blk * TB
        if blk == 0:
            k.op("pool", lambda g: g.memset(xin[s][:, :, 0:4], 0.0), outs=[bxin[s]])
            k.dma(xin[s][:, :, 4:4 + TB], mqk_v[:, :, 0:TB], outs=[bxin[s]])
        else:
            k.dma(xin[s][:, :, 0:4 + TB], mqk_v[:, :, t0 - 4:t0 + TB], outs=[bxin[s]])
        k.dma(vaug[s][:, :, 0:128], mv_v[:, blk * CPB:(blk + 1) * CPB, :], outs=[bva[s]])
        k.dma(mo[s][:], mo_d[:, t0:t0 + TB], outs=[bmo[s]])
        k.op("act", lambda g: g.activation(out=sigo[s][:], in_=mo[s][:], func=AF.Sigmoid), ins=[bmo[s]], outs=[bsigo[s]])
        for X in range(4):
            k.op("dve", lambda g: g.tensor_scalar(out=acc[:], in0=xin[s][:, X, 4:4 + TB], scalar1=cw[:, X, 3:4], scalar2=None, op0=ALU.mult),
                 ins=[bxin[s], bcw], outs=[bacc])
            for j in (2, 1, 0):
                k.op("dve", lambda g, j=j: g.scalar_tensor_tensor(out=acc[:], in0=xin[s][:, X, 1 + j:1 + j + TB], scalar=cw[:, X, j:j + 1],
                                                                  in1=acc[:], op0=ALU.mult, op1=ALU.add), ins=[bxin[s], bcw, bacc], outs=[bacc])
            k.op("act", lambda g: g.activation(out=qk[s][:, X, :], in_=acc[:], func=AF.Silu), ins=[bacc], outs=[bqk[s]])
        for ci in range(CPB):
            cg = blk * CPB + ci
            csl = slice(ci * 128, (ci + 1) * 128)
            d = cg % 2
            k.op("dve", lambda g: g.tensor_scalar(out=diagb[d][:], in0=identf[:], scalar1=negb[:, cg:cg + 1], scalar2=None, op0=ALU.mult),
                 ins=[bc, bg], outs=[bdiag[d]])
            k.op("pe", lambda g: g.matmul(Eps[:, 0:128], lhsT=onesf[:], rhs=diagb[d][:], start=True, stop=False), ins=[bc, bdiag[d]], outs=[bEps])
            k.op("pe", lambda g: g.matmul(Eps[:, 0:128], lhsT=identf[:], rhs=negmask[:], start=False, stop=True), ins=[bc], outs=[bEps])
            k.op("pe", lambda g: g.matmul(Eps[:, 128:256], lhsT=onesf[:], rhs=diagb[d][:], start=True, stop=True), ins=[bc, bdiag[d]], outs=[bEps])
            k.op("act", lambda g: g.activation(out=Dt[:], in_=Eps[:, 0:128], func=AF.Exp, bias=imb[:, cg:cg + 1]), ins=[bEps, bg], outs=[bDt])
            k.op("act", lambda g: g.activation(out=ebb[:], in_=Eps[:, 128:256], func=AF.Exp), ins=[bEps], outs=[bebb])
            for dk in range(2):
                k.op("pe", lambda g, dk=dk: g.matmul(Sps[:, 0:128], lhsT=qk[s][:, 2 + dk, csl], rhs=qk[s][:, dk, csl], start=(dk == 0), stop=(dk == 1)),
                     ins=[bqk[s]], outs=[bSps])
            k.op("dve", lambda g: g.tensor_tensor(out=Pt[:], in0=Sps[:, 0:128], in1=Dt[:], op=ALU.mult), ins=[bSps, bDt], outs=[bPt])
            for dk in range(2):
                k.op("dve", lambda g, dk=dk: g.tensor_tensor(out=qtl[:, dk, :], in0=qk[s][:, dk, csl], in1=ebb[:], op=ALU.mult),
                     ins=[bqk[s], bebb], outs=[bqtl])
            k.op("pe", lambda g: g.matmul(NDps[:, 0:128], lhsT=vaug[s][:, ci, 0:128], rhs=Pt[:], start=True, stop=False), ins=[bva[s], bPt], outs=[bND])
            for dk in range(2):
                k.op("pe", lambda g, dk=dk: g.matmul(NDps[:, 0:128], lhsT=C0bf[:, dk, :], rhs=qtl[:, dk, :], start=False, stop=(dk == 1)),
                     ins=[bC0, bqtl], outs=[bND])
            k.op("pe", lambda g: g.matmul(NDps[:, 128:256], lhsT=onesbf[:], rhs=Pt[:], start=True, stop=False), ins=[bc, bPt], outs=[bND])
            for dk in range(2):
                k.op("pe", lambda g, dk=dk: g.matmul(NDps[:, 128:256], lhsT=n0bc[:, dk, :], rhs=qtl[:, dk, :], start=False, stop=(dk == 1)),
                     ins=[bC0, bqtl], outs=[bND])
            k.op("act", lambda g: g.activation(out=dn[:], in_=NDps[:, 128:256], func=AF.Abs), ins=[bND], outs=[bdn])
            k.op("dve", lambda g: g.tensor_scalar(out=dn[:], in0=dn[:], scalar1=1.0, scalar2=None, op0=ALU.max), ins=[bdn], outs=[bdn])
            k.op("dve", lambda g: g.reciprocal(out=dn[:], in_=dn[:]), ins=[bdn], outs=[bdn])
            k.op("dve", lambda g: g.tensor_tensor(out=hh[:], in0=NDps[:, 0:128], in1=dn[:], op=ALU.mult), ins=[bND, bdn], outs=[bhh])
            k.op("dve", lambda g: g.tensor_tensor(out=yb[s][:, csl], in0=hh[:], in1=sigo[s][:, csl], op=ALU.mult), ins=[bhh, bsigo[s]], outs=[byb[s]])
            for dk in range(2):
                k.op("pe", lambda g, dk=dk: g.transpose(ktps[:, dk * 128:(dk + 1) * 128], qk[s][:, 2 + dk, csl], identbf[:]),
                     ins=[bqk[s], bc], outs=[bktps])
            k.op("dve", lambda g: g.tensor_scalar(out=kt[:], in0=ktps[:, 0:256], scalar1=wa[:, cg:cg + 1], scalar2=None, op0=ALU.mult),
                 ins=[bktps, bg], outs=[bkt])
            for dk in range(2):
                k.op("pe", lambda g, dk=dk: g.matmul(dCps[:, dk, 0:129], lhsT=kt[:, dk * 128:(dk + 1) * 128], rhs=vaug[s][:, ci, :], start=True, stop=True),
                     ins=[bkt, bva[s]], outs=[bdC])
            for dk in range(2):
                k.op("dve", lambda g, dk=dk: g.scalar_tensor_tensor(out=Cst[:, dk, :], in0=Cst[:, dk, :], scalar=ebtot[:, cg:cg + 1], in1=dCps[:, dk, 0:129],
                                                                    op0=ALU.mult, op1=ALU.add), ins=[bCst, bg, bdC], outs=[bCst])
            k.op("pool", lambda g: g.tensor_copy(out=C0bf[:], in_=Cst[:, :, 0:128]), ins=[bCst], outs=[bC0])
            for dk in range(2):
                k.op("dve", lambda g, dk=dk: g.tensor_scalar(out=n0bc[:, dk, :], in0=onesbf[:], scalar1=Cst[:, dk, 128:129], scalar2=None, op0=ALU.mult),
                     ins=[bc, bCst, bC0], outs=[bC0])
        k.dma(y_dram[:, t0:t0 + TB], yb[s][:], ins=[byb[s]], outs=[by], q="pool")


def build_ML():
    nc = new_nc()
    mqk_d = nc.dram_tensor("mqk", [512, T], BF16, kind="ExternalInput").ap()
    mv_d = nc.dram_tensor("mv", [T, 128], BF16, kind="ExternalInput").ap()
    mo_d = nc.dram_tensor("mo", [128, T], BF16, kind="ExternalInput").ap()
    gif_d = nc.dram_tensor("gif", [128, 2, 128], F32, kind="ExternalInput").ap()
    cw_d = nc.dram_tensor("cw", [128, 4, 4], F32, kind="ExternalInput").ap()
    gb_d = nc.dram_tensor("gb", [128, 2], F32, kind="ExternalInput").ap()
    yd = nc.dram_tensor("yml", [128, T], BF16, kind="ExternalOutput").ap()
    with contextlib.ExitStack() as st:
        k = K(nc, st)
        by = k.buf()
        emit_ml(k, mqk_d, mv_d, mo_d, gif_d, cw_d, gb_d, yd, by)
        k.finish([by])
    return nc


def build_MIX():
    nc = new_nc()
    qTd = nc.dram_tensor("qT", [128, T], BF16, kind="ExternalInput").ap()
    kTd = nc.dram_tensor("kT", [128, T], BF16, kind="ExternalInput").ap()
    vd = nc.dram_tensor("v", [T, 128], BF16, kind="ExternalInput").ap()
    ysb_d = nc.dram_tensor("ysb", [128, T], BF16, kind="ExternalOutput").ap()
    u16_d = nc.dram_tensor("u16", [16, 8, T], BF16, kind="ExternalInput").ap()
    lamst_d = nc.dram_tensor("lamst", [128, 3, 8], F32, kind="ExternalInput").ap()
    bst_d = nc.dram_tensor("bst", [128, 8, 16], F32, kind="ExternalInput").ap()
    cst_d = nc.dram_tensor("cst", [128, 8, 16], F32, kind="ExternalInput").ap()
    dsk_d = nc.dram_tensor("dsk", [16, 8], F32, kind="ExternalInput").ap()
    ys5_d = nc.dram_tensor("ys5", [16, 8, T], BF16, kind="ExternalOutput").ap()
    mqk_d = nc.dram_tensor("mqk", [512, T], BF16, kind="ExternalInput").ap()
    mv_d = nc.dram_tensor("mv", [T, 128], BF16, kind="ExternalInput").ap()
    mo_d = nc.dram_tensor("mo", [128, T], BF16, kind="ExternalInput").ap()
    gif_d = nc.dram_tensor("gif", [128, 2, 128], F32, kind="ExternalInput").ap()
    cw_d = nc.dram_tensor("cw", [128, 4, 4], F32, kind="ExternalInput").ap()
    gb_d = nc.dram_tensor("gb", [128, 2], F32, kind="ExternalInput").ap()
    yml_d = nc.dram_tensor("yml", [128, T], BF16, kind="ExternalOutput").ap()
    with contextlib.ExitStack() as st:
        k = K(nc, st)
        by = k.buf()
        outer = k.stack
        with contextlib.ExitStack() as p1:
            k.stack = p1
            qT = k.sb("qTs", [128, T], BF16); bq = k.buf()
            kT = k.sb("kTs", [128, T], BF16); bk = k.buf()
            vS = k.sb("vS", [128, T // 128, 128], BF16); bv = k.buf()
            for i in range(4):
                sl = slice(i * 4096, (i + 1) * 4096)
                k.dma(qT[:, sl], qTd[:, sl], outs=[bq]); k.dma(kT[:, sl], kTd[:, sl], outs=[bk])
                k.dma(vS[:, i * 32:(i + 1) * 32, :], vd.rearrange("(b p) d -> p b d", p=128)[:, i * 32:(i + 1) * 32, :], outs=[bv])
            emit_sb(k, qT, bq, kT, bk, vS, bv, ysb_d, by)
            k.barrier()
        k.stack = outer
        with contextlib.ExitStack() as p2:
            k.stack = p2
            emit_ml(k, mqk_d, mv_d, mo_d, gif_d, cw_d, gb_d, yml_d, by)
            k.barrier()
        k.stack = outer
        with contextlib.ExitStack() as p3:
            k.stack = p3
            emit_s5(k, u16_d, lamst_d, bst_d, cst_d, dsk_d, ys5_d, by)
            k.barrier()
        k.stack = outer
        k.finish([by])
    return nc


def ml_host_inputs(c, l, mq, mk, mv, mo, mi, mf, ml_conv, ml_i_bias, ml_f_bias):
    bf = ml_dtypes.bfloat16
    hd, vh = c // 2, c % 2
    q = mq[:, hd * 256:(hd + 1) * 256]; kk = mk[:, hd * 256:(hd + 1) * 256]
    mqk = np.concatenate([q.T, kk.T], axis=0).astype(bf)
    v = mv[:, hd * 256 + vh * 128: hd * 256 + (vh + 1) * 128].astype(bf)
    o = mo[:, hd * 256 + vh * 128: hd * 256 + (vh + 1) * 128].T.astype(bf)
    gi = mi[:, hd].reshape(128, 128).T; gf = mf[:, hd].reshape(128, 128).T
    gif = np.stack([gi, gf], axis=1).astype(np.float32)
    cwq = ml_conv[:, hd * 256:(hd + 1) * 256]; cwk = ml_conv[:, 1024 + hd * 256:1024 + (hd + 1) * 256]
    cw = np.stack([cwq[:, 0:128].T, cwq[:, 128:256].T, cwk[:, 0:128].T, cwk[:, 128:256].T], axis=1).astype(np.float32)
    gb = np.tile(np.array([[ml_i_bias[hd], ml_f_bias[hd]]], np.float32), (128, 1))
    return {"mqk": np.ascontiguousarray(mqk), "mv": np.ascontiguousarray(v), "mo": np.ascontiguousarray(o),
            "gif": np.ascontiguousarray(gif), "cw": np.ascontiguousarray(cw), "gb": gb}


def emit_s5(k, u16_d, lamst_d, bst_d, cst_d, dsk_d, y_dram, by, tag="s5"):
    L = 16
    NBLK = 1024
    NB = T // NBLK
    CPB = NBLK // L
    NC = T // L
    NLEV = 10
    PI = float(np.pi)
    identf = k.sb(tag + "identf", [128, 128], F32); onesf = k.sb(tag + "onesf", [128, 128], F32)
    swap = k.sb(tag + "swap", [128, 128], F32); sw2 = k.sb(tag + "sw2", [128, 128], F32)
    sgn = k.sb(tag + "sgn", [128, 1], F32)
    bc = k.buf()
    k.op("pool", lambda g: g.memset(onesf[:], 1.0), outs=[bc])
    k.op("pool", lambda g: g.affine_select(out=identf[:], in_=onesf[:], pattern=[[-1, 128]], compare_op=ALU.is_equal,
                                           fill=0.0, base=0, channel_multiplier=1), ins=[bc], outs=[bc])
    k.op("pool", lambda g: g.affine_select(out=swap[:], in_=onesf[:], pattern=[[-1, 128]], compare_op=ALU.is_equal,
                                           fill=0.0, base=64, channel_multiplier=1), ins=[bc], outs=[bc])
    k.op("pool", lambda g: g.affine_select(out=sw2[:], in_=onesf[:], pattern=[[-1, 128]], compare_op=ALU.is_equal,
                                           fill=0.0, base=-64, channel_multiplier=1), ins=[bc], outs=[bc])
    k.op("pool", lambda g: g.tensor_tensor(out=swap[:], in0=swap[:], in1=sw2[:], op=ALU.add), ins=[bc], outs=[bc])
    k.op("pool", lambda g: g.memset(sgn[0:64, :], 1.0), ins=[bc], outs=[bc])
    k.op("pool", lambda g: g.memset(sgn[64:128, :], -1.0), ins=[bc], outs=[bc])
    pat01 = k.sb(tag + "pat01", [128, NBLK // L, L], F32)
    k.op("pool", lambda g: g.memset(pat01[:], 1.0), ins=[bc], outs=[bc])
    k.op("pool", lambda g: g.memset(pat01[:, :, 0:1], 0.0), ins=[bc], outs=[bc])
    lamst = k.sb(tag + "lamst", [128, 3, 8], F32); bst = k.sb(tag + "bst", [128, 8, 16], F32)
    cst = k.sb(tag + "cst", [128, 8, 16], F32); dsk = k.sb(tag + "dsk", [16, 8], F32)
    bpar = k.buf()
    k.dma(lamst[:], lamst_d, outs=[bpar]); k.dma(bst[:], bst_d, outs=[bpar]); k.dma(cst[:], cst_d, outs=[bpar]); k.dma(dsk[:], dsk_d, outs=[bpar])
    k.op("dve", lambda g: g.tensor_scalar(out=cst[64:128], in0=cst[64:128], scalar1=-1.0, scalar2=None, op0=ALU.mult), ins=[bpar], outs=[bpar])
    NTAB = 24
    tab = k.sb(tag + "tab", [128, NTAB, 8], F32); bt = k.buf()
    tmp = k.sb(tag + "tmpt", [128, 8, 8], F32)
    (I_DT, I_ER, I_ANG, I_SIN, I_COS, I_LR, I_LI, I_KR, I_KI, I_T0, I_T1, I_T2, I_T3) = range(13)
    tv = lambda i: tab[:, i, :]
    dv = lambda fn, **kw: k.op("dve", fn, ins=[bt, bpar, bc], outs=[bt])
    av = lambda fn: k.op("act", fn, ins=[bt, bpar], outs=[bt])
    TT = lambda o, a, b, op: dv(lambda g: g.tensor_tensor(out=o, in0=a, in1=b, op=op))
    TS = lambda o, a, s1, op0, s2=None, op1=None: dv(lambda g: g.tensor_scalar(out=o, in0=a, scalar1=s1, scalar2=s2, op0=op0, **({"op1": op1} if op1 is not None else {})))

    def cmul(o_r, o_i, ar, ai, br, bi, t1, t2):
        TT(t1, ar, br, ALU.mult); TT(t2, ai, bi, ALU.mult); TT(o_r, t1, t2, ALU.subtract)
        TT(t1, ar, bi, ALU.mult); TT(t2, ai, br, ALU.mult); TT(o_i, t1, t2, ALU.add)

    av(lambda g: g.activation(out=tv(I_DT), in_=lamst[:, 2, :], func=AF.Exp))
    TT(tv(I_T0), lamst[:, 0, :], tv(I_DT), ALU.mult)
    av(lambda g: g.activation(out=tv(I_ER), in_=tv(I_T0), func=AF.Exp))
    TT(tv(I_ANG), lamst[:, 1, :], tv(I_DT), ALU.mult)

    def rr(dst, src, shift):
        TS(dst, src, shift, ALU.add)
        for _ in range(8):
            TS(tv(I_T1), dst, PI, ALU.is_gt)
            dv(lambda g: g.scalar_tensor_tensor(out=dst, in0=tv(I_T1), scalar=-2.0 * PI, in1=dst, op0=ALU.mult, op1=ALU.add))
        TS(dst, dst, -PI, ALU.max)
        TS(dst, dst, PI, ALU.min)

    rr(tv(I_T2), tv(I_ANG), 0.0)
    av(lambda g: g.activation(out=tv(I_SIN), in_=tv(I_T2), func=AF.Sin))
    rr(tv(I_T2), tv(I_ANG), PI / 2)
    av(lambda g: g.activation(out=tv(I_COS), in_=tv(I_T2), func=AF.Sin))
    TT(tv(I_LR), tv(I_ER), tv(I_COS), ALU.mult)
    TT(tv(I_LI), tv(I_ER), tv(I_SIN), ALU.mult)
    TS(tv(I_T0), tv(I_LR), -1.0, ALU.add)
    TT(tv(I_T1), lamst[:, 0, :], lamst[:, 0, :], ALU.mult)
    TT(tv(I_T2), lamst[:, 1, :], lamst[:, 1, :], ALU.mult)
    TT(tv(I_T1), tv(I_T1), tv(I_T2), ALU.add)
    dv(lambda g: g.reciprocal(out=tv(I_T1), in_=tv(I_T1)))
    TT(tv(I_T2), tv(I_T0), lamst[:, 0, :], ALU.mult)
    TT(tv(I_T3), tv(I_LI), lamst[:, 1, :], ALU.mult)
    TT(tv(I_T2), tv(I_T2), tv(I_T3), ALU.add)
    TT(tv(I_KR), tv(I_T2), tv(I_T1), ALU.mult)
    TT(tv(I_T2), tv(I_LI), lamst[:, 0, :], ALU.mult)
    TT(tv(I_T3), tv(I_T0), lamst[:, 1, :], ALU.mult)
    TT(tv(I_T2), tv(I_T2), tv(I_T3), ALU.subtract)
    TT(tv(I_KI), tv(I_T2), tv(I_T1), ALU.mult)
    ct = k.sb(tag + "ct", [128, L, 8], F32); stt = k.sb(tag + "stt", [128, L, 8], F32)
    pwr = k.sb(tag + "pwr", [128, L, 8], F32); pwi = k.sb(tag + "pwi", [128, L, 8], F32)
    rhr = k.sb(tag + "rhr", [128, L, 8], F32); rhi = k.sb(tag + "rhi", [128, L, 8], F32)
    mur = k.sb(tag + "mur", [128, NLEV, 8], F32); mui = k.sb(tag + "mui", [128, NLEV, 8], F32)
    nst = k.sb(tag + "nst", [128, L, 8], F32)
    dv(lambda g: g.memset(ct[:, 0, :], 1.0)); dv(lambda g: g.memset(stt[:, 0, :], 0.0))
    dv(lambda g: g.tensor_copy(out=pwr[:, 0, :], in_=tv(I_LR))); dv(lambda g: g.tensor_copy(out=pwi[:, 0, :], in_=tv(I_LI)))
    for t in range(1, L):
        cmul(ct[:, t, :], stt[:, t, :], ct[:, t - 1, :], stt[:, t - 1, :], tv(I_COS), tv(I_SIN), tmp[:, 0, :], tmp[:, 1, :])
        cmul(pwr[:, t, :], pwi[:, t, :], pwr[:, t - 1, :], pwi[:, t - 1, :], tv(I_LR), tv(I_LI), tmp[:, 0, :], tmp[:, 1, :])
    dv(lambda g: g.tensor_scalar(out=nst[:], in0=stt[:], scalar1=-1.0, scalar2=None, op0=ALU.mult))
    for t in range(L):
        cmul(rhr[:, t, :], rhi[:, t, :], ct[:, t, :], nst[:, t, :], tv(I_KR), tv(I_KI), tmp[:, 0, :], tmp[:, 1, :])
    dv(lambda g: g.tensor_copy(out=mur[:, 0, :], in_=pwr[:, L - 1, :])); dv(lambda g: g.tensor_copy(out=mui[:, 0, :], in_=pwi[:, L - 1, :]))
    for lv in range(1, NLEV):
        cmul(mur[:, lv, :], mui[:, lv, :], mur[:, lv - 1, :], mui[:, lv - 1, :], mur[:, lv - 1, :], mui[:, lv - 1, :], tmp[:, 0, :], tmp[:, 1, :])
    rhis = k.sb(tag + "rhis", [128, L, 8], F32); nsts = k.sb(tag + "nsts", [128, L, 8], F32)
    npwis = k.sb(tag + "npwis", [128, L, 8], F32); muis = k.sb(tag + "muis", [128, NLEV, 8], F32)
    sts = k.sb(tag + "sts", [128, 8], F32)
    TS(rhis[:], rhi[:], sgn[:, 0:1], ALU.mult)
    TS(nsts[:], nst[:], sgn[:, 0:1], ALU.mult)
    TS(npwis[:], pwi[:], sgn[:, 0:1], ALU.mult, -1.0, ALU.mult)
    TS(muis[:], mui[:], sgn[:, 0:1], ALU.mult)
    TS(sts[:], stt[:, L - 1, :], sgn[:, 0:1], ALU.mult)
    Bin = k.sb(tag + "Bin", [16, 8, L, 128], BF16); Cloc = k.sb(tag + "Cloc", [128, 8, L, 16], BF16)
    Ccor = k.sb(tag + "Ccor", [128, 8, L, 16], BF16)
    ErT = k.sb(tag + "ErT", [128, 8, 128], F32); MkT = k.sb(tag + "MkT", [128, 2, NLEV, 128], F32)
    bw = k.buf()
    bm = [k.sb("%sbm%d" % (tag, i), [128, 128], F32) for i in range(4)]; bbm = k.bufs(4)
    pps = [k.ps("%spps%d" % (tag, i), [128, 512]) for i in range(2)]; bpps = k.bufs(2)
    cnt = [0]

    def blockmat(a_ap, bs_ap):
        i = cnt[0] % 4
        cnt[0] += 1
        k.op("dve", lambda g: g.tensor_scalar(out=bm[i][:], in0=identf[:], scalar1=a_ap, scalar2=None, op0=ALU.mult), ins=[bc, bt], outs=[bbm[i]])
        k.op("dve", lambda g: g.scalar_tensor_tensor(out=bm[i][:], in0=swap[:], scalar=bs_ap, in1=bm[i][:], op0=ALU.mult, op1=ALU.add),
             ins=[bc, bt, bbm[i]], outs=[bbm[i]])
        return bm[i], bbm[i]

    ev = [0]

    def evac(out_ap, in_ap, bin_, bout):
        ev[0] += 1
        _evac(k, ev[0], out_ap, in_ap, [bin_], [bout])

    def group_weights(g_):
        gs = slice(g_, g_ + 1)
        for t in range(L):
            m, bm_ = blockmat(rhr[:, t, gs], rhis[:, t, gs])
            p = cnt[0] % 2
            k.op("pe", lambda g, m=m, p=p: g.matmul(pps[p][0:16, 0:128], lhsT=bst[:, g_, :], rhs=m[:], start=True, stop=True), ins=[bpar, bm_], outs=[bpps[p]])
            evac(Bin[:, g_, t, :], pps[p][0:16, 0:128], bpps[p], bw)
            m, bm_ = blockmat(ct[:, t, gs], nsts[:, t, gs])
            p = cnt[0] % 2
            k.op("pe", lambda g, m=m, p=p: g.matmul(pps[p][:, 0:16], lhsT=m[:], rhs=cst[:, g_, :], start=True, stop=True), ins=[bpar, bm_], outs=[bpps[p]])
            evac(Cloc[:, g_, t, :], pps[p][:, 0:16], bpps[p], bw)
            m, bm_ = blockmat(pwr[:, t, gs], npwis[:, t, gs])
            p = cnt[0] % 2
            k.op("pe", lambda g, m=m, p=p: g.matmul(pps[p][:, 0:16], lhsT=m[:], rhs=cst[:, g_, :], start=True, stop=True), ins=[bpar, bm_], outs=[bpps[p]])
            evac(Ccor[:, g_, t, :], pps[p][:, 0:16], bpps[p], bw)
    group_weights(0)
    def blockmat_into(out_ap, a_ap, bs_ap):
        k.op("dve", lambda g: g.tensor_scalar(out=out_ap, in0=identf[:], scalar1=a_ap, scalar2=None, op0=ALU.mult), ins=[bc, bt, bw], outs=[bw])
        k.op("dve", lambda g: g.scalar_tensor_tensor(out=out_ap, in0=swap[:], scalar=bs_ap, in1=out_ap, op0=ALU.mult, op1=ALU.add),
             ins=[bc, bt, bw], outs=[bw])
    for g_ in range(8):
        gs = slice(g_, g_ + 1)
        blockmat_into(ErT[:, g_, :], ct[:, L - 1, gs], sts[:, gs])
    ug = [k.sb("%sug%d" % (tag, i), [16, T], BF16) for i in range(1)] * 2; bug = [k.buf()] * 2
    zbf = k.sb(tag + "zbf", [128, T], BF16); bzbf = k.buf()
    Ag = k.sb(tag + "Ag", [128, NBLK], F32); bAg = k.buf()
    zin = [k.sb("%szin%d" % (tag, i), [128, NBLK], F32) for i in range(2)]; bzin = k.bufs(2)
    zf = [k.sb("%szf%d" % (tag, i), [128, NBLK], F32) for i in range(2)]; bzf = k.bufs(2)
    zps = [k.ps("%szps%d" % (tag, i), [128, L, CPB]) for i in range(2)]; bzps = k.bufs(2)
    yps = k.ps(tag + "yps", [128, L, CPB]); byps = k.buf()
    Ssc = [k.sb("%sSsc%d" % (tag, i), [128, NC], F32) for i in range(2)]; bS = k.bufs(2)
    Sbf = k.sb(tag + "Sbf", [128, NC + 1], BF16); bSbf = k.buf()
    y1 = [k.sb("%sy1_%d" % (tag, i), [16, NBLK], F32) for i in range(2)]; by1 = k.bufs(2)
    y2 = [k.sb("%sy2_%d" % (tag, i), [16, NBLK], F32) for i in range(2)]; by2 = k.bufs(2)
    yo = [k.sb("%syo_%d" % (tag, i), [16, NBLK], BF16) for i in range(2)]; byo = k.bufs(2)
    k.op("pool", lambda g: g.memset(Sbf[:, 0:1], 0.0), outs=[bSbf])
    for g_ in range(8):
        gs = slice(g_, g_ + 1)
        u_ = ug[g_ % 2]; bu_ = bug[g_ % 2]
        for i in range(4):
            k.dma(u_[:, i * 4096:(i + 1) * 4096], u16_d[:, g_, i * 4096:(i + 1) * 4096], outs=[bu_])
        k.op("dve", lambda g: g.tensor_scalar(out=Ag[:], in0=pat01[:].rearrange("p c l -> p (c l)"), scalar1=tab[:, I_ER, gs], scalar2=None, op0=ALU.mult),
             ins=[bc, bt], outs=[bAg])
        for lv in range(NLEV):
            blockmat_into(MkT[:, g_ % 2, lv, :], mur[:, lv, gs], muis[:, lv, gs])
        uv = u_[:].rearrange("p (c l) -> p c l", l=L)
        zbv = zbf[:].rearrange("p (c l) -> p c l", l=L)
        for blk in range(NB):
            s = blk % 2
            c0 = blk * CPB
            for t in range(L):
                k.op("pe", lambda g, t=t: g.matmul(zps[s][:, t, :], lhsT=Bin[:, g_, t, :], rhs=uv[:, c0:c0 + CPB, t], start=True, stop=True),
                     ins=[bw, bu_], outs=[bzps[s]])
            evac(zin[s][:].rearrange("p (c l) -> p l c", l=L), zps[s][:], bzps[s], bzin[s])
            k.op("dve", lambda g: g.tensor_tensor_scan(out=zf[s][:], data0=Ag[:], data1=zin[s][:], initial=0.0, op0=ALU.mult, op1=ALU.add),
                 ins=[bAg, bzin[s]], outs=[bzf[s]])
            k.op("pool", lambda g: g.tensor_copy(out=zbf[:, blk * NBLK:(blk + 1) * NBLK], in_=zf[s][:]), ins=[bzf[s]], outs=[bzbf])
            p = blk % 2
            k.op("pe", lambda g, p=p: g.matmul(pps[p][:, 0:CPB], lhsT=ErT[:, g_, :], rhs=zf[s][:].rearrange("p (c l) -> p c l", l=L)[:, :, L - 1],
                                               start=True, stop=True), ins=[bw, bzf[s]], outs=[bpps[p]])
            k.op("dve", lambda g, p=p: g.tensor_copy(out=Ssc[0][:, c0:c0 + CPB], in_=pps[p][:, 0:CPB]), ins=[bpps[p]], outs=[bS[0]])
        if g_ + 1 < 8:
            group_weights(g_ + 1)
        for lv in range(NLEV):
            d = 1 << lv
            src, dst = Ssc[lv % 2], Ssc[(lv + 1) % 2]
            bsrc, bdst = bS[lv % 2], bS[(lv + 1) % 2]
            n = NC - d
            k.op("pool", lambda g: g.tensor_copy(out=dst[:, 0:d], in_=src[:, 0:d]), ins=[bsrc], outs=[bdst])
            for j0 in range(0, n, 512):
                nn = min(512, n - j0)
                p = (j0 // 512) % 2
                k.op("pe", lambda g, p=p: g.matmul(pps[p][:, 0:nn], lhsT=MkT[:, g_ % 2, lv, :], rhs=src[:, j0:j0 + nn], start=True, stop=True),
                     ins=[bw, bsrc], outs=[bpps[p]])
                k.op("dve", lambda g, p=p: g.tensor_tensor(out=dst[:, d + j0:d + j0 + nn], in0=src[:, d + j0:d + j0 + nn], in1=pps[p][:, 0:nn], op=ALU.add),
                     ins=[bsrc, bpps[p]], outs=[bdst])
        fin = Ssc[NLEV % 2]; bfin = bS[NLEV % 2]
        k.op("act", lambda g: g.activation(out=Sbf[:, 1:NC + 1], in_=fin[:], func=AF.Identity), ins=[bfin], outs=[bSbf])
        for blk in range(NB):
            s = blk % 2
            c0 = blk * CPB
            t0 = blk * NBLK
            for t in range(L):
                k.op("pe", lambda g, t=t: g.matmul(yps[0:16, t, :], lhsT=Cloc[:, g_, t, :], rhs=zbv[:, c0:c0 + CPB, t], start=True, stop=False),
                     ins=[bw, bzbf], outs=[byps])
                k.op("pe", lambda g, t=t: g.matmul(yps[0:16, t, :], lhsT=Ccor[:, g_, t, :], rhs=Sbf[:, c0:c0 + CPB], start=False, stop=True),
                     ins=[bw, bSbf], outs=[byps])
            evac(y1[s][:].rearrange("p (c l) -> p l c", l=L), yps[0:16], byps, by1[s])
            k.op("dve", lambda g: g.scalar_tensor_tensor(out=y1[s][:], in0=u_[:, t0:t0 + NBLK], scalar=dsk[:, gs], in1=y1[s][:], op0=ALU.mult, op1=ALU.add),
                 ins=[bu_, bpar, by1[s]], outs=[by1[s]])
            k.op("act", lambda g: g.activation(out=y2[s][:], in_=y1[s][:], func=AF.Square), ins=[by1[s]], outs=[by2[s]])
            k.op("dve", lambda g: g.tensor_scalar(out=y2[s][:], in0=y2[s][:], scalar1=0.044715, scalar2=1.0, op0=ALU.mult, op1=ALU.add), ins=[by2[s]], outs=[by2[s]])
            k.op("dve", lambda g: g.tensor_tensor(out=y2[s][:], in0=y2[s][:], in1=y1[s][:], op=ALU.mult), ins=[by2[s], by1[s]], outs=[by2[s]])
            k.op("act", lambda g: g.activation(out=y2[s][:], in_=y2[s][:], func=AF.Sigmoid, scale=1.5957691216057308), ins=[by2[s]], outs=[by2[s]])
            k.op("dve", lambda g: g.tensor_tensor(out=yo[s][:], in0=y2[s][:], in1=y1[s][:], op=ALU.mult), ins=[by2[s], by1[s]], outs=[byo[s]])
            k.dma(y_dram[:, g_, t0:t0 + NBLK], yo[s][:], ins=[byo[s]], outs=[by])


def build_S5():
    nc = new_nc()
    u16_d = nc.dram_tensor("u16", [16, 8, T], BF16, kind="ExternalInput").ap()
    lamst_d = nc.dram_tensor("lamst", [128, 3, 8], F32, kind="ExternalInput").ap()
    bst_d = nc.dram_tensor("bst", [128, 8, 16], F32, kind="ExternalInput").ap()
    cst_d = nc.dram_tensor("cst", [128, 8, 16], F32, kind="ExternalInput").ap()
    dsk_d = nc.dram_tensor("dsk", [16, 8], F32, kind="ExternalInput").ap()
    yd = nc.dram_tensor("ys5", [16, 8, T], BF16, kind="ExternalOutput").ap()
    with contextlib.ExitStack() as st:
        k = K(nc, st)
        by = k.buf()
        emit_s5(k, u16_d, lamst_d, bst_d, cst_d, dsk_d, yd, by)
        k.finish([by])
    return nc


def s5_host_inputs(c, u, lam_re, lam_im, log_dt, b_re, b_im, c_re, c_im, d_skip):
    bf = ml_dtypes.bfloat16
    G = slice(8 * c, 8 * c + 8)
    u16 = u[:, 128 * c:128 * (c + 1)].reshape(T, 8, 16).transpose(2, 1, 0).astype(bf)
    st2 = lambda a: np.concatenate([a, a], axis=0)
    lamst = np.stack([st2(lam_re[G].T), st2(lam_im[G].T), np.tile(log_dt[G][None, :], (128, 1))], axis=1).astype(np.float32)
    bst = np.concatenate([b_re[G].transpose(1, 0, 2), b_im[G].transpose(1, 0, 2)], axis=0).astype(np.float32)
    cst = np.concatenate([c_re[G].transpose(2, 0, 1), c_im[G].transpose(2, 0, 1)], axis=0).astype(np.float32)
    dsk = d_skip[128 * c:128 * (c + 1)].reshape(8, 16).T.astype(np.float32)
    return {"u16": np.ascontiguousarray(u16), "lamst": np.ascontiguousarray(lamst), "bst": np.ascontiguousarray(bst),
            "cst": np.ascontiguousarray(cst), "dsk": np.ascontiguousarray(dsk)}


def build_ADA():
    NCOL = 2 * 6 * D // NCORES
    nc = new_nc()
    cT = nc.dram_tensor("cT", [128, 16], F32, kind="ExternalInput").ap()
    w = nc.dram_tensor("w", [D, NCOL], F32, kind="ExternalInput").ap()
    b = nc.dram_tensor("b", [1, NCOL], F32, kind="ExternalInput").ap()
    o = nc.dram_tensor("mod", [1, NCOL], F32, kind="ExternalOutput").ap()
    w_v = w.rearrange("(kc p) n -> p kc n", p=128)
    with contextlib.ExitStack() as st:
        k = K(nc, st)
        cs = k.sb("cs", [128, 16], F32); bcs = k.buf()
        bs = k.sb("bs", [1, NCOL], F32); bbs = k.buf()
        os_ = k.sb("os", [1, NCOL], F32); bos = k.buf()
        wt = [k.sb("wt%d" % i, [128, 16, 512], F32) for i in range(2)]; bwt = k.bufs(2)
        ps = [k.ps("ps%d" % i, [128, 512]) for i in range(2)]; bps = k.bufs(2)
        bo = k.buf()
        k.dma(cs[:], cT, outs=[bcs]); k.dma(bs[:], b, outs=[bbs])
        k.op("act", lambda g: g.activation(out=cs[:], in_=cs[:], func=AF.Silu), ins=[bcs], outs=[bcs])
        for j in range(NCOL // 512):
            s = j % 2
            k.dma(wt[s][:], w_v[:, :, j * 512:(j + 1) * 512], outs=[bwt[s]])
            for kc in range(16):
                k.op("pe", lambda g, kc=kc: g.matmul(ps[s][0:1, :], lhsT=cs[:, kc:kc + 1], rhs=wt[s][:, kc, :], start=(kc == 0), stop=(kc == 15)),
                     ins=[bcs, bwt[s]], outs=[bps[s]])
            k.op("dve", lambda g: g.tensor_tensor(out=os_[:, j * 512:(j + 1) * 512], in0=ps[s][0:1, :], in1=bs[:, j * 512:(j + 1) * 512], op=ALU.add),
                 ins=[bps[s], bbs], outs=[bos])
        k.dma(o, os_[:], ins=[bos], outs=[bo])
        k.finish([bo])
    return nc


def build_C(last):
    NTOK = 2048
    NT = 512
    nc = new_nc()
    xT = nc.dram_tensor("xT", [D, NTOK], F32, kind="ExternalInput").ap()
    pgT = nc.dram_tensor("pgT", [3 * D, NTOK], F32, kind="ExternalInput").ap()
    yT = nc.dram_tensor("yT", [3072, NTOK], BF16, kind="ExternalInput").ap()
    vecs = nc.dram_tensor("vecs", [128, 8, 16], F32, kind="ExternalInput").ap()
    wglu = nc.dram_tensor("wglu", [1024, 1024], F32, kind="ExternalInput").ap()
    pw = [nc.dram_tensor(n, [1024, D], F32, kind="ExternalInput").ap() for n in ("psb", "ps5", "pml")]
    wout = nc.dram_tensor("wout", [D, D], F32, kind="ExternalInput").ap()
    wgr = nc.dram_tensor("wgr", [D, 36], F32, kind="ExternalInput").ap()
    bgr = nc.dram_tensor("bgr", [128, 36], F32, kind="ExternalInput").ap()
    w1 = nc.dram_tensor("w1", [32, D, 256], F32, kind="ExternalInput").ap()
    w3 = nc.dram_tensor("w3", [32, D, 256], F32, kind="ExternalInput").ap()
    w2 = nc.dram_tensor("w2", [32, 256, D], F32, kind="ExternalInput").ap()
    xnT = nc.dram_tensor("xnT", [D, NTOK], F32, kind="ExternalOutput").ap()
    if last:
        outT = nc.dram_tensor("outT", [D, NTOK], F32, kind="ExternalOutput").ap()
    xT_v = xT.rearrange("(kc p) t -> p kc t", p=128)
    xnT_v = xnT.rearrange("(kc p) t -> p kc t", p=128)
    yT_v = yT.rearrange("(kc p) t -> p kc t", p=128)
    pg_v = pgT.rearrange("(br n p) t -> p br n t", br=3, p=128)
    wglu_v = wglu.rearrange("(kc p) n -> p kc n", p=128)
    pw_v = [a.rearrange("(kc p) n -> p kc n", p=128) for a in pw]
    wout_v = wout.rearrange("(kc p) n -> p kc n", p=128)
    wgr_v = wgr.rearrange("(kc p) n -> p kc n", p=128)
    w1_v = w1.rearrange("e (kc p) f -> p e kc f", p=128)
    w3_v = w3.rearrange("e (kc p) f -> p e kc f", p=128)
    w2_v = w2.rearrange("e (f p) n -> p e f n", p=128)
    with contextlib.ExitStack() as st:
        k = K(nc, st)
        identf = k.sb("identf", [128, 128], F32); onesf = k.sb("onesf", [128, 128], F32); ones_bf = k.sb("ones_bf", [128, 128], BF16)
        bc = k.buf()
        k.op("pool", lambda g: g.memset(onesf[:], 1.0), outs=[bc])
        k.op("pool", lambda g: g.memset(ones_bf[:], 1.0), ins=[bc], outs=[bc])
        k.op("pool", lambda g: g.affine_select(out=identf[:], in_=onesf[:], pattern=[[-1, 128]], compare_op=ALU.is_equal,
                                               fill=0.0, base=0, channel_multiplier=1), ins=[bc], outs=[bc])
        vin = k.sb("vin", [128, 8, 16], F32); bvin = k.buf()
        vec = k.sb("vec", [128, 2, 16], F32); bvec = k.buf()
        vecf = k.sb("vecf", [128, 2, 16], F32); bvecf = k.buf()
        wgrs = k.sb("wgrs", [128, 16, 36], F32); bgrs = k.sb("bgrs", [128, 36], F32); bwgr = k.buf()
        k.dma(vin[:], vecs, outs=[bvin]); k.dma(wgrs[:], wgr_v, outs=[bwgr]); k.dma(bgrs[:], bgr, outs=[bwgr])
        k.op("dve", lambda g: g.scalar_tensor_tensor(out=vec[:, 0, :], in0=vin[:, 2, :], scalar=1.0, in1=vin[:, 1, :], op0=ALU.add, op1=ALU.mult), ins=[bvin], outs=[bvec])
        k.op("dve", lambda g: g.tensor_copy(out=vec[:, 1, :], in_=vin[:, 3, :]), ins=[bvin, bvec], outs=[bvec])
        k.op("dve", lambda g: g.tensor_copy(out=vecf[:, 0, :], in_=vin[:, 5, :]), ins=[bvin], outs=[bvecf])
        k.op("dve", lambda g: g.memset(vecf[:, 1, :], 0.0), ins=[bvecf], outs=[bvecf])
        xt = k.sb("xt", [128, 16, NT], F32); bx = k.buf()
        yt = k.sb("yt", [128, 24, NT], BF16); byt = k.buf()
        ysg = k.sb("ysg", [128, 8, NT], BF16); bysg = k.buf()
        mh = k.sb("mh", [128, 16, NT], BF16); bmh = k.buf()
        hid = k.sb("hid", [128, 32, NT], BF16); bhid = k.buf()
        sq = hid[:, 0:16, :]
        wst = [k.sb("wst%d" % i, [128, 4096], F32) for i in range(2)]; bwst = k.bufs(2)
        wbf = [k.sb("wbf%d" % i, [128, 4096], BF16) for i in range(2)]; bwbf = k.bufs(2)
        pgt = [k.sb("pgt%d" % i, [128, 3, NT], F32) for i in range(1)] * 2; bpgt = [k.buf()] * 2
        sg = [k.sb("sg%d" % i, [128, NT], F32) for i in range(3)]; bsg = k.bufs(3)
        t1 = k.sb("t1", [128, NT], F32); t2 = k.sb("t2", [128, NT], F32); bt1 = k.buf(); bt2 = k.buf()
        rstd = k.sb("rstd", [128, NT], F32); brstd = k.buf()
        tmp = k.sb("tmp", [128, 2, NT], F32); btmp = k.bufs(2)
        h2f = [k.sb("h2f%d" % i, [128, NT], F32) for i in range(2)]; bh2f = k.bufs(2)
        gT = k.sb("gT", [32, NT], F32); bgT = k.buf()
        gsel = [k.sb("gsel%d" % i, [32, NT], F32) for i in range(2)]; bgsel = k.bufs(2)
        gbs = [k.sb("gbs%d" % i, [128, NT], F32) for i in range(2)]; bgbs = k.bufs(2)
        rt = k.sb("rt", [128, 16, 36], F32); brt = k.buf()
        P = [k.ps("P%d" % i, [128, 512]) for i in range(8)]; bP = k.bufs(8)
        ACC = [0, 1, 2, 7]
        bout = k.buf()
        wcnt = [0]

        def wtile(view, shape):
            i = wcnt[0] % 2
            wcnt[0] += 1
            a, b = shape
            sv = wst[i][:, 0:a * b].rearrange("p (a b) -> p a b", b=b)
            bv = wbf[i][:, 0:a * b].rearrange("p (a b) -> p a b", b=b)
            k.dma(sv, view, outs=[bwst[i]])
            k.op("pool", lambda g: g.tensor_copy(out=bv, in_=sv), ins=[bwst[i]], outs=[bwbf[i]])
            return bv, bwbf[i]

        acnt = [0]

        def accbank():
            i = ACC[acnt[0] % 4]
            acnt[0] += 1
            return P[i], bP[i]

        for tt in range(NTOK // NT):
            tsl = slice(tt * NT, (tt + 1) * NT)
            k.dma(xt[:], xT_v[:, :, tsl], outs=[bx])
            k.dma(yt[:], yT_v[:, :, tsl], outs=[byt])
            for n in range(8):
                wv, bwv = wtile(wglu_v[:, :, n * 128:(n + 1) * 128], (8, 128))
                ps, bps = accbank()
                for kc in range(8):
                    k.op("pe", lambda g, kc=kc: g.matmul(ps[:], lhsT=wv[:, kc, :], rhs=yt[:, 8 + kc, :], start=(kc == 0), stop=(kc == 7)), ins=[bwv, byt], outs=[bps])
                k.op("act", lambda g: g.activation(out=sg[0][:], in_=ps[:], func=AF.Sigmoid), ins=[bps], outs=[bsg[0]])
                k.op("dve", lambda g: g.tensor_tensor(out=ysg[:, n, :], in0=yt[:, 8 + n, :], in1=sg[0][:], op=ALU.mult), ins=[byt, bsg[0]], outs=[bysg])
            for n in range(16):
                pgs = pgt[n % 2]; bpgs = bpgt[n % 2]
                k.dma(pgs[:], pg_v[:, :, n, tsl], outs=[bpgs])
                banks = []
                for br in range(3):
                    wv, bwv = wtile(pw_v[br][:, :, n * 128:(n + 1) * 128], (8, 128))
                    ps, bps = accbank()
                    for kc in range(8):
                        rhs = ysg[:, kc, :] if br == 1 else yt[:, br * 8 + kc, :]
                        k.op("pe", lambda g, kc=kc, rhs=rhs: g.matmul(ps[:], lhsT=wv[:, kc, :], rhs=rhs, start=(kc == 0), stop=(kc == 7)),
                             ins=[bwv, byt, bysg], outs=[bps])
                    k.op("act", lambda g, br=br: g.activation(out=sg[br][:], in_=pgs[:, br, :], func=AF.Sigmoid), ins=[bpgs], outs=[bsg[br]])
                    banks.append((ps, bps))
                k.op("dve", lambda g: g.tensor_tensor(out=t1[:], in0=banks[0][0][:], in1=sg[0][:], op=ALU.mult), ins=[banks[0][1], bsg[0]], outs=[bt1])
                k.op("dve", lambda g: g.tensor_tensor(out=t2[:], in0=banks[1][0][:], in1=sg[1][:], op=ALU.mult), ins=[banks[1][1], bsg[1]], outs=[bt2])
                k.op("pool", lambda g: g.tensor_tensor(out=t1[:], in0=t1[:], in1=t2[:], op=ALU.add), ins=[bt1, bt2], outs=[bt1])
                k.op("dve", lambda g: g.tensor_tensor(out=t2[:], in0=banks[2][0][:], in1=sg[2][:], op=ALU.mult), ins=[banks[2][1], bsg[2], bt1], outs=[bt2])
                k.op("pool", lambda g: g.tensor_tensor(out=mh[:, n, :], in0=t1[:], in1=t2[:], op=ALU.add), ins=[bt1, bt2], outs=[bmh])
            for n in range(16):
                wv, bwv = wtile(wout_v[:, :, n * 128:(n + 1) * 128], (16, 128))
                ps, bps = accbank()
                for kc in range(16):
                    k.op("pe", lambda g, kc=kc: g.matmul(ps[:], lhsT=wv[:, kc, :], rhs=mh[:, kc, :], start=(kc == 0), stop=(kc == 15)), ins=[bwv, bmh], outs=[bps])
                k.op("dve", lambda g: g.scalar_tensor_tensor(out=xt[:, n, :], in0=ps[:], scalar=vin[:, 0, n:n + 1], in1=xt[:, n, :], op0=ALU.mult, op1=ALU.add),
                     ins=[bps, bvin, bx], outs=[bx])
            k.op("act", lambda g: g.activation(out=sq, in_=xt[:], func=AF.Square), ins=[bx], outs=[bhid])
            for kc in range(16):
                k.op("pe", lambda g, kc=kc: g.matmul(P[6][:], lhsT=ones_bf[:], rhs=sq[:, kc, :], start=(kc == 0), stop=(kc == 15)), ins=[bc, bhid], outs=[bP[6]])
            k.op("act", lambda g: g.activation(out=rstd[:], in_=P[6][:], func=AF.Sqrt, scale=1.0 / D, bias=EPS), ins=[bP[6]], outs=[brstd])
            k.op("dve", lambda g: g.reciprocal(out=rstd[:], in_=rstd[:]), ins=[brstd], outs=[brstd])
            for kc in range(16):
                i = kc % 2
                k.op("dve", lambda g, kc=kc: g.tensor_tensor(out=tmp[:, i, :], in0=xt[:, kc, :], in1=rstd[:], op=ALU.mult), ins=[bx, brstd], outs=[btmp[i]])
                k.op("act", lambda g, kc=kc: g.activation(out=h2f[i][:], in_=tmp[:, i, :], func=AF.Identity, scale=vec[:, 0, kc:kc + 1], bias=vec[:, 1, kc:kc + 1]),
                     ins=[btmp[i], bvec], outs=[bh2f[i]])
                k.op("pool", lambda g, kc=kc: g.tensor_copy(out=mh[:, kc, :], in_=h2f[i][:]), ins=[bh2f[i]], outs=[bmh])
                for j in range(4):
                    k.op("pe", lambda g, kc=kc, j=j: g.matmul(P[4][:, j * 36:(j + 1) * 36], lhsT=h2f[i][:, j * 128:(j + 1) * 128], rhs=wgrs[:, kc, :],
                                                             start=(kc == 0 and j == 0), stop=(kc == 15), skip_group_check=True), ins=[bh2f[i], bwgr], outs=[bP[4]])
            R = lambda a, b_: rt[:, a, 0:b_]
            def dv(fn, extra=()):
                k.op("dve", fn, ins=[brt, bP[4], bwgr] + list(extra), outs=[brt])
            for j in range(4):
                lg = rt[:, 0, :]
                dv(lambda g: g.tensor_tensor(out=lg, in0=P[4][:, j * 36:(j + 1) * 36], in1=bgrs[:], op=ALU.add))
                dv(lambda g: g.tensor_reduce(out=R(1, 1), in_=lg[:, 0:4], axis=AX.X, op=ALU.max))
                dv(lambda g: g.tensor_scalar(out=R(2, 4), in0=lg[:, 0:4], scalar1=R(1, 1), scalar2=None, op0=ALU.is_equal))
                dv(lambda g: g.tensor_scalar(out=R(3, 1), in0=R(1, 1), scalar1=-1.0, scalar2=None, op0=ALU.mult))
                k.op("act", lambda g: g.activation(out=R(4, 4), in_=lg[:, 0:4], func=AF.Exp, bias=R(3, 1)), ins=[brt], outs=[brt])
                dv(lambda g: g.tensor_reduce(out=R(5, 1), in_=R(4, 4), axis=AX.X, op=ALU.add))
                dv(lambda g: g.reciprocal(out=R(5, 1), in_=R(5, 1)))
                dv(lambda g: g.tensor_scalar(out=R(6, 8), in0=lg[:, 4:12], scalar1=rt[:, 2, 0:1], scalar2=None, op0=ALU.mult))
                for gi in range(1, 4):
                    dv(lambda g, gi=gi: g.scalar_tensor_tensor(out=R(6, 8), in0=lg[:, 4 + 8 * gi:12 + 8 * gi], scalar=rt[:, 2, gi:gi + 1], in1=R(6, 8),
                                                               op0=ALU.mult, op1=ALU.add))
                dv(lambda g: g.tensor_reduce(out=R(7, 1), in_=R(6, 8), axis=AX.X, op=ALU.max))
                dv(lambda g: g.tensor_scalar(out=R(8, 8), in0=R(6, 8), scalar1=R(7, 1), scalar2=None, op0=ALU.is_equal))
                dv(lambda g: g.scalar_tensor_tensor(out=R(9, 8), in0=R(8, 8), scalar=-1e30, in1=R(6, 8), op0=ALU.mult, op1=ALU.add))
                dv(lambda g: g.tensor_reduce(out=R(10, 1), in_=R(9, 8), axis=AX.X, op=ALU.max))
                dv(lambda g: g.tensor_scalar(out=R(11, 8), in0=R(9, 8), scalar1=R(10, 1), scalar2=None, op0=ALU.is_equal))
                dv(lambda g: g.tensor_tensor(out=R(12, 1), in0=R(10, 1), in1=R(7, 1), op=ALU.subtract))
                k.op("act", lambda g: g.activation(out=R(12, 1), in_=R(12, 1), func=AF.Exp), ins=[brt], outs=[brt])
                dv(lambda g: g.tensor_scalar(out=R(13, 1), in0=R(12, 1), scalar1=1.0, scalar2=None, op0=ALU.add))
                dv(lambda g: g.reciprocal(out=R(13, 1), in_=R(13, 1)))
                dv(lambda g: g.tensor_tensor(out=R(13, 1), in0=R(13, 1), in1=R(5, 1), op=ALU.mult))
                dv(lambda g: g.tensor_tensor(out=R(14, 1), in0=R(13, 1), in1=R(12, 1), op=ALU.mult))
                dv(lambda g: g.tensor_scalar(out=R(15, 8), in0=R(8, 8), scalar1=R(13, 1), scalar2=None, op0=ALU.mult))
                dv(lambda g: g.scalar_tensor_tensor(out=R(15, 8), in0=R(11, 8), scalar=R(14, 1), in1=R(15, 8), op0=ALU.mult, op1=ALU.add))
                gts = rt[:, 1, 4:36]
                for gi in range(4):
                    dv(lambda g, gi=gi: g.tensor_scalar(out=gts[:, gi * 8:(gi + 1) * 8], in0=R(15, 8), scalar1=rt[:, 2, gi:gi + 1], scalar2=None, op0=ALU.mult))
                k.op("pe", lambda g: g.transpose(P[5][0:32, 0:128], gts, identf[:]), ins=[brt, bc], outs=[bP[5]])
                k.op("act", lambda g, j=j: g.activation(out=gT[:, j * 128:(j + 1) * 128], in_=P[5][0:32, 0:128], func=AF.Identity), ins=[bP[5]], outs=[bgT])
            for half in range(2):
                for el in range(16):
                    e = half * 16 + el
                    gsl = gbs[e % 2]; bgsl = bgbs[e % 2]
                    k.op("pool", lambda g, e=e: g.tensor_scalar(out=gsel[e % 2][:], in0=gT[:], scalar1=identf[0:32, e:e + 1], scalar2=None, op0=ALU.mult),
                         ins=[bc, bgT], outs=[bgsel[e % 2]])
                    k.op("pe", lambda g, e=e: g.matmul(P[3][:], lhsT=onesf[0:32, :], rhs=gsel[e % 2][:], start=True, stop=True), ins=[bc, bgsel[e % 2]], outs=[bP[3]])
                    k.op("act", lambda g: g.activation(out=gsl[:], in_=P[3][:], func=AF.Identity), ins=[bP[3]], outs=[bgsl])
                    w1v, bw1 = wtile(w1_v[:, e, :, :], (16, 256))
                    w3v, bw3 = wtile(w3_v[:, e, :, :], (16, 256))
                    for f in range(2):
                        pa, bpa = accbank()
                        pb, bpb = accbank()
                        for kc in range(16):
                            k.op("pe", lambda g, kc=kc: g.matmul(pa[:], lhsT=w1v[:, kc, f * 128:(f + 1) * 128], rhs=mh[:, kc, :], start=(kc == 0), stop=(kc == 15)),
                                 ins=[bw1, bmh], outs=[bpa])
                        for kc in range(16):
                            k.op("pe", lambda g, kc=kc: g.matmul(pb[:], lhsT=w3v[:, kc, f * 128:(f + 1) * 128], rhs=mh[:, kc, :], start=(kc == 0), stop=(kc == 15)),
                                 ins=[bw3, bmh], outs=[bpb])
                        k.op("act", lambda g: g.activation(out=t1[:], in_=pa[:], func=AF.Silu), ins=[bpa], outs=[bt1])
                        k.op("dve", lambda g: g.tensor_tensor(out=t2[:], in0=pb[:], in1=t1[:], op=ALU.mult), ins=[bpb, bt1], outs=[bt2])
                        k.op("pool", lambda g, el=el, f=f: g.tensor_tensor(out=hid[:, el * 2 + f, :], in0=t2[:], in1=gsl[:], op=ALU.mult), ins=[bt2, bgsl], outs=[bhid])
                for n in range(16):
                    wv, bwv = wtile(w2_v[:, half * 16:(half + 1) * 16, :, n * 128:(n + 1) * 128].rearrange("p e f n -> p (e f) n"), (32, 128))
                    ps, bps = accbank()
                    for kk in range(32):
                        k.op("pe", lambda g, kk=kk: g.matmul(ps[:], lhsT=wv[:, kk, :], rhs=hid[:, kk, :], start=(kk == 0), stop=(kk == 31)), ins=[bwv, bhid], outs=[bps])
                    k.op("dve", lambda g: g.scalar_tensor_tensor(out=xt[:, n, :], in0=ps[:], scalar=vin[:, 4, n:n + 1], in1=xt[:, n, :], op0=ALU.mult, op1=ALU.add),
                         ins=[bps, bvin, bx], outs=[bx])
            k.dma(xnT_v[:, :, tsl], xt[:], ins=[bx], outs=[bout])
            if last:
                k.op("act", lambda g: g.activation(out=sq, in_=xt[:], func=AF.Square), ins=[bx], outs=[bhid])
                for kc in range(16):
                    k.op("pe", lambda g, kc=kc: g.matmul(P[6][:], lhsT=ones_bf[:], rhs=sq[:, kc, :], start=(kc == 0), stop=(kc == 15)), ins=[bc, bhid], outs=[bP[6]])
                k.op("act", lambda g: g.activation(out=rstd[:], in_=P[6][:], func=AF.Sqrt, scale=1.0 / D, bias=EPS), ins=[bP[6]], outs=[brstd])
                k.op("dve", lambda g: g.reciprocal(out=rstd[:], in_=rstd[:]), ins=[brstd], outs=[brstd])
                for kc in range(16):
                    i = kc % 2
                    k.op("dve", lambda g, kc=kc: g.tensor_tensor(out=tmp[:, i, :], in0=xt[:, kc, :], in1=rstd[:], op=ALU.mult), ins=[bx, brstd], outs=[btmp[i]])
                    k.op("act", lambda g, kc=kc: g.activation(out=h2f[i][:], in_=tmp[:, i, :], func=AF.Identity, scale=vecf[:, 0, kc:kc + 1], bias=vecf[:, 1, kc:kc + 1]),
                         ins=[btmp[i], bvecf], outs=[bh2f[i]])
                    k.dma(outT[kc * 128:(kc + 1) * 128, tsl], h2f[i][:], ins=[bh2f[i]], outs=[bout])
        k.finish([bout])
    return nc


def build_C2(last, withA=False):
    NTOK = 2048
    NT = 512
    nc = new_nc()
    xT = nc.dram_tensor("xT", [D, NTOK], F32, kind="ExternalInput").ap()
    pgT = nc.dram_tensor("pgT", [3 * D, NTOK], F32, kind="ExternalInput").ap()
    yT = nc.dram_tensor("yT", [3072, NTOK], BF16, kind="ExternalInput").ap()
    vecs = nc.dram_tensor("vecs", [128, 8, 16], F32, kind="ExternalInput").ap()
    wglu = nc.dram_tensor("wglu", [1024, 1024], F32, kind="ExternalInput").ap()
    pw = [nc.dram_tensor(n, [1024, D], F32, kind="ExternalInput").ap() for n in ("psb", "ps5", "pml")]
    wout = nc.dram_tensor("wout", [D, D], F32, kind="ExternalInput").ap()
    wgr = nc.dram_tensor("wgr", [D, 36], F32, kind="ExternalInput").ap()
    bgr = nc.dram_tensor("bgr", [128, 36], F32, kind="ExternalInput").ap()
    w1 = nc.dram_tensor("w1", [32, D, 256], F32, kind="ExternalInput").ap()
    w3 = nc.dram_tensor("w3", [32, D, 256], F32, kind="ExternalInput").ap()
    w2 = nc.dram_tensor("w2", [32, 256, D], F32, kind="ExternalInput").ap()
    xnT = nc.dram_tensor("xnT", [D, NTOK], F32, kind="ExternalOutput").ap()
    if last:
        outT = nc.dram_tensor("outT", [D, NTOK], F32, kind="ExternalOutput").ap()
    if withA:
        vecsA = nc.dram_tensor("vecsA", [128, 3, 16], F32, kind="ExternalInput").ap()
        wA = nc.dram_tensor("wA", [D, IN_COLS], F32, kind="ExternalInput").ap()
        paA = nc.dram_tensor("pa", [8192, NTOK], BF16, kind="ExternalOutput").ap()
        pifA = nc.dram_tensor("pif", [8, NTOK], F32, kind="ExternalOutput").ap()
        pgA = nc.dram_tensor("pg", [6144, NTOK], F32, kind="ExternalOutput").ap()
    xT_v = xT.rearrange("(kc p) t -> p kc t", p=128)
    xnT_v = xnT.rearrange("(kc p) t -> p kc t", p=128)
    yT_v = yT.rearrange("(kc p) t -> p kc t", p=128)
    pg_v = pgT.rearrange("(br n p) t -> p br n t", br=3, p=128)
    wglu_v = wglu.rearrange("(kc p) n -> p kc n", p=128)
    pw_v = [a.rearrange("(kc p) n -> p kc n", p=128) for a in pw]
    wout_v = wout.rearrange("(kc p) n -> p kc n", p=128)
    wgr_v = wgr.rearrange("(kc p) n -> p kc n", p=128)
    w1_v = w1.rearrange("e (kc p) f -> p e kc f", p=128)
    w3_v = w3.rearrange("e (kc p) f -> p e kc f", p=128)
    w2_v = w2.rearrange("e (f p) n -> p e f n", p=128)
    xmid_d = nc.dram_tensor("xmid_d", [D, NTOK], F32).ap()
    xmid_v = xmid_d.rearrange("(kc p) t -> p kc t", p=128)
    with contextlib.ExitStack() as st:
        k = K(nc, st)
        identf = k.sb("identf", [128, 128], F32); onesf = k.sb("onesf", [128, 128], F32); ones_bf = k.sb("ones_bf", [128, 128], BF16)
        bc = k.buf()
        k.op("pool", lambda g: g.memset(onesf[:], 1.0), outs=[bc])
        k.op("pool", lambda g: g.memset(ones_bf[:], 1.0), ins=[bc], outs=[bc])
        k.op("pool", lambda g: g.affine_select(out=identf[:], in_=onesf[:], pattern=[[-1, 128]], compare_op=ALU.is_equal,
                                               fill=0.0, base=0, channel_multiplier=1), ins=[bc], outs=[bc])
        vin = k.sb("vin", [128, 8, 16], F32); bvin = k.buf()
        vec = k.sb("vec", [128, 2, 16], F32); bvec = k.buf()
        vecf = k.sb("vecf", [128, 2, 16], F32); bvecf = k.buf()
        wgrs = k.sb("wgrs", [128, 16, 36], F32); bgrs = k.sb("bgrs", [128, 36], F32); bwgr = k.buf()
        k.dma(vin[:], vecs, outs=[bvin]); k.dma(wgrs[:], wgr_v, outs=[bwgr]); k.dma(bgrs[:], bgr, outs=[bwgr])
        k.op("dve", lambda g: g.scalar_tensor_tensor(out=vec[:, 0, :], in0=vin[:, 2, :], scalar=1.0, in1=vin[:, 1, :], op0=ALU.add, op1=ALU.mult), ins=[bvin], outs=[bvec])
        k.op("dve", lambda g: g.tensor_copy(out=vec[:, 1, :], in_=vin[:, 3, :]), ins=[bvin, bvec], outs=[bvec])
        k.op("dve", lambda g: g.tensor_copy(out=vecf[:, 0, :], in_=vin[:, 5, :]), ins=[bvin], outs=[bvecf])
        k.op("dve", lambda g: g.memset(vecf[:, 1, :], 0.0), ins=[bvecf], outs=[bvecf])
        mid = contextlib.ExitStack()
        k.stack = mid
        mh2 = k.sb("mh2", [128, 16, NTOK], BF16); bmh2 = k.bufs(NTOK // NT)
        gT = k.sb("gT", [32, NTOK], F32); bgT = k.bufs(NTOK // NT)
        P = [k.ps("P%d" % i, [128, 512]) for i in range(8)]; bP = k.bufs(8)
        ACC = [0, 1, 2, 7]
        bout = k.buf()
        bxm = [[k.buf() for _ in range(NTOK // NT)] for _ in range(16)]
        wcnt = [0]
        ccnt = [0]
        acnt = [0]

        def accbank():
            i = ACC[acnt[0] % 4]
            acnt[0] += 1
            return P[i], bP[i]

        outer = k.stack
        with contextlib.ExitStack() as s1:
            k.stack = s1
            xt = k.sb("xt", [128, 16, NT], F32); bx = k.buf()
            yt = k.sb("yt", [128, 24, NT], BF16); byt = k.buf()
            sq = yt[:, 0:16, :]
            ysg = k.sb("ysg", [128, 8, NT], BF16); bysg = k.buf()
            mh = k.sb("mh", [128, 16, NT], BF16); bmh = k.buf()
            NW = 2
            wst = [k.sb("wst%d" % i, [128, 2048], F32) for i in range(NW)]; bwst = k.bufs(NW)
            wbf = [k.sb("wbf%d" % i, [128, 2048], BF16) for i in range(NW)]; bwbf = k.bufs(NW)
            pgt = [k.sb("pgt%d" % i, [128, 3, NT], F32) for i in range(1)] * 2; bpgt = [k.buf()] * 2
            sg = [k.sb("sg%d" % i, [128, NT], F32) for i in range(3)]; bsg = k.bufs(3)
            t1 = k.sb("t1", [128, NT], F32); t2 = k.sb("t2", [128, NT], F32); bt1 = k.buf(); bt2 = k.buf()
            rstd = k.sb("rstd", [128, NT], F32); brstd = k.buf()
            tmp = [t1, t2]; btmp = [bt1, bt2]
            h2f = [k.sb("h2f%d" % i, [128, NT], F32) for i in range(2)]; bh2f = k.bufs(2)
            rt = k.sb("rt", [128, 16, 36], F32); brt = k.buf()

            def wtile(view, shape):
                i = wcnt[0] % NW
                wcnt[0] += 1
                a, b = shape
                sv = wst[i][:, 0:a * b].rearrange("p (a b) -> p a b", b=b)
                bv = wbf[i][:, 0:a * b].rearrange("p (a b) -> p a b", b=b)
                k.dma(sv, view, outs=[bwst[i]])
                ce = ("dve", "act")[ccnt[0] % 2]
                ccnt[0] += 1
                if ce == "act":
                    k.op("act", lambda g: g.activation(out=bv, in_=sv, func=AF.Identity), ins=[bwst[i]], outs=[bwbf[i]])
                else:
                    k.op(ce, lambda g: g.tensor_copy(out=bv, in_=sv), ins=[bwst[i]], outs=[bwbf[i]])
                return bv, bwbf[i]

            for tt in range(NTOK // NT):
                tsl = slice(tt * NT, (tt + 1) * NT)
                k.dma(xt[:], xT_v[:, :, tsl], outs=[bx])
                k.dma(yt[:], yT_v[:, :, tsl], outs=[byt])
                for n in range(8):
                    wv, bwv = wtile(wglu_v[:, :, n * 128:(n + 1) * 128], (8, 128))
                    ps, bps = accbank()
                    for kc in range(8):
                        k.op("pe", lambda g, kc=kc: g.matmul(ps[:], lhsT=wv[:, kc, :], rhs=yt[:, 8 + kc, :], start=(kc == 0), stop=(kc == 7)), ins=[bwv, byt], outs=[bps])
                    k.op("act", lambda g: g.activation(out=sg[0][:], in_=ps[:], func=AF.Sigmoid), ins=[bps], outs=[bsg[0]])
                    k.op("dve", lambda g: g.tensor_tensor(out=ysg[:, n, :], in0=yt[:, 8 + n, :], in1=sg[0][:], op=ALU.mult), ins=[byt, bsg[0]], outs=[bysg])
                for n in range(16):
                    pgs = pgt[n % 2]; bpgs = bpgt[n % 2]
                    k.dma(pgs[:], pg_v[:, :, n, tsl], outs=[bpgs])
                    banks = []
                    for br in range(3):
                        wv, bwv = wtile(pw_v[br][:, :, n * 128:(n + 1) * 128], (8, 128))
                        ps, bps = accbank()
                        for kc in range(8):
                            rhs = ysg[:, kc, :] if br == 1 else yt[:, br * 8 + kc, :]
                            k.op("pe", lambda g, kc=kc, rhs=rhs: g.matmul(ps[:], lhsT=wv[:, kc, :], rhs=rhs, start=(kc == 0), stop=(kc == 7)),
                                 ins=[bwv, byt, bysg], outs=[bps])
                        k.op("act", lambda g, br=br: g.activation(out=sg[br][:], in_=pgs[:, br, :], func=AF.Sigmoid), ins=[bpgs], outs=[bsg[br]])
                        banks.append((ps, bps))
                    k.op("dve", lambda g: g.tensor_tensor(out=t1[:], in0=banks[0][0][:], in1=sg[0][:], op=ALU.mult), ins=[banks[0][1], bsg[0]], outs=[bt1])
                    k.op("dve", lambda g: g.tensor_tensor(out=t2[:], in0=banks[1][0][:], in1=sg[1][:], op=ALU.mult), ins=[banks[1][1], bsg[1]], outs=[bt2])
                    k.op("pool", lambda g: g.tensor_tensor(out=t1[:], in0=t1[:], in1=t2[:], op=ALU.add), ins=[bt1, bt2], outs=[bt1])
                    k.op("dve", lambda g: g.tensor_tensor(out=t2[:], in0=banks[2][0][:], in1=sg[2][:], op=ALU.mult), ins=[banks[2][1], bsg[2], bt1], outs=[bt2])
                    k.op("pool", lambda g: g.tensor_tensor(out=mh[:, n, :], in0=t1[:], in1=t2[:], op=ALU.add), ins=[bt1, bt2], outs=[bmh])
                for n in range(16):
                    wv, bwv = wtile(wout_v[:, :, n * 128:(n + 1) * 128], (16, 128))
                    ps, bps = accbank()
                    for kc in range(16):
                        k.op("pe", lambda g, kc=kc: g.matmul(ps[:], lhsT=wv[:, kc, :], rhs=mh[:, kc, :], start=(kc == 0), stop=(kc == 15)), ins=[bwv, bmh], outs=[bps])
                    k.op("dve", lambda g: g.scalar_tensor_tensor(out=xt[:, n, :], in0=ps[:], scalar=vin[:, 0, n:n + 1], in1=xt[:, n, :], op0=ALU.mult, op1=ALU.add),
                         ins=[bps, bvin, bx], outs=[bx])
                k.op("act", lambda g: g.activation(out=sq, in_=xt[:], func=AF.Square), ins=[bx], outs=[byt])
                for kc in range(16):
                    k.op("pe", lambda g, kc=kc: g.matmul(P[6][:], lhsT=ones_bf[:], rhs=sq[:, kc, :], start=(kc == 0), stop=(kc == 15)), ins=[bc, byt], outs=[bP[6]])
                k.op("act", lambda g: g.activation(out=rstd[:], in_=P[6][:], func=AF.Sqrt, scale=1.0 / D, bias=EPS), ins=[bP[6]], outs=[brstd])
                k.op("dve", lambda g: g.reciprocal(out=rstd[:], in_=rstd[:]), ins=[brstd], outs=[brstd])
                for kc in range(16):
                    i = kc % 2
                    k.op("dve", lambda g, kc=kc: g.tensor_tensor(out=tmp[i][:], in0=xt[:, kc, :], in1=rstd[:], op=ALU.mult), ins=[bx, brstd], outs=[btmp[i]])
                    k.op("act", lambda g, kc=kc: g.activation(out=h2f[i][:], in_=tmp[i][:], func=AF.Identity, scale=vec[:, 0, kc:kc + 1], bias=vec[:, 1, kc:kc + 1]),
                         ins=[btmp[i], bvec], outs=[bh2f[i]])
                    k.op("pool", lambda g, kc=kc: g.tensor_copy(out=mh2[:, kc, tsl], in_=h2f[i][:]), ins=[bh2f[i]], outs=[bmh2[tt]])
                    for j in range(4):
                        k.op("pe", lambda g, kc=kc, j=j: g.matmul(P[4][:, j * 36:(j + 1) * 36], lhsT=h2f[i][:, j * 128:(j + 1) * 128], rhs=wgrs[:, kc, :],
                                                                 start=(kc == 0 and j == 0), stop=(kc == 15), skip_group_check=True), ins=[bh2f[i], bwgr], outs=[bP[4]])
                R = lambda a, b_: rt[:, a, 0:b_]
                def dv(fn, extra=()):
                    k.op("dve", fn, ins=[brt, bP[4], bwgr] + list(extra), outs=[brt])
                for j in range(4):
                    lg = rt[:, 0, :]
                    dv(lambda g: g.tensor_tensor(out=lg, in0=P[4][:, j * 36:(j + 1) * 36], in1=bgrs[:], op=ALU.add))
                    dv(lambda g: g.tensor_reduce(out=R(1, 1), in_=lg[:, 0:4], axis=AX.X, op=ALU.max))
                    dv(lambda g: g.tensor_scalar(out=R(2, 4), in0=lg[:, 0:4], scalar1=R(1, 1), scalar2=None, op0=ALU.is_equal))
                    dv(lambda g: g.tensor_scalar(out=R(3, 1), in0=R(1, 1), scalar1=-1.0, scalar2=None, op0=ALU.mult))
                    k.op("act", lambda g: g.activation(out=R(4, 4), in_=lg[:, 0:4], func=AF.Exp, bias=R(3, 1)), ins=[brt], outs=[brt])
                    dv(lambda g: g.tensor_reduce(out=R(5, 1), in_=R(4, 4), axis=AX.X, op=ALU.add))
                    dv(lambda g: g.reciprocal(out=R(5, 1), in_=R(5, 1)))
                    dv(lambda g: g.tensor_scalar(out=R(6, 8), in0=lg[:, 4:12], scalar1=rt[:, 2, 0:1], scalar2=None, op0=ALU.mult))
                    for gi in range(1, 4):
                        dv(lambda g, gi=gi: g.scalar_tensor_tensor(out=R(6, 8), in0=lg[:, 4 + 8 * gi:12 + 8 * gi], scalar=rt[:, 2, gi:gi + 1], in1=R(6, 8),
                                                                   op0=ALU.mult, op1=ALU.add))
                    dv(lambda g: g.tensor_reduce(out=R(7, 1), in_=R(6, 8), axis=AX.X, op=ALU.max))
                    dv(lambda g: g.tensor_scalar(out=R(8, 8), in0=R(6, 8), scalar1=R(7, 1), scalar2=None, op0=ALU.is_equal))
                    dv(lambda g: g.scalar_tensor_tensor(out=R(9, 8), in0=R(8, 8), scalar=-1e30, in1=R(6, 8), op0=ALU.mult, op1=ALU.add))
                    dv(lambda g: g.tensor_reduce(out=R(10, 1), in_=R(9, 8), axis=AX.X, op=ALU.max))
                    dv(lambda g: g.tensor_scalar(out=R(11, 8), in0=R(9, 8), scalar1=R(10, 1), scalar2=None, op0=ALU.is_equal))
                    dv(lambda g: g.tensor_tensor(out=R(12, 1), in0=R(10, 1), in1=R(7, 1), op=ALU.subtract))
                    k.op("act", lambda g: g.activation(out=R(12, 1), in_=R(12, 1), func=AF.Exp), ins=[brt], outs=[brt])
                    dv(lambda g: g.tensor_scalar(out=R(13, 1), in0=R(12, 1), scalar1=1.0, scalar2=None, op0=ALU.add))
                    dv(lambda g: g.reciprocal(out=R(13, 1), in_=R(13, 1)))
                    dv(lambda g: g.tensor_tensor(out=R(13, 1), in0=R(13, 1), in1=R(5, 1), op=ALU.mult))
                    dv(lambda g: g.tensor_tensor(out=R(14, 1), in0=R(13, 1), in1=R(12, 1), op=ALU.mult))
                    dv(lambda g: g.tensor_scalar(out=R(15, 8), in0=R(8, 8), scalar1=R(13, 1), scalar2=None, op0=ALU.mult))
                    dv(lambda g: g.scalar_tensor_tensor(out=R(15, 8), in0=R(11, 8), scalar=R(14, 1), in1=R(15, 8), op0=ALU.mult, op1=ALU.add))
                    gts = rt[:, 1, 4:36]
                    for gi in range(4):
                        dv(lambda g, gi=gi: g.tensor_scalar(out=gts[:, gi * 8:(gi + 1) * 8], in0=R(15, 8), scalar1=rt[:, 2, gi:gi + 1], scalar2=None, op0=ALU.mult))
                    k.op("pe", lambda g: g.transpose(P[5][0:32, 0:128], gts, identf[:]), ins=[brt, bc], outs=[bP[5]])
                    k.op("act", lambda g, j=j: g.activation(out=gT[:, tt * NT + j * 128:tt * NT + (j + 1) * 128], in_=P[5][0:32, 0:128], func=AF.Identity), ins=[bP[5]], outs=[bgT[tt]])
                k.dma(xmid_v[:, :, tsl], xt[:], ins=[bx], outs=[bxm[n][tt] for n in range(16)], q="pool")
            k.barrier()
        k.stack = outer
        with contextlib.ExitStack() as s2:
            k.stack = s2
            hid = k.sb("hid", [128, 16, NTOK], BF16); bhid = k.bufs(NTOK // NT)
            wst = [k.sb("wstb%d" % i, [128, 4096], F32) for i in range(2)]; bwst = k.bufs(2)
            wbf = [k.sb("wbfb%d" % i, [128, 4096], BF16) for i in range(2)]; bwbf = k.bufs(2)
            gsel = [k.sb("gsel%d" % i, [32, NT], F32) for i in range(2)]; bgsel = k.bufs(2)
            gbs = [k.sb("gbs%d" % i, [128, NT], F32) for i in range(2)]; bgbs = k.bufs(2)
            t1 = k.sb("t1b", [128, NT], F32); t2 = k.sb("t2b", [128, NT], F32); bt1 = k.buf(); bt2 = k.buf()
            NX = 3
            xs = [k.sb("xs%d" % i, [128, NT], F32) for i in range(NX)]; bxs = k.bufs(NX)
            xcnt = [0]
            wcnt[0] = 0

            def wtile2(view, shape):
                i = wcnt[0] % 2
                wcnt[0] += 1
                a, b = shape
                sv = wst[i][:, 0:a * b].rearrange("p (a b) -> p a b", b=b)
                bv = wbf[i][:, 0:a * b].rearrange("p (a b) -> p a b", b=b)
                k.dma(sv, view, outs=[bwst[i]])
                ce = "dve"
                ccnt[0] += 1
                if ce == "act":
                    k.op("act", lambda g: g.activation(out=bv, in_=sv, func=AF.Identity), ins=[bwst[i]], outs=[bwbf[i]])
                else:
                    k.op(ce, lambda g: g.tensor_copy(out=bv, in_=sv), ins=[bwst[i]], outs=[bwbf[i]])
                return bv, bwbf[i]

            NG = 4
            for grp in range(NG):
                for el in range(8):
                    e = grp * 8 + el
                    w1v, bw1 = wtile2(w1_v[:, e, :, :], (16, 256))
                    w3v, bw3 = wtile2(w3_v[:, e, :, :], (16, 256))
                    for tt in range(NTOK // NT):
                        tsl = slice(tt * NT, (tt + 1) * NT)
                        gi = (e * 4 + tt) % 2
                        gsl = gbs[gi]; bgsl = bgbs[gi]
                        k.op("dve", lambda g, e=e, gi=gi, tsl=tsl: g.tensor_scalar(out=gsel[gi][:], in0=gT[:, tsl], scalar1=identf[0:32, e:e + 1], scalar2=None, op0=ALU.mult),
                             ins=[bc, bgT[tt]], outs=[bgsel[gi]])
                        k.op("pe", lambda g, gi=gi: g.matmul(P[3][:], lhsT=onesf[0:32, :], rhs=gsel[gi][:], start=True, stop=True), ins=[bc, bgsel[gi]], outs=[bP[3]])
                        k.op("act", lambda g, gsl=gsl: g.activation(out=gsl[:], in_=P[3][:], func=AF.Identity), ins=[bP[3]], outs=[bgsl])
                        for f in range(2):
                            pa, bpa = accbank()
                            pb, bpb = accbank()
                            for kc in range(16):
                                k.op("pe", lambda g, kc=kc, pa=pa, f=f, tsl=tsl: g.matmul(pa[:], lhsT=w1v[:, kc, f * 128:(f + 1) * 128], rhs=mh2[:, kc, tsl], start=(kc == 0), stop=(kc == 15)),
                                     ins=[bw1, bmh2[tt]], outs=[bpa])
                            for kc in range(16):
                                k.op("pe", lambda g, kc=kc, pb=pb, f=f, tsl=tsl: g.matmul(pb[:], lhsT=w3v[:, kc, f * 128:(f + 1) * 128], rhs=mh2[:, kc, tsl], start=(kc == 0), stop=(kc == 15)),
                                     ins=[bw3, bmh2[tt]], outs=[bpb])
                            k.op("act", lambda g, pa=pa: g.activation(out=t1[:], in_=pa[:], func=AF.Silu), ins=[bpa], outs=[bt1])
                            k.op("dve", lambda g, pb=pb: g.tensor_tensor(out=t2[:], in0=pb[:], in1=t1[:], op=ALU.mult), ins=[bpb, bt1], outs=[bt2])
                            k.op("pool", lambda g, el=el, f=f, tsl=tsl, gsl=gsl: g.tensor_tensor(out=hid[:, el * 2 + f, tsl], in0=t2[:], in1=gsl[:], op=ALU.mult),
                                 ins=[bt2, bgsl], outs=[bhid[tt]])
                for n in range(16):
                    wv, bwv = wtile2(w2_v[:, grp * 8:(grp + 1) * 8, :, n * 128:(n + 1) * 128].rearrange("p e f n -> p (e f) n"), (16, 128))
                    for tt in range(NTOK // NT):
                        tsl = slice(tt * NT, (tt + 1) * NT)
                        xi = xcnt[0] % NX
                        xcnt[0] += 1
                        k.dma(xs[xi][:], xmid_d[n * 128:(n + 1) * 128, tsl], ins=[bxm[n][tt]], outs=[bxs[xi]])
                        ps, bps = accbank()
                        for kk in range(16):
                            k.op("pe", lambda g, kk=kk, ps=ps, tsl=tsl: g.matmul(ps[:], lhsT=wv[:, kk, :], rhs=hid[:, kk, tsl], start=(kk == 0), stop=(kk == 15)), ins=[bwv, bhid[tt]], outs=[bps])
                        k.op("dve", lambda g, ps=ps, xi=xi, n=n: g.scalar_tensor_tensor(out=xs[xi][:], in0=ps[:], scalar=vin[:, 4, n:n + 1], in1=xs[xi][:], op0=ALU.mult, op1=ALU.add),
                             ins=[bps, bvin, bxs[xi]], outs=[bxs[xi]])
                        if grp < NG - 1 or last or withA:
                            k.dma(xmid_d[n * 128:(n + 1) * 128, tsl], xs[xi][:], ins=[bxs[xi]], outs=[bxm[n][tt]], q="pool")
                        if grp == NG - 1:
                            k.dma(xnT[n * 128:(n + 1) * 128, tsl], xs[xi][:], ins=[bxs[xi]], outs=[bout], q="pool")
            k.barrier()
        k.stack = outer
        if last:
            with contextlib.ExitStack() as s3:
                k.stack = s3
                xt = k.sb("xt3", [128, 16, NT], F32); bx = k.buf()
                sq = k.sb("sq3", [128, 16, NT], BF16); bsq = k.buf()
                rstd = k.sb("rstd3", [128, NT], F32); brstd = k.buf()
                tmp = k.sb("tmp3", [128, 2, NT], F32); btmp = k.bufs(2)
                h2f = [k.sb("o3_%d" % i, [128, NT], F32) for i in range(2)]; bh2f = k.bufs(2)
                for tt in range(NTOK // NT):
                    tsl = slice(tt * NT, (tt + 1) * NT)
                    k.dma(xt[:], xmid_v[:, :, tsl], ins=[bxm[n][tt] for n in range(16)], outs=[bx])
                    k.op("act", lambda g: g.activation(out=sq[:], in_=xt[:], func=AF.Square), ins=[bx], outs=[bsq])
                    for kc in range(16):
                        k.op("pe", lambda g, kc=kc: g.matmul(P[6][:], lhsT=ones_bf[:], rhs=sq[:, kc, :], start=(kc == 0), stop=(kc == 15)), ins=[bc, bsq], outs=[bP[6]])
                    k.op("act", lambda g: g.activation(out=rstd[:], in_=P[6][:], func=AF.Sqrt, scale=1.0 / D, bias=EPS), ins=[bP[6]], outs=[brstd])
                    k.op("dve", lambda g: g.reciprocal(out=rstd[:], in_=rstd[:]), ins=[brstd], outs=[brstd])
                    for kc in range(16):
                        i = kc % 2
                        k.op("dve", lambda g, kc=kc, i=i: g.tensor_tensor(out=tmp[:, i, :], in0=xt[:, kc, :], in1=rstd[:], op=ALU.mult), ins=[bx, brstd], outs=[btmp[i]])
                        k.op("act", lambda g, kc=kc, i=i: g.activation(out=h2f[i][:], in_=tmp[:, i, :], func=AF.Identity, scale=vecf[:, 0, kc:kc + 1], bias=vecf[:, 1, kc:kc + 1]),
                             ins=[btmp[i], bvecf], outs=[bh2f[i]])
                        k.dma(outT[kc * 128:(kc + 1) * 128, tsl], h2f[i][:], ins=[bh2f[i]], outs=[bout], q="pool")
                k.barrier()
            k.stack = outer
        mid.close()
        k.stack = st
        if withA:
            with contextlib.ExitStack() as s4:
                k.stack = s4
                emit_A(k, xmid_v, vecsA, wA.rearrange("(kc p) n -> p kc n", p=128), paA, pifA, pgA, bout,
                       xins=lambda tt: [bxm[n][tt] for n in range(16)], pfx="A_")
                k.barrier()
            k.stack = st
        k.finish([bout])
    return nc


_NC_CACHE = {}


def _get_nc(name, builder, *a):
    key = (name,) + a
    if key not in _NC_CACHE:
        _NC_CACHE[key] = builder(*a)
    return _NC_CACHE[key]


def _run(nc, in_maps):
    res = run_bass_kernel_spmd(nc, in_maps, core_ids=list(range(len(in_maps))))
    return res.results


def _pk(v):
    return np.ascontiguousarray(np.asarray(v, np.float32).reshape(16, 128).T)


def _c_inputs(c, l, xT_c, pg_c, yT_c, mod_l, inp):
    shift1, scale1, gate1, shift2, scale2, gate2 = np.split(mod_l, 6)
    vecs = np.stack([_pk(gate1), _pk(inp['norm_moe_g'][l]), _pk(scale2), _pk(shift2), _pk(gate2), _pk(inp['final_g']),
                     _pk(gate2), _pk(gate2)], axis=1)
    wgr = np.concatenate([inp['moe_w_group'][l], inp['moe_w_router'][l]], axis=1)
    bgr = np.tile(np.concatenate([inp['moe_b_group'][l], inp['moe_b_router'][l]])[None, :], (128, 1))
    return {"xT": xT_c, "pgT": pg_c, "yT": yT_c, "vecs": np.ascontiguousarray(vecs.astype(np.float32)),
            "wglu": np.ascontiguousarray(inp['s5_w_glu'][l]), "psb": np.ascontiguousarray(inp['p_sb'][l]),
            "ps5": np.ascontiguousarray(inp['p_s5'][l]), "pml": np.ascontiguousarray(inp['p_ml'][l]),
            "wout": np.ascontiguousarray(inp['w_out'][l]), "wgr": np.ascontiguousarray(wgr.astype(np.float32)),
            "bgr": np.ascontiguousarray(bgr.astype(np.float32)), "w1": np.ascontiguousarray(inp['moe_w1'][l]),
            "w3": np.ascontiguousarray(inp['moe_w3'][l]), "w2": np.ascontiguousarray(inp['moe_w2'][l])}


def kernel(**inp):
    inp = {k_: np.asarray(v) for k_, v in inp.items()}
    x = inp['x'][0]
    TS = T // NCORES
    wcat = np.concatenate([inp['w_ada'][0], inp['w_ada'][1]], axis=1)
    bcat = np.concatenate([inp['b_ada'][0], inp['b_ada'][1]])
    ncol = wcat.shape[1] // NCORES
    cT = _pk(inp['c'][0])
    r = _run(_get_nc("ada", build_ADA), [{"cT": cT, "w": np.ascontiguousarray(wcat[:, c * ncol:(c + 1) * ncol]),
                                          "b": np.ascontiguousarray(bcat[None, c * ncol:(c + 1) * ncol])} for c in range(NCORES)])
    mod = np.concatenate([np.asarray(r[c]["mod"])[0] for c in range(NCORES)])
    del wcat

    def vecs_A(l):
        mod_l = mod[l * 6 * D:(l + 1) * 6 * D]
        return np.ascontiguousarray(np.stack([_pk(inp['norm_mix_g'][l]), _pk(mod_l[D:2 * D]), _pk(mod_l[0:D])], axis=1))

    xT = [np.ascontiguousarray(x[c * TS:(c + 1) * TS].T) for c in range(NCORES)]
    wA = np.ascontiguousarray(inp['w_in'][0])
    vA = vecs_A(0)
    r = _run(_get_nc("A", build_A), [{"xT": xT[c], "vecs": vA, "w": wA} for c in range(NCORES)])
    out = None
    for l in range(DEPTH):
        mod_l = mod[l * 6 * D:(l + 1) * 6 * D]
        pa = np.concatenate([np.asarray(r[c]["pa"]).T for c in range(NCORES)], axis=0)
        pif = np.concatenate([np.asarray(r[c]["pif"]).T for c in range(NCORES)], axis=0)
        pg = [np.asarray(r[c]["pg"]) for c in range(NCORES)]
        del r
        P5 = [inp[k_][l] for k_ in ('s5_lam_re', 's5_lam_im', 's5_log_dt', 's5_b_re', 's5_b_im', 's5_c_re', 's5_c_im', 's5_d')]
        ims = []
        for c in range(NCORES):
            m = {"qT": np.ascontiguousarray(pa[:, c * 128:(c + 1) * 128].T), "kT": np.ascontiguousarray(pa[:, 1024 + c * 128:1024 + (c + 1) * 128].T),
                 "v": np.ascontiguousarray(pa[:, 2048 + c * 128:2048 + (c + 1) * 128])}
            m.update(s5_host_inputs(c, pa[:, 3072:4096], *P5))
            m.update(ml_host_inputs(c, l, pa[:, 4096:5120], pa[:, 5120:6144], pa[:, 6144:7168], pa[:, 7168:8192],
                                    pif[:, 0:4], pif[:, 4:8], inp['ml_conv'][l], inp['ml_i_bias'][l], inp['ml_f_bias'][l]))
            ims.append(m)
        r = _run(_get_nc("MIX", build_MIX), ims)
        ysb = np.concatenate([np.asarray(r[c]["ysb"]).T for c in range(NCORES)], axis=1)
        ys5 = np.concatenate([np.asarray(r[c]["ys5"]).transpose(2, 1, 0).reshape(T, 128) for c in range(NCORES)], axis=1)
        yml = np.concatenate([np.asarray(r[c]["yml"]).T for c in range(NCORES)], axis=1)
        yall = np.concatenate([ysb, ys5, yml], axis=1)
        del pa, ysb, ys5, yml, r, ims
        last = (l == DEPTH - 1)
        ims = [_c_inputs(c, l, xT[c], pg[c], np.ascontiguousarray(yall[c * TS:(c + 1) * TS].T), mod_l, inp) for c in range(NCORES)]
        if not last:
            wA = np.ascontiguousarray(inp['w_in'][l + 1])
            vA = vecs_A(l + 1)
            for m in ims:
                m["vecsA"] = vA
                m["wA"] = wA
        r = _run(_get_nc("C", build_C2, last, not last), ims)
        xT = [np.asarray(r[c]["xnT"]) for c in range(NCORES)]
        if last:
            out = np.concatenate([np.asarray(r[c]["outT"]).T for c in range(NCORES)], axis=0)
        del ims
    return np.ascontiguousarray(out[None].astype(np.float32))
```
